# Optimizing a Trainium2 kernel written in Bass

```python
import jax, jax.numpy as jnp
from jax import lax
import numpy as np

D_MODEL = 1024
BATCH = 16
SEQ = 2048
DEPTH = 1

RNN_WIDTH = 1024
RNN_BLOCKS = 8
RNN_BLOCK_W = RNN_WIDTH // RNN_BLOCKS
CONV_WIDTH = 4
LRU_C = 8.0
GLA_HEADS = 4
GLA_DK = D_MODEL // 2
GLA_DV = D_MODEL
GLA_HEAD_K = GLA_DK // GLA_HEADS
GLA_HEAD_V = GLA_DV // GLA_HEADS
GLA_RANK = 16
GLA_TAU = 16.0
GLA_CHUNK = 64
N_GROUPS = 4
EXPERTS_PER_GROUP = 8
N_EXPERTS = N_GROUPS * EXPERTS_PER_GROUP
TOP_K = 2
EXPERT_FF = 512
MOE_BLOCK = 128
DN_ALPHA = (2.0 * DEPTH) ** 0.25
DN_BETA = (8.0 * DEPTH) ** -0.25
LN_EPS = 1e-5
RMS_EPS = 1e-6
IN_SPLITS = (RNN_WIDTH, RNN_WIDTH, GLA_DK, GLA_DK, GLA_DV, GLA_DV, GLA_RANK, D_MODEL, D_MODEL)
IN_COL_BETA = (DN_BETA, 1.0, 1.0, 1.0, DN_BETA, 1.0, 1.0, 1.0, 1.0)
IN_COLS = sum(IN_SPLITS)
SPLIT_POINTS = tuple(np.cumsum(IN_SPLITS)[:-1].tolist())

kernel_name = "hawk_gla_hier_moe_deepnorm_block"


def layer_norm(x, g, b):
    xf = x.astype(jnp.float32)
    mu = jnp.mean(xf, axis=-1, keepdims=True)
    var = jnp.mean(jnp.square(xf - mu), axis=-1, keepdims=True)
    y = (xf - mu) * lax.rsqrt(var + LN_EPS) * g.astype(jnp.float32) + b.astype(jnp.float32)
    return y.astype(x.dtype)


def causal_depthwise_conv(u, w, b):
    S = u.shape[1]
    up = jnp.pad(u, ((0, 0), (CONV_WIDTH - 1, 0), (0, 0)))
    out = b
    for k in range(CONV_WIDTH):
        out = out + w[k] * up[:, k:k + S]
    return out


def _linear_combine(c1, c2):
    a1, b1 = c1
    a2, b2 = c2
    return a1 * a2, a2 * b1 + b2


def rg_lru(u, w_a, b_a, w_x, b_x, lam):
    B, S, _ = u.shape
    ub = u.reshape(B, S, RNN_BLOCKS, RNN_BLOCK_W)
    r = jax.nn.sigmoid(jnp.einsum('bshi,hij->bshj', ub, w_a).reshape(B, S, RNN_WIDTH) + b_a)
    i = jax.nn.sigmoid(jnp.einsum('bshi,hij->bshj', ub, w_x).reshape(B, S, RNN_WIDTH) + b_x)
    log_a = -LRU_C * r.astype(jnp.float32) * jax.nn.softplus(-lam.astype(jnp.float32))
    a = jnp.exp(log_a)
    mult = jnp.sqrt(-jnp.expm1(2.0 * log_a))
    bvals = mult * (i * u).astype(jnp.float32)
    _, h = lax.associative_scan(_linear_combine, (a, bvals), axis=1)
    return h.astype(u.dtype)


def gla_chunked(q, k, v, log_alpha):
    B, S, H, K = q.shape
    V = v.shape[-1]
    C = GLA_CHUNK
    n = S // C
    q = q.reshape(B, n, C, H, K)
    k = k.reshape(B, n, C, H, K)
    v = v.reshape(B, n, C, H, V)
    bcum = jnp.cumsum(log_alpha.reshape(B, n, C, H, K), axis=2)
    b_last = bcum[:, :, -1:]
    q_dec = q * jnp.exp(bcum)
    k_inv = k * jnp.exp(-bcum)
    k_end = k * jnp.exp(b_last - bcum)
    mask = jnp.tril(jnp.ones((C, C), dtype=bool))
    scores = jnp.einsum('bnthk,bnshk->bnhts', q_dec, k_inv)
    scores = jnp.where(mask, scores, 0.0)
    o_intra = jnp.einsum('bnhts,bnshv->bnthv', scores, v)
    chunk_decay = jnp.exp(b_last[:, :, 0])

    def step(state, inp):
        qd, ke, vc, dec = inp
        o = jnp.einsum('bthk,bhkv->bthv', qd, state)
        state = dec[..., None] * state + jnp.einsum('bshk,bshv->bhkv', ke, vc)
        return state, o

    xs = (jnp.moveaxis(q_dec, 1, 0), jnp.moveaxis(k_end, 1, 0),
          jnp.moveaxis(v, 1, 0), jnp.moveaxis(chunk_decay, 1, 0))
    state0 = jnp.zeros((B, H, K, V), jnp.float32)
    _, o_inter = lax.scan(step, state0, xs)
    o = o_intra + jnp.moveaxis(o_inter, 0, 1)
    return o.reshape(B, S, H, V)


def hierarchical_moe(xf, w_rg, b_rg, w_re, b_re, w1, w3, w2):
    N, D = xf.shape
    g_logits = (xf @ w_rg + b_rg).astype(jnp.float32)
    p_group = jax.nn.softmax(g_logits, axis=-1)
    grp = jnp.argmax(g_logits, axis=-1).astype(jnp.int32)
    p_grp_sel = jnp.take_along_axis(p_group, grp[:, None], axis=1)[:, 0]
    e_logits = (xf @ w_re + b_re).astype(jnp.float32).reshape(N, N_GROUPS, EXPERTS_PER_GROUP)
    e_sel = jnp.take_along_axis(e_logits, grp[:, None, None], axis=1)[:, 0]
    p_exp = jax.nn.softmax(e_sel, axis=-1)
    top_p, top_i = lax.top_k(p_exp, TOP_K)
    top_p = top_p / jnp.sum(top_p, axis=-1, keepdims=True)
    weights = p_grp_sel[:, None] * top_p
    expert_id = grp[:, None] * EXPERTS_PER_GROUP + top_i.astype(jnp.int32)

    e_flat = expert_id.reshape(-1)
    w_flat = weights.reshape(-1)
    tok_flat = jnp.repeat(jnp.arange(N, dtype=jnp.int32), TOP_K)
    order = jnp.argsort(e_flat)
    e_s, w_s, tok_s = e_flat[order], w_flat[order], tok_flat[order]
    counts = jnp.zeros((N_EXPERTS,), jnp.int32).at[e_flat].add(1)
    offsets = jnp.cumsum(counts) - counts
    pcounts = (counts + MOE_BLOCK - 1) // MOE_BLOCK * MOE_BLOCK
    pend = jnp.cumsum(pcounts)
    poffsets = pend - pcounts
    rank = jnp.arange(N * TOP_K, dtype=jnp.int32) - offsets[e_s]
    dest = poffsets[e_s] + rank
    n_blocks = -(-(N * TOP_K) // MOE_BLOCK) + N_EXPERTS
    P = n_blocks * MOE_BLOCK
    buf_tok = jnp.zeros((P,), jnp.int32).at[dest].set(tok_s)
    buf_w = jnp.zeros((P,), jnp.float32).at[dest].set(w_s)
    block_start = jnp.arange(n_blocks, dtype=jnp.int32) * MOE_BLOCK
    blk_e = jnp.minimum(jnp.searchsorted(pend, block_start, side='right'), N_EXPERTS - 1)

    def run_block(args):
        tok_b, w_b, e_b = args
        xb = xf[tok_b]
        h = jax.nn.silu(xb @ w1[e_b]) * (xb @ w3[e_b])
        return (h @ w2[e_b]) * w_b[:, None]

    y = lax.map(run_block, (buf_tok.reshape(n_blocks, MOE_BLOCK),
                            buf_w.reshape(n_blocks, MOE_BLOCK).astype(xf.dtype), blk_e))
    return jax.ops.segment_sum(y.reshape(P, D), buf_tok, num_segments=N)


def hybrid_layer(x, w_in, b_in, conv_w, conv_b, rg_w_a, rg_b_a, rg_w_x, rg_b_x, rg_lambda,
                 gla_w_a2, gla_b_a, gla_norm_g, w_proj_rnn, w_proj_gla, w_o, b_o,
                 ln1_g, ln1_b, router_w_group, router_b_group, router_w_expert,
                 router_b_expert, exp_w1, exp_w3, exp_w2, ln2_g, ln2_b):
    B, S, D = x.shape
    proj = x @ w_in + b_in
    rx, ry, q, k, v, g, alr, ga, gb = jnp.split(proj, SPLIT_POINTS, axis=-1)

    u = causal_depthwise_conv(rx, conv_w, conv_b)
    h = rg_lru(u, rg_w_a, rg_b_a, rg_w_x, rg_b_x, rg_lambda)
    out_a = (h * jax.nn.gelu(ry)) @ w_proj_rnn

    log_alpha = jax.nn.log_sigmoid((alr @ gla_w_a2 + gla_b_a).astype(jnp.float32)) / GLA_TAU
    qh = q.reshape(B, S, GLA_HEADS, GLA_HEAD_K).astype(jnp.float32) * (GLA_HEAD_K ** -0.5)
    kh = k.reshape(B, S, GLA_HEADS, GLA_HEAD_K).astype(jnp.float32)
    vh = v.reshape(B, S, GLA_HEADS, GLA_HEAD_V).astype(jnp.float32)
    la = log_alpha.reshape(B, S, GLA_HEADS, GLA_HEAD_K)
    o = gla_chunked(qh, kh, vh, la)
    o = o * lax.rsqrt(jnp.mean(jnp.square(o), axis=-1, keepdims=True) + RMS_EPS)
    o = o * gla_norm_g.astype(jnp.float32).reshape(GLA_HEADS, GLA_HEAD_V)
    o = o.reshape(B, S, GLA_DV).astype(x.dtype) * jax.nn.silu(g)
    out_b = o @ w_proj_gla

    merged = jax.nn.sigmoid(ga) * out_a + jax.nn.sigmoid(gb) * out_b
    y_mix = merged @ w_o + b_o
    x1 = layer_norm(DN_ALPHA * x + y_mix, ln1_g, ln1_b)

    y_moe = hierarchical_moe(x1.reshape(B * S, D), router_w_group, router_b_group,
                             router_w_expert, router_b_expert, exp_w1, exp_w3, exp_w2)
    x2 = layer_norm(DN_ALPHA * x1 + y_moe.reshape(B, S, D), ln2_g, ln2_b)
    return x2


def setup_inputs(seed: int = 0) -> dict:
    key = jax.random.key(seed)
    ks = jax.random.split(key, 28)
    L = DEPTH
    D = D_MODEL

    def nrm(k, shape, scale):
        return jax.random.normal(k, shape, jnp.float32) * scale

    col_scale = jnp.concatenate([jnp.full((n,), s, jnp.float32) for n, s in zip(IN_SPLITS, IN_COL_BETA)])
    a_c = jax.random.uniform(ks[9], (L, RNN_WIDTH), jnp.float32, minval=0.9, maxval=0.999)
    a0 = a_c ** (1.0 / LRU_C)
    return {
        "x": nrm(ks[0], (BATCH, SEQ, D), 1.0),
        "w_in": nrm(ks[1], (L, D, IN_COLS), D ** -0.5) * col_scale,
        "b_in": nrm(ks[2], (L, IN_COLS), 0.02),
        "conv_w": nrm(ks[3], (L, CONV_WIDTH, RNN_WIDTH), CONV_WIDTH ** -0.5),
        "conv_b": nrm(ks[4], (L, RNN_WIDTH), 0.02),
        "rg_w_a": nrm(ks[5], (L, RNN_BLOCKS, RNN_BLOCK_W, RNN_BLOCK_W), RNN_BLOCK_W ** -0.5),
        "rg_b_a": nrm(ks[6], (L, RNN_WIDTH), 0.02),
        "rg_w_x": nrm(ks[7], (L, RNN_BLOCKS, RNN_BLOCK_W, RNN_BLOCK_W), RNN_BLOCK_W ** -0.5),
        "rg_b_x": nrm(ks[8], (L, RNN_WIDTH), 0.02),
        "rg_lambda": jnp.log(a0) - jnp.log1p(-a0),
        "gla_w_a2": nrm(ks[10], (L, GLA_RANK, GLA_DK), GLA_RANK ** -0.5),
        "gla_b_a": nrm(ks[11], (L, GLA_DK), 0.1),
        "gla_norm_g": 1.0 + nrm(ks[12], (L, GLA_DV), 0.02),
        "w_proj_rnn": nrm(ks[13], (L, RNN_WIDTH, D), RNN_WIDTH ** -0.5 * DN_BETA),
        "w_proj_gla": nrm(ks[14], (L, GLA_DV, D), GLA_DV ** -0.5 * DN_BETA),
        "w_o": nrm(ks[15], (L, D, D), D ** -0.5 * DN_BETA),
        "b_o": nrm(ks[16], (L, D), 0.02),
        "ln1_g": 1.0 + nrm(ks[17], (L, D), 0.02),
        "ln1_b": nrm(ks[18], (L, D), 0.02),
        "router_w_group": nrm(ks[19], (L, D, N_GROUPS), D ** -0.5),
        "router_b_group": nrm(ks[20], (L, N_GROUPS), 0.01),
        "router_w_expert": nrm(ks[21], (L, D, N_EXPERTS), D ** -0.5),
        "router_b_expert": nrm(ks[22], (L, N_EXPERTS), 0.01),
        "exp_w1": nrm(ks[23], (L, N_EXPERTS, D, EXPERT_FF), D ** -0.5 * DN_BETA),
        "exp_w3": nrm(ks[24], (L, N_EXPERTS, D, EXPERT_FF), D ** -0.5 * DN_BETA),
        "exp_w2": nrm(ks[25], (L, N_EXPERTS, EXPERT_FF, D), EXPERT_FF ** -0.5 * DN_BETA),
        "ln2_g": 1.0 + nrm(ks[26], (L, D), 0.02),
        "ln2_b": nrm(ks[27], (L, D), 0.02),
    }


def reference(x, w_in, b_in, conv_w, conv_b, rg_w_a, rg_b_a, rg_w_x, rg_b_x, rg_lambda,
              gla_w_a2, gla_b_a, gla_norm_g, w_proj_rnn, w_proj_gla, w_o, b_o,
              ln1_g, ln1_b, router_w_group, router_b_group, router_w_expert,
              router_b_expert, exp_w1, exp_w3, exp_w2, ln2_g, ln2_b):
    h = x
    for l in range(DEPTH):
        h = hybrid_layer(h, w_in[l], b_in[l], conv_w[l], conv_b[l], rg_w_a[l], rg_b_a[l],
                         rg_w_x[l], rg_b_x[l], rg_lambda[l], gla_w_a2[l], gla_b_a[l],
                         gla_norm_g[l], w_proj_rnn[l], w_proj_gla[l], w_o[l], b_o[l],
                         ln1_g[l], ln1_b[l], router_w_group[l], router_b_group[l],
                         router_w_expert[l], router_b_expert[l], exp_w1[l], exp_w3[l],
                         exp_w2[l], ln2_g[l], ln2_b[l])
    return h
```

```python
import numpy as np
from contextlib import ExitStack
import concourse.bass as bass
import concourse.mybir as mybir
from concourse.bass_utils import run_bass_kernel_spmd

F32 = mybir.dt.float32
BF16 = mybir.dt.bfloat16
I32 = mybir.dt.int32
AF = mybir.ActivationFunctionType
ALU = mybir.AluOpType
AX = mybir.AxisListType

SAME_ENGINE_SYNC = True
DMA_RING = 12
NCORES = 8
NTOK = 4096
SEQ = 2048
CAP = 384
NEXP = 32
ALPHA = 2.0 ** 0.25
SBUF_WORDS = 44 * 1024

C_RX, C_RY, C_Q, C_K, C_V, C_G, C_ALR, C_GA, C_GB = 0, 1024, 2048, 2560, 3072, 4096, 5120, 5136, 6160
P_BRX, P_BRY, P_BQ, P_BK, P_BG, P_BALR, P_BGA, P_BGB = 0, 8, 16, 20, 24, 32, 33, 41
P_CW, P_CB, P_RBA, P_RBX, P_LAM, P_GBA, P_NG = 49, 81, 89, 97, 105, 113, 117
R_BV, R_BO, R_L1G, R_L1B, R_L2G, R_L2B, R_RB = 0, 1024, 2048, 3072, 4096, 5120, 6144
NRP = 6180


class Buf:
    __slots__ = ("w", "r")

    def __init__(self):
        self.w = None
        self.r = {}


class _Eng:
    def __init__(self, name, sem, dma_sems):
        self.name = name
        self.sem = sem
        self.count = 0
        self.seen = {}
        self.ops = []
        self.dma_sems = dma_sems
        self.dma_n = 0


class Prog:
    def __init__(self, nc, stack):
        self.nc = nc
        self.E = {}
        for name in ("pe", "act", "dve", "pool", "sp"):
            sem = stack.enter_context(nc.semaphore("s_" + name))
            dsems = []
            if name in ("sp", "pool"):
                dsems = [stack.enter_context(nc.semaphore("d_%s%d" % (name, i))) for i in range(DMA_RING)]
            self.E[name] = _Eng(name, sem, dsems)

    def op(self, eng, fn, reads=(), writes=(), dma=False):
        E = self.E[eng]
        deps = {}

        def add(t):
            if t is None:
                return
            if deps.get(t[0], (None, 0))[1] < t[1]:
                deps[t[0]] = t

        for b in reads:
            add(b.w)
        for b in writes:
            add(b.w)
            for t in b.r.values():
                add(t)
        waits = []
        for s, v in deps.values():
            if E.seen.get(s, 0) >= v:
                continue
            if s is E.sem and (eng == "pe" or not SAME_ENGINE_SYNC):
                continue
            waits.append((s, v))
            E.seen[s] = v
        if dma:
            slot = E.dma_n % DMA_RING
            sem = E.dma_sems[slot]
            val = 16 * (E.dma_n // DMA_RING + 1)
            if val > 16 and E.seen.get(sem, 0) < val - 16:
                waits.append((sem, val - 16))
                E.seen[sem] = val - 16
            E.dma_n += 1
            tok = (sem, val)
            inc = 16
        else:
            E.count += 1
            tok = (E.sem, E.count)
            inc = 1
        E.ops.append((waits, fn, tok[0], inc))
        for b in reads:
            if b.r.get(tok[0], (None, 0))[1] < tok[1]:
                b.r[tok[0]] = tok
        for b in writes:
            b.w = tok
            b.r = {}
        return tok

    def barrier(self):
        toks = []
        for E in self.E.values():
            if E.count:
                toks.append((E.sem, E.count))
            for i, s in enumerate(E.dma_sems):
                n = (E.dma_n - 1 - i) // DMA_RING + 1 if E.dma_n > i else 0
                if n > 0:
                    toks.append((s, 16 * n))
        for E in self.E.values():
            waits = []
            for s, v in toks:
                if s is E.sem or E.seen.get(s, 0) >= v:
                    continue
                waits.append((s, v))
                E.seen[s] = v
            if waits:
                E.ops.append((waits, None, None, 0))

    def emit(self, block):
        def run(E):
            def body(eng):
                for waits, fn, sem, inc in E.ops:
                    for s, v in waits:
                        eng.wait_ge(s, v)
                    if fn is not None:
                        fn(eng).then_inc(sem, inc)
            return body

        block.tensor(run(self.E["pe"]))
        block.scalar(run(self.E["act"]))
        block.vector(run(self.E["dve"]))
        block.gpsimd(run(self.E["pool"]))
        block.sync(run(self.E["sp"]))


def build(debug=False):
    nc = bass.Bass("TRN2", target_bir_lowering=False)

    def din(name, shape, dt=F32):
        return nc.dram_tensor(name, list(shape), dt, kind="ExternalInput").ap()

    xT = din("xT", [1024, NTOK]); xn = din("xn", [NTOK, 1024]); w_in = din("w_in", [1024, 7184])
    pp = din("pp", [128, 128]); rp = din("rp", [1, NRP]); wr = din("wr", [1024, 36])
    rgw = din("rgw", [2, 8, 128, 128]); wa2 = din("wa2", [16, 512])
    wrnn = din("wrnn", [1024, 1024]); wgla = din("wgla", [1024, 1024]); wo = din("wo", [1024, 1024])
    ew1 = din("ew1", [NEXP, 1024, 512]); ew3 = din("ew3", [NEXP, 1024, 512]); ew2 = din("ew2", [NEXP, 512, 1024])
    c_id = din("c_id", [128, 128]); c_lt = din("c_lt", [128, 128]); c_cm = din("c_cm", [128, 512])
    c_mc = din("c_mc", [128, 512]); c_eb = din("c_eb", [128, 32])
    out = nc.dram_tensor("out", [NTOK, 1024], F32, kind="ExternalOutput").ap()
    sk = "ExternalOutput" if debug else "Internal"
    AG = nc.dram_tensor("AG", [1024, NTOK], BF16, kind=sk).ap()
    MG = nc.dram_tensor("MG", [1024, NTOK], BF16, kind=sk).ap()
    X1S = nc.dram_tensor("X1S", [NTOK, 1024], F32, kind=sk).ap()
    XS = nc.dram_tensor("XS", [NEXP * CAP, 1024], BF16, kind=sk).ap()
    YS = nc.dram_tensor("YS", [NEXP * CAP, 1024], F32, kind=sk).ap()
    if debug:
        d_pos = nc.dram_tensor("d_pos", [128, 64], I32, kind="ExternalOutput").ap()
        d_wts = nc.dram_tensor("d_wts", [128, 64], F32, kind="ExternalOutput").ap()

    st = ExitStack()
    with st:
        big = st.enter_context(nc.sbuf_tensor("big", [128, SBUF_WORDS], F32))
        PSB = [st.enter_context(nc.psum_tensor("ps%d" % i, [128, 512], F32)) for i in range(8)]
        PSBUF = [Buf() for _ in range(8)]
        P = Prog(nc, st)
        off = [0]
        psn = [0]

        def view(shape, dt=F32):
            n = 1
            for s in shape[1:]:
                n *= s
            nw = (n * (2 if dt == BF16 else 4) + 3) // 4
            nw = (nw + 7) // 8 * 8
            assert off[0] + nw <= SBUF_WORDS, ("SBUF overflow", off[0], nw)
            v = big[:, off[0]:off[0] + nw]
            off[0] += nw
            if dt != F32:
                v = v.bitcast(dt)
            if dt == BF16 and n % 2:
                v = v[:, 0:n]
            elif dt == BF16:
                v = v[:, 0:n]
            else:
                v = v[:, 0:n]
            if len(shape) > 2:
                names = " ".join("a%d" % i for i in range(len(shape) - 1))
                kw = {"a%d" % i: shape[i + 1] for i in range(len(shape) - 1)}
                v = v.rearrange("p (%s) -> p %s" % (names, names), **kw)
            if shape[0] != 128:
                v = v[0:shape[0]]
            return v

        def next_ps():
            i = psn[0] % 8
            psn[0] += 1
            return PSB[i], PSBUF[i]

        def mm(o, lhsT, rhs, start, stop, reads, writes):
            P.op("pe", lambda e: e.matmul(o, lhsT=lhsT, rhs=rhs, start=start, stop=stop), reads, writes)

        def tp(o, in_, ident, reads, writes):
            P.op("pe", lambda e: e.transpose(o, in_, ident), reads, writes)

        def act(o, in_, func, reads, writes, bias=None, scale=None, accum=None):
            kw = {}
            if bias is not None:
                kw["bias"] = bias
            if scale is not None:
                kw["scale"] = scale
            if accum is not None:
                kw["accum_out"] = accum
            P.op("act", lambda e: e.activation(out=o, in_=in_, func=func, **kw), reads, writes)

        def ts(eng, o, in0, s1, s2, op0, op1, reads, writes):
            if op1 is None:
                P.op(eng, lambda e: e.tensor_scalar(out=o, in0=in0, scalar1=s1, scalar2=None, op0=op0), reads, writes)
            else:
                P.op(eng, lambda e: e.tensor_scalar(out=o, in0=in0, scalar1=s1, scalar2=s2, op0=op0, op1=op1), reads, writes)

        def tt(eng, o, in0, in1, op, reads, writes):
            P.op(eng, lambda e: e.tensor_tensor(out=o, in0=in0, in1=in1, op=op), reads, writes)

        def stt(eng, o, in0, sc, in1, op0, op1, reads, writes):
            P.op(eng, lambda e: e.scalar_tensor_tensor(out=o, in0=in0, scalar=sc, in1=in1, op0=op0, op1=op1), reads, writes)

        def cp(eng, o, in_, reads, writes):
            P.op(eng, lambda e: e.tensor_copy(out=o, in_=in_), reads, writes)

        def ms(eng, o, val, writes):
            P.op(eng, lambda e: e.memset(o, val), (), writes)

        def dma(eng, o, in_, reads, writes):
            P.op(eng, lambda e: e.dma_start(out=o, in_=in_), reads, writes, dma=True)

        def wview(w, c0, c1):
            return w[:, c0:c1].rearrange("(kt p) n -> p kt n", p=128)

        PP = view([128, 128]); B_PP = Buf()
        DER = view([128, 32]); B_DER = Buf()
        TMP8 = view([128, 8]); B_TMP8 = Buf()
        IDB = view([128, 128], BF16); IDF = view([128, 128]); LTB = view([128, 128], BF16)
        ALL1 = view([128, 128], BF16); ONES256 = view([128, 128], BF16); ONE1 = view([128, 128], BF16)
        CMB = view([128, 512], BF16); MCF = view([128, 512]); CNTB = view([128, 32])
        POS = view([128, 32, 2], I32); WTS = view([128, 32, 2])
        B_CONST = Buf(); B_CNT = Buf(); B_POS = Buf(); B_WTS = Buf()
        dma("sp", PP, pp, (), [B_PP])
        dma("sp", IDF, c_id, (), [B_CONST])
        dma("sp", MCF, c_mc, (), [B_CONST])
        dma("sp", CNTB, c_eb, (), [B_CNT])
        dma("pool", IDB, c_id, (), [B_CONST])
        dma("pool", LTB, c_lt, (), [B_CONST])
        dma("pool", CMB, c_cm, (), [B_CONST])
        ms("dve", ALL1, 1.0, [B_CONST])
        ms("dve", ONES256, 1.0 / 256.0, [B_CONST])
        ms("dve", ONE1, 1.0, [B_CONST])
        act(TMP8, PP[:, P_LAM:P_LAM + 8], AF.Exp, [B_PP], [B_TMP8], scale=-1.0)
        act(TMP8, TMP8, AF.Ln, [B_TMP8], [B_TMP8], bias=1.0)
        ts("dve", DER[:, 0:8], TMP8, -8.0, None, ALU.mult, None, [B_TMP8], [B_DER])
        ts("dve", DER[:, 8:16], TMP8, -16.0, None, ALU.mult, None, [B_TMP8], [B_DER])
        ts("dve", DER[:, 16:20], PP[:, P_GBA:P_GBA + 4], -1.0, None, ALU.mult, None, [B_PP], [B_DER])
        g_mark = off[0]
        PR = [B_PP, B_DER]

        def pcol(c):
            return PP[:, c:c + 1]

        W1 = view([128, 8, 3072], BF16); B_W1 = [Buf() for _ in range(3)]
        WRNN = view([128, 8, 1024], BF16); B_WRNN = Buf()
        RGW = view([128, 2, 8, 128], BF16); B_RGW = Buf()
        XT = [view([128, 8, 512], BF16) for _ in range(2)]; B_XT = [Buf() for _ in range(2)]
        RX = view([128, 4, 516]); B_RX = [Buf() for _ in range(4)]
        U = view([128, 4, 512]); B_U = [Buf() for _ in range(4)]
        UBF = view([128, 4, 512], BF16); B_UBF = [Buf() for _ in range(4)]
        TMPC = view([128, 4, 512]); B_TMPC = [Buf() for _ in range(4)]
        THR = view([128, 4, 512]); B_THR = [Buf() for _ in range(4)]
        A2 = view([128, 4, 512]); B_A2 = [Buf() for _ in range(4)]
        THI = view([128, 4, 512]); B_THI = [Buf() for _ in range(4)]
        GY = view([128, 4, 512], BF16); B_GY = [Buf() for _ in range(4)]
        HG = view([128, 8, 512], BF16); B_HG = [Buf() for _ in range(8)]
        HALO = view([128, 8, 4]); B_HALO = [Buf() for _ in range(8)]
        HST = view([128, 8]); B_HST = [Buf() for _ in range(8)]
        SGA = [view([128, 512]) for _ in range(2)]; B_SGA = [Buf() for _ in range(2)]
        AGO = [view([128, 512], BF16) for _ in range(2)]; B_AGO = [Buf() for _ in range(2)]
        B_AG = Buf()

        for i, c0 in enumerate((C_RX, C_RY, C_GA)):
            dma("pool", W1[:, :, i * 1024:(i + 1) * 1024], wview(w_in, c0, c0 + 1024), (), [B_W1[i]])
        dma("pool", RGW, rgw.rearrange("g h i j -> i g h j"), (), [B_RGW])
        dma("pool", WRNN, wview(wrnn, 0, 1024), (), [B_WRNN])

        n_ag = 0
        for j in range(NTOK // 512):
            t0 = j * 512
            first = (j % 4 == 0)
            xt = XT[j % 2]; bxt = B_XT[j % 2]
            dma("pool", xt, xT[:, t0:t0 + 512].rearrange("(kt p) n -> p kt n", p=128), (), [bxt])
            for grp in range(2):
                for ci in range(4):
                    c = grp * 4 + ci
                    ps, bps = next_ps()
                    for kt in range(8):
                        mm(ps[:, :], W1[:, kt, c * 128:(c + 1) * 128], xt[:, kt, :], kt == 0, kt == 7,
                           [B_W1[0], bxt], [bps])
                    if first:
                        ms("pool", RX[:, ci, 0:3], 0.0, [B_RX[ci]])
                    else:
                        cp("pool", RX[:, ci, 0:3], HALO[:, c, 0:3], [B_HALO[c]], [B_RX[ci]])
                    act(RX[:, ci, 3:515], ps[:, :], AF.Identity, [bps] + PR, [B_RX[ci]], bias=pcol(P_BRX + c))
                    cp("pool", HALO[:, c, 0:3], RX[:, ci, 512:515], [B_RX[ci]], [B_HALO[c]])
                    ts("pool", U[:, ci, :], RX[:, ci, 3:515], pcol(P_CW + c * 4 + 3), pcol(P_CB + c),
                       ALU.mult, ALU.add, [B_RX[ci]] + PR, [B_U[ci]])
                    ts("pool", TMPC[:, ci, :], RX[:, ci, 0:512], pcol(P_CW + c * 4 + 0), None, ALU.mult, None,
                       [B_RX[ci]] + PR, [B_TMPC[ci]])
                    tt("pool", U[:, ci, :], U[:, ci, :], TMPC[:, ci, :], ALU.add, [B_U[ci], B_TMPC[ci]], [B_U[ci]])
                    for k in (1, 2):
                        stt("dve", U[:, ci, :], RX[:, ci, k:k + 512], pcol(P_CW + c * 4 + k), U[:, ci, :],
                            ALU.mult, ALU.add, [B_RX[ci], B_U[ci]] + PR, [B_U[ci]])
                    act(UBF[:, ci, :], U[:, ci, :], AF.Copy, [B_U[ci]], [B_UBF[ci]])
                for ci in range(4):
                    c = grp * 4 + ci
                    ps, bps = next_ps()
                    for kt in range(8):
                        mm(ps[:, :], W1[:, kt, 1024 + c * 128:1024 + (c + 1) * 128], xt[:, kt, :], kt == 0, kt == 7,
                           [B_W1[1], bxt], [bps])
                    act(GY[:, ci, :], ps[:, :], AF.Gelu_apprx_tanh, [bps] + PR, [B_GY[ci]], bias=pcol(P_BRY + c))
                for ci in range(4):
                    c = grp * 4 + ci
                    ps, bps = next_ps()
                    mm(ps[:, :], RGW[:, 0, c, :], UBF[:, ci, :], True, True, [B_RGW, B_UBF[ci]], [bps])
                    act(THR[:, ci, :], ps[:, :], AF.Sigmoid, [bps] + PR, [B_THR[ci]], bias=pcol(P_RBA + c))
                    ps, bps = next_ps()
                    mm(ps[:, :], RGW[:, 1, c, :], UBF[:, ci, :], True, True, [B_RGW, B_UBF[ci]], [bps])
                    act(THI[:, ci, :], ps[:, :], AF.Sigmoid, [bps] + PR, [B_THI[ci]], bias=pcol(P_RBX + c))
                for ci in range(4):
                    c = grp * 4 + ci
                    act(A2[:, ci, :], THR[:, ci, :], AF.Exp, [B_THR[ci]] + PR, [B_A2[ci]], scale=DER[:, 8 + c:9 + c])
                    act(THR[:, ci, :], THR[:, ci, :], AF.Exp, [B_THR[ci]] + PR, [B_THR[ci]], scale=DER[:, c:c + 1])
                for ci in range(4):
                    ts("dve", A2[:, ci, :], A2[:, ci, :], 0.99999994, -1.0, ALU.min, ALU.mult, [B_A2[ci]], [B_A2[ci]])
                for ci in range(4):
                    act(A2[:, ci, :], A2[:, ci, :], AF.Sqrt, [B_A2[ci]], [B_A2[ci]], bias=1.0)
                for ci in range(4):
                    c = grp * 4 + ci
                    tt("dve", THI[:, ci, :], THI[:, ci, :], U[:, ci, :], ALU.mult, [B_THI[ci], B_U[ci]], [B_THI[ci]])
                    tt("dve", THI[:, ci, :], THI[:, ci, :], A2[:, ci, :], ALU.mult, [B_A2[ci], B_THI[ci]], [B_THI[ci]])
                    init = 0.0 if first else HST[:, c:c + 1]
                    P.op("dve", lambda e, o=U[:, ci, :], d0=THR[:, ci, :], d1=THI[:, ci, :], ini=init:
                         e.tensor_tensor_scan(out=o, data0=d0, data1=d1, initial=ini, op0=ALU.mult, op1=ALU.add),
                         [B_THR[ci], B_THI[ci], B_HST[c], B_U[ci]], [B_U[ci]])
                    cp("dve", HST[:, c:c + 1], U[:, ci, 511:512], [B_U[ci]], [B_HST[c]])
                    tt("dve", HG[:, c, :], U[:, ci, :], GY[:, ci, :], ALU.mult, [B_U[ci], B_GY[ci]], [B_HG[c]])
            for dt in range(8):
                ps, bps = next_ps()
                for kt in range(8):
                    mm(ps[:, :], W1[:, kt, 2048 + dt * 128:2048 + (dt + 1) * 128], xt[:, kt, :], kt == 0, kt == 7,
                       [B_W1[2], bxt], [bps])
                sga = SGA[n_ag % 2]; bsga = B_SGA[n_ag % 2]
                ago = AGO[n_ag % 2]; bago = B_AGO[n_ag % 2]
                n_ag += 1
                act(sga, ps[:, :], AF.Sigmoid, [bps] + PR, [bsga], bias=pcol(P_BGA + dt))
                ps2, bps2 = next_ps()
                for c in range(8):
                    mm(ps2[:, :], WRNN[:, c, dt * 128:(dt + 1) * 128], HG[:, c, :], c == 0, c == 7,
                       [B_WRNN, B_HG[c]], [bps2])
                tt("dve", ago, ps2[:, :], sga, ALU.mult, [bps2, bsga], [bago])
                dma("sp", AG[dt * 128:(dt + 1) * 128, t0:t0 + 512], ago, [bago], [B_AG])
        P.barrier()

        off[0] = g_mark
        TB = 256
        NCH = TB // 128
        NW2 = 4112
        W2 = view([128, 8, NW2], BF16); B_W2 = Buf()
        o_q, o_k, o_v, o_g, o_alr, o_gb = 0, 512, 1024, 2048, 3072, 3088
        WGLA = view([128, 8, 1024], BF16); B_WGLA = Buf()
        WA2 = view([16, 512], BF16); B_WA2 = Buf()
        BVB = view([1, 1024], BF16); B_BVB = Buf()
        XT2 = [view([128, 8, TB], BF16) for _ in range(2)]; B_XT2 = [Buf() for _ in range(2)]
        ALRB = view([16, TB], BF16); B_ALRB = Buf()
        ECS = view([128, 4, TB]); B_ECS = [Buf() for _ in range(4)]
        CS = view([128, 4, TB]); B_CS = [Buf() for _ in range(4)]
        EB = [view([128, TB]) for _ in range(2)]; B_EB = [Buf() for _ in range(2)]
        EINV = [view([128, TB]) for _ in range(2)]; B_EINV = [Buf() for _ in range(2)]
        EBL = view([128, 4, NCH]); B_EBL = [Buf() for _ in range(4)]
        QD = view([128, 4, TB], BF16); B_QD = [Buf() for _ in range(4)]
        KI = view([128, 4, TB], BF16); B_KI = [Buf() for _ in range(4)]
        KT = view([128, NCH, 512], BF16); B_KT = [Buf() for _ in range(NCH)]
        VT = view([128, NCH, 1024], BF16); B_VT = [Buf() for _ in range(NCH)]
        SC = view([128, NCH, 512], BF16); B_SC = [Buf() for _ in range(NCH)]
        S = view([128, 4, 256]); B_S = [Buf() for _ in range(4)]
        SBF = view([128, 4, 256], BF16); B_SBF = [Buf() for _ in range(4)]
        T1 = view([128, 4, 256]); B_T1 = [Buf() for _ in range(4)]
        SQ = view([128, 2, 512], BF16); B_SQ = [Buf() for _ in range(2)]
        RSTD = view([128, 512]); B_RSTD = Buf()
        ON = view([128, 8, TB]); B_ON = [Buf() for _ in range(8)]
        SG = view([128, 8, TB]); B_SG = [Buf() for _ in range(8)]
        OFIN = view([128, 8, TB], BF16); B_OFIN = [Buf() for _ in range(8)]
        AGI = view([128, 8, TB], BF16); B_AGI = Buf()
        SGB = [view([128, TB]) for _ in range(2)]; B_SGB = [Buf() for _ in range(2)]
        TBB = [view([128, TB]) for _ in range(2)]; B_TBB = [Buf() for _ in range(2)]
        B_MG = Buf()

        for (o0, c0, n) in ((o_q, C_Q, 1024), (o_v, C_V, 1024), (o_g, C_G, 1024), (o_gb, C_GB, 1024)):
            dma("pool", W2[:, :, o0:o0 + n], wview(w_in, c0, c0 + n), (), [B_W2])
        dma("pool", W2[:, :, o_alr:o_alr + 16], wview(w_in, C_ALR, C_ALR + 16), (), [B_W2])
        dma("pool", WGLA, wview(wgla, 0, 1024), (), [B_WGLA])
        dma("pool", WA2, wa2, (), [B_WA2])
        dma("pool", BVB, rp[:, R_BV:R_BV + 1024], (), [B_BVB])

        n_mg = 0
        for j in range(NTOK // TB):
            t0 = j * TB
            first = (t0 % SEQ == 0)
            xt = XT2[j % 2]; bxt = B_XT2[j % 2]
            dma("pool", xt, xT[:, t0:t0 + TB].rearrange("(kt p) n -> p kt n", p=128), (), [bxt])
            dma("sp", AGI, AG[:, t0:t0 + TB].rearrange("(dt p) n -> p dt n", p=128), [B_AG], [B_AGI])
            ps, bps = next_ps()
            for kt in range(8):
                mm(ps[0:16, 0:TB], W2[:, kt, o_alr:o_alr + 16], xt[:, kt, :], kt == 0, kt == 7, [B_W2, bxt], [bps])
            act(ALRB, ps[0:16, 0:TB], AF.Identity, [bps] + PR, [B_ALRB], bias=PP[0:16, P_BALR:P_BALR + 1])
            for hd in range(4):
                psz, bpsz = next_ps()
                mm(psz[:, 0:TB], WA2[:, hd * 128:(hd + 1) * 128], ALRB, True, True, [B_WA2, B_ALRB], [bpsz])
                act(ECS[:, hd, :], psz[:, 0:TB], AF.Exp, [bpsz] + PR, [B_ECS[hd]], bias=DER[:, 16 + hd:17 + hd], scale=-1.0)
            for hd in range(4):
                act(ECS[:, hd, :], ECS[:, hd, :], AF.Ln, [B_ECS[hd]], [B_ECS[hd]], bias=1.0)
            for hd in range(4):
                P.op("dve", lambda e, o=CS[:, hd, :], d0=MCF[:, 0:TB], d1=ECS[:, hd, :]:
                     e.tensor_tensor_scan(out=o, data0=d0, data1=d1, initial=0.0, op0=ALU.mult, op1=ALU.add),
                     [B_ECS[hd], B_CONST], [B_CS[hd]])
            for hd in range(4):
                psq, bpsq = next_ps()
                for kt in range(8):
                    mm(psq[:, 0:TB], W2[:, kt, o_q + hd * 128:o_q + (hd + 1) * 128], xt[:, kt, :], kt == 0, kt == 7,
                       [B_W2, bxt], [bpsq])
                psk, bpsk = next_ps()
                for kt in range(8):
                    mm(psk[:, 0:TB], W2[:, kt, o_k + hd * 128:o_k + (hd + 1) * 128], xt[:, kt, :], kt == 0, kt == 7,
                       [B_W2, bxt], [bpsk])
                eb = EB[hd % 2]; beb = B_EB[hd % 2]
                einv = EINV[hd % 2]; beinv = B_EINV[hd % 2]
                act(eb, CS[:, hd, :], AF.Exp, [B_CS[hd]], [beb], scale=-1.0 / 16.0, bias=float(-0.5 * np.log(128.0)))
                act(einv, CS[:, hd, :], AF.Exp, [B_CS[hd]], [beinv], scale=1.0 / 16.0)
                act(EBL[:, hd, :], CS[:, hd, :].rearrange("p (c t) -> p c t", t=128)[:, :, 127], AF.Exp, [B_CS[hd]],
                    [B_EBL[hd]], scale=-1.0 / 16.0)
                stt("dve", QD[:, hd, :], psq[:, 0:TB], pcol(P_BQ + hd), eb, ALU.add, ALU.mult,
                    [bpsq, beb] + PR, [B_QD[hd]])
                stt("dve", KI[:, hd, :], psk[:, 0:TB], pcol(P_BK + hd), einv, ALU.add, ALU.mult,
                    [bpsk, beinv] + PR, [B_KI[hd]])
            for c in range(NCH):
                pst, bpst = next_ps()
                pstb = pst[:, :].bitcast(BF16)
                for hd in range(4):
                    tp(pstb[:, hd * 128:(hd + 1) * 128], KI[:, hd, c * 128:(c + 1) * 128], IDB,
                       [B_KI[hd], B_CONST], [bpst])
                act(KT[:, c, :], pstb[:, 0:512], AF.Copy, [bpst], [B_KT[c]])
                for half in range(2):
                    psv, bpsv = next_ps()
                    for kt in range(8):
                        mm(psv[:, :], xt[:, kt, c * 128:(c + 1) * 128],
                           W2[:, kt, o_v + half * 512:o_v + (half + 1) * 512], kt == 0, False, [B_W2, bxt], [bpsv])
                    mm(psv[:, :], ONE1[0:1, :], BVB[0:1, half * 512:(half + 1) * 512], False, True,
                       [B_CONST, B_BVB], [bpsv])
                    act(VT[:, c, half * 512:(half + 1) * 512], psv[:, :], AF.Copy, [bpsv], [B_VT[c]])
                pss, bpss = next_ps()
                for hd in range(4):
                    mm(pss[:, hd * 128:(hd + 1) * 128], KI[:, hd, c * 128:(c + 1) * 128],
                       QD[:, hd, c * 128:(c + 1) * 128], True, True, [B_KI[hd], B_QD[hd]], [bpss])
                tt("dve", SC[:, c, :], pss[:, :], CMB, ALU.mult, [bpss, B_CONST], [B_SC[c]])
            if first:
                for hd in range(4):
                    ms("dve", S[:, hd, :], 0.0, [B_S[hd]])
                    ms("dve", SBF[:, hd, :], 0.0, [B_SBF[hd]])
            for c in range(NCH):
                pso = [next_ps(), next_ps()]
                for hd in range(4):
                    po, bpo = pso[hd // 2]
                    for vh in range(2):
                        col = ((hd % 2) * 2 + vh) * 128
                        mm(po[:, col:col + 128], VT[:, c, hd * 256 + vh * 128:hd * 256 + (vh + 1) * 128],
                           SC[:, c, hd * 128:(hd + 1) * 128], True, False, [B_VT[c], B_SC[c]], [bpo])
                        mm(po[:, col:col + 128], SBF[:, hd, vh * 128:(vh + 1) * 128],
                           QD[:, hd, c * 128:(c + 1) * 128], False, True, [B_SBF[hd], B_QD[hd]], [bpo])
                    pkv, bpkv = next_ps()
                    mm(pkv[:, 0:256], KT[:, c, hd * 128:(hd + 1) * 128], VT[:, c, hd * 256:(hd + 1) * 256],
                       True, True, [B_KT[c], B_VT[c]], [bpkv])
                    act(T1[:, hd, :], S[:, hd, :], AF.Identity, [B_S[hd], B_EBL[hd]], [B_T1[hd]],
                        scale=EBL[:, hd, c:c + 1])
                    stt("dve", S[:, hd, :], pkv[:, 0:256], EBL[:, hd, c:c + 1], T1[:, hd, :], ALU.mult, ALU.add,
                        [bpkv, B_EBL[hd], B_T1[hd]], [B_S[hd]])
                    act(SBF[:, hd, :], S[:, hd, :], AF.Copy, [B_S[hd]], [B_SBF[hd]])
                for hh in range(2):
                    po, bpo = pso[hh]
                    act(SQ[:, hh, :], po[:, :], AF.Square, [bpo], [B_SQ[hh]])
                pn, bpn = next_ps()
                for hd in range(4):
                    for vh in range(2):
                        col = ((hd % 2) * 2 + vh) * 128
                        mm(pn[:, hd * 128:(hd + 1) * 128], ONES256, SQ[:, hd // 2, col:col + 128], vh == 0, vh == 1,
                           [B_CONST, B_SQ[hd // 2]], [bpn])
                act(RSTD, pn[:, :], AF.Sqrt, [bpn], [B_RSTD], bias=1e-6)
                P.op("dve", lambda e: e.reciprocal(out=RSTD, in_=RSTD), [B_RSTD], [B_RSTD])
                for hd in range(4):
                    po, bpo = pso[hd // 2]
                    for vh in range(2):
                        col = ((hd % 2) * 2 + vh) * 128
                        vt = hd * 2 + vh
                        tt("dve", ON[:, vt, c * 128:(c + 1) * 128], po[:, col:col + 128],
                           RSTD[:, hd * 128:(hd + 1) * 128], ALU.mult, [bpo, B_RSTD], [B_ON[vt]])
            for vt in range(8):
                ps, bps = next_ps()
                for kt in range(8):
                    mm(ps[:, 0:TB], W2[:, kt, o_g + vt * 128:o_g + (vt + 1) * 128], xt[:, kt, :], kt == 0, kt == 7,
                       [B_W2, bxt], [bps])
                act(SG[:, vt, :], ps[:, 0:TB], AF.Silu, [bps] + PR, [B_SG[vt]], bias=pcol(P_BG + vt))
                tt("pool", ON[:, vt, :], ON[:, vt, :], SG[:, vt, :], ALU.mult, [B_ON[vt], B_SG[vt]], [B_ON[vt]])
                ts("pool", OFIN[:, vt, :], ON[:, vt, :], pcol(P_NG + vt), None, ALU.mult, None,
                   [B_ON[vt]] + PR, [B_OFIN[vt]])
            for dt in range(8):
                ps, bps = next_ps()
                for kt in range(8):
                    mm(ps[:, 0:TB], W2[:, kt, o_gb + dt * 128:o_gb + (dt + 1) * 128], xt[:, kt, :], kt == 0, kt == 7,
                       [B_W2, bxt], [bps])
                sgb = SGB[n_mg % 2]; bsgb = B_SGB[n_mg % 2]
                tbb = TBB[n_mg % 2]; btbb = B_TBB[n_mg % 2]
                n_mg += 1
                act(sgb, ps[:, 0:TB], AF.Sigmoid, [bps] + PR, [bsgb], bias=pcol(P_BGB + dt))
                ps2, bps2 = next_ps()
                for vt in range(8):
                    mm(ps2[:, 0:TB], WGLA[:, vt, dt * 128:(dt + 1) * 128], OFIN[:, vt, :], vt == 0, vt == 7,
                       [B_WGLA, B_OFIN[vt]], [bps2])
                tt("dve", tbb, ps2[:, 0:TB], sgb, ALU.mult, [bps2, bsgb], [btbb])
                tt("pool", AGI[:, dt, :], tbb, AGI[:, dt, :], ALU.add, [btbb, B_AGI], [B_AGI])
            dma("sp", MG[:, t0:t0 + TB].rearrange("(dt p) n -> p dt n", p=128), AGI, [B_AGI], [B_MG])
        P.barrier()

        off[0] = g_mark
        WO = view([128, 8, 1024], BF16); B_WO = Buf()
        WRS = view([128, 8, 36]); B_WRS = Buf()
        RPB = view([128, 3 * 1024]); B_RPB = Buf()
        RBB = view([128, 36]); B_RBB = Buf()
        MGI = [view([128, 8, 512], BF16) for _ in range(2)]; B_MGI = [Buf() for _ in range(2)]
        XTM = [view([128, 1024]) for _ in range(2)]; B_XTM = [Buf() for _ in range(2)]
        Z = [view([128, 1024]) for _ in range(2)]; B_Z = [Buf() for _ in range(2)]
        X1B = [view([128, 1024], BF16) for _ in range(2)]; B_X1B = [Buf() for _ in range(2)]
        X1T = view([128, 8, 128]); B_X1T = Buf()
        STATS = view([128, 2, 6]); B_STATS = Buf()
        MV = view([128, 4]); B_MV = Buf()
        RT = view([128, 264]); B_RT = Buf()
        CBF = view([128, 32], BF16); B_CBF = Buf()
        B_X1S = Buf(); B_XS = Buf()
        dma("pool", WO, wview(wo, 0, 1024), (), [B_WO])
        dma("sp", WRS, wr.rearrange("(kt p) n -> p kt n", p=128), (), [B_WRS])
        dma("sp", RPB, rp[:, R_BO:R_BO + 3072].partition_broadcast(128), (), [B_RPB])
        dma("sp", RBB, rp[:, R_RB:R_RB + 36].partition_broadcast(128), (), [B_RBB])
        LG = RT[:, 0:36]; GMAX = RT[:, 36:37]; NGMAX = RT[:, 37:38]; OHG = RT[:, 40:44]; EXG = RT[:, 44:48]
        SUMG = RT[:, 48:49]; PG = RT[:, 49:50]; ESEL = RT[:, 52:60]; M1 = RT[:, 60:61]; OH1 = RT[:, 64:72]
        E2 = RT[:, 72:80]; M2 = RT[:, 80:81]; OH2 = RT[:, 84:92]; DD = RT[:, 92:93]; W1c = RT[:, 93:94]
        OH1F = RT[:, 96:128]; OH2F = RT[:, 128:160]; CSUM = RT[:, 160:192]; RK = RT[:, 192:224]; TMP32 = RT[:, 224:256]
        PF = RT[:, 256:258]

        for j in range(NTOK // 512):
            mgi = MGI[j % 2]; bmgi = B_MGI[j % 2]
            dma("sp", mgi, MG[:, j * 512:(j + 1) * 512].rearrange("(dt p) n -> p dt n", p=128), [B_MG], [bmgi])
            for cc in range(4):
                ch = j * 4 + cc
                t0 = ch * 128
                xtm = XTM[ch % 2]; bxtm = B_XTM[ch % 2]
                z = Z[ch % 2]; bz = B_Z[ch % 2]
                x1b = X1B[ch % 2]; bx1b = B_X1B[ch % 2]
                dma("sp", xtm, xn[t0:t0 + 128, :], (), [bxtm])
                ts("pool", xtm, xtm, ALPHA, None, ALU.mult, None, [bxtm], [bxtm])
                tt("pool", xtm, xtm, RPB[:, 0:1024], ALU.add, [bxtm, B_RPB], [bxtm])
                for half in range(2):
                    ps, bps = next_ps()
                    for jt in range(8):
                        mm(ps[:, :], mgi[:, jt, cc * 128:(cc + 1) * 128], WO[:, jt, half * 512:(half + 1) * 512],
                           jt == 0, jt == 7, [bmgi, B_WO], [bps])
                    tt("dve", z[:, half * 512:(half + 1) * 512], ps[:, :], xtm[:, half * 512:(half + 1) * 512], ALU.add,
                       [bps, bxtm], [bz])
                    P.op("dve", lambda e, o=STATS[:, half, :], i=z[:, half * 512:(half + 1) * 512]: e.bn_stats(out=o, in_=i),
                         [bz], [B_STATS])
                P.op("dve", lambda e, o=MV[:, 0:2], i=STATS.rearrange("p a b -> p (a b)"): e.bn_aggr(out=o, in_=i),
                     [B_STATS], [B_MV])
                act(MV[:, 2:3], MV[:, 1:2], AF.Sqrt, [B_MV], [B_MV], bias=1e-5)
                P.op("dve", lambda e: e.reciprocal(out=MV[:, 2:3], in_=MV[:, 2:3]), [B_MV], [B_MV])
                stt("dve", MV[:, 3:4], MV[:, 0:1], -1.0, MV[:, 2:3], ALU.mult, ALU.mult, [B_MV], [B_MV])
                act(z, z, AF.Identity, [bz, B_MV], [bz], scale=MV[:, 2:3], bias=MV[:, 3:4])
                tt("pool", z, z, RPB[:, 1024:2048], ALU.mult, [bz, B_RPB], [bz])
                tt("pool", z, z, RPB[:, 2048:3072], ALU.add, [bz, B_RPB], [bz])
                dma("sp", X1S[t0:t0 + 128, :], z, [bz], [B_X1S])
                act(x1b, z, AF.Copy, [bz], [bx1b])
                for half in range(2):
                    ps, bps = next_ps()
                    for q4 in range(4):
                        dtl = half * 4 + q4
                        tp(ps[:, q4 * 128:(q4 + 1) * 128], z[:, dtl * 128:(dtl + 1) * 128], IDF, [bz, B_CONST], [bps])
                    act(X1T[:, half * 4:(half + 1) * 4, :], ps[:, :].rearrange("p (a b) -> p a b", b=128), AF.Copy,
                        [bps], [B_X1T])
                ps, bps = next_ps()
                for dtl in range(8):
                    mm(ps[:, 0:36], X1T[:, dtl, :], WRS[:, dtl, :], dtl == 0, dtl == 7, [B_X1T, B_WRS], [bps])
                R_ = [B_RT]
                tt("dve", LG, ps[:, 0:36], RBB, ALU.add, [bps, B_RBB], R_)
                P.op("dve", lambda e: e.tensor_reduce(out=GMAX, in_=LG[:, 0:4], axis=AX.X, op=ALU.max), R_, R_)
                ts("dve", NGMAX, GMAX, -1.0, None, ALU.mult, None, R_, R_)
                ts("dve", OHG, LG[:, 0:4], GMAX, None, ALU.is_equal, None, R_, R_)
                act(EXG, LG[:, 0:4], AF.Exp, R_, R_, bias=NGMAX, accum=SUMG)
                P.op("dve", lambda e: e.reciprocal(out=PG, in_=SUMG), R_, R_)
                ts("dve", ESEL, LG[:, 4:12], OHG[:, 0:1], None, ALU.mult, None, R_, R_)
                for g in range(1, 4):
                    stt("dve", ESEL, LG[:, 4 + g * 8:12 + g * 8], OHG[:, g:g + 1], ESEL, ALU.mult, ALU.add, R_, R_)
                P.op("dve", lambda e: e.tensor_reduce(out=M1, in_=ESEL, axis=AX.X, op=ALU.max), R_, R_)
                ts("dve", OH1, ESEL, M1, None, ALU.is_equal, None, R_, R_)
                stt("dve", E2, OH1, -1e30, ESEL, ALU.mult, ALU.add, R_, R_)
                P.op("dve", lambda e: e.tensor_reduce(out=M2, in_=E2, axis=AX.X, op=ALU.max), R_, R_)
                ts("dve", OH2, E2, M2, None, ALU.is_equal, None, R_, R_)
                tt("dve", DD, M2, M1, ALU.subtract, R_, R_)
                act(DD, DD, AF.Exp, R_, R_)
                ts("dve", DD, DD, 1.0, None, ALU.add, None, R_, R_)
                P.op("dve", lambda e: e.reciprocal(out=W1c, in_=DD), R_, R_)
                tt("dve", WTS[:, ch, 0:1], W1c, PG, ALU.mult, R_, [B_WTS])
                tt("dve", WTS[:, ch, 1:2], PG, WTS[:, ch, 0:1], ALU.subtract, R_ + [B_WTS], [B_WTS])
                for g in range(4):
                    ts("dve", OH1F[:, g * 8:(g + 1) * 8], OH1, OHG[:, g:g + 1], None, ALU.mult, None, R_, R_)
                    ts("dve", OH2F[:, g * 8:(g + 1) * 8], OH2, OHG[:, g:g + 1], None, ALU.mult, None, R_, R_)
                tt("dve", CBF, OH1F, OH2F, ALU.add, R_, [B_CBF])
                ps, bps = next_ps()
                mm(ps[:, 0:32], LTB, CBF, True, True, [B_CONST, B_CBF], [bps])
                mm(ps[:, 32:64], ALL1, CBF, True, True, [B_CONST, B_CBF], [bps])
                tt("dve", RK, ps[:, 0:32], CNTB, ALU.add, [bps, B_CNT], R_)
                tt("dve", CNTB, CNTB, ps[:, 32:64], ALU.add, [bps, B_CNT], [B_CNT])
                tt("dve", TMP32, OH1F, RK, ALU.mult, R_, R_)
                P.op("dve", lambda e, o=PF[:, 0:1]: e.tensor_reduce(out=o, in_=TMP32, axis=AX.X, op=ALU.add), R_, R_)
                tt("dve", TMP32, OH2F, RK, ALU.mult, R_, R_)
                P.op("dve", lambda e, o=PF[:, 1:2]: e.tensor_reduce(out=o, in_=TMP32, axis=AX.X, op=ALU.add), R_, R_)
                cp("dve", POS[:, ch, :], PF, R_, [B_POS])
                for k in range(2):
                    P.op("pool", lambda e, ix=POS[:, ch, k:k + 1], src=x1b: e.indirect_dma_start(
                        out=XS, out_offset=bass.IndirectOffsetOnAxis(ap=ix, axis=0), in_=src, in_offset=None),
                        [B_POS, bx1b], [B_XS], dma=True)
        if debug:
            dma("sp", d_pos, POS.rearrange("p a b -> p (a b)"), [B_POS], [Buf()])
            dma("sp", d_wts, WTS.rearrange("p a b -> p (a b)"), [B_WTS], [Buf()])
        P.barrier()

        off[0] = g_mark
        NST = CAP // 128
        EW1 = [view([128, 8, 512], BF16) for _ in range(2)]
        EW3 = [view([128, 8, 512], BF16) for _ in range(2)]
        EW2 = [view([128, 4, 1024], BF16) for _ in range(2)]
        B_EW = [[Buf(), Buf(), Buf()] for _ in range(2)]
        XSL = [view([128, NST, 1024], BF16) for _ in range(2)]; B_XSL = [Buf() for _ in range(2)]
        XST = [view([128, 8, CAP], BF16) for _ in range(2)]; B_XST = [Buf() for _ in range(2)]
        HT = [view([128, 4, CAP], BF16) for _ in range(2)]; B_HT = [[Buf() for _ in range(4)] for _ in range(2)]
        S1 = [view([128, CAP]) for _ in range(2)]; B_S1 = [Buf() for _ in range(2)]
        YSB = [view([128, 1024]) for _ in range(2)]; B_YSB = [Buf() for _ in range(2)]
        B_YS = Buf()
        n_s1 = 0
        n_y = 0
        for e in range(NEXP):
            p = e % 2
            dma("pool", EW1[p], ew1[e].rearrange("(kt p) n -> p kt n", p=128), (), [B_EW[p][0]])
            dma("pool", EW3[p], ew3[e].rearrange("(kt p) n -> p kt n", p=128), (), [B_EW[p][1]])
            dma("pool", EW2[p], ew2[e].rearrange("(kt p) n -> p kt n", p=128), (), [B_EW[p][2]])
            dma("sp", XSL[p], XS[e * CAP:(e + 1) * CAP, :].rearrange("(s p) n -> p s n", p=128), [B_XS], [B_XSL[p]])
            for s_ in range(NST):
                pst, bpst = next_ps()
                pstb = pst[:, :].bitcast(BF16)
                for dtl in range(8):
                    tp(pstb[:, dtl * 128:(dtl + 1) * 128], XSL[p][:, s_, dtl * 128:(dtl + 1) * 128], IDB,
                       [B_XSL[p], B_CONST], [bpst])
                act(XST[p][:, :, s_ * 128:(s_ + 1) * 128], pstb.rearrange("p (a b) -> p a b", b=128), AF.Copy,
                    [bpst], [B_XST[p]])
            for ft in range(4):
                ps1, bps1 = next_ps()
                for kt in range(8):
                    mm(ps1[:, 0:CAP], EW1[p][:, kt, ft * 128:(ft + 1) * 128], XST[p][:, kt, :], kt == 0, kt == 7,
                       [B_EW[p][0], B_XST[p]], [bps1])
                ps3, bps3 = next_ps()
                for kt in range(8):
                    mm(ps3[:, 0:CAP], EW3[p][:, kt, ft * 128:(ft + 1) * 128], XST[p][:, kt, :], kt == 0, kt == 7,
                       [B_EW[p][1], B_XST[p]], [bps3])
                s1 = S1[n_s1 % 2]; bs1 = B_S1[n_s1 % 2]
                n_s1 += 1
                act(s1, ps1[:, 0:CAP], AF.Silu, [bps1], [bs1])
                tt("dve", HT[p][:, ft, :], ps3[:, 0:CAP], s1, ALU.mult, [bps3, bs1], [B_HT[p][ft]])
            for s_ in range(NST):
                ysb = YSB[n_y % 2]; bysb = B_YSB[n_y % 2]
                n_y += 1
                for half in range(2):
                    ps, bps = next_ps()
                    for ft in range(4):
                        mm(ps[:, :], HT[p][:, ft, s_ * 128:(s_ + 1) * 128], EW2[p][:, ft, half * 512:(half + 1) * 512],
                           ft == 0, ft == 3, [B_HT[p][ft], B_EW[p][2]], [bps])
                    if half == 0:
                        act(ysb[:, 0:512], ps[:, :], AF.Copy, [bps], [bysb])
                    else:
                        cp("dve", ysb[:, 512:1024], ps[:, :], [bps], [bysb])
                r0 = e * CAP + s_ * 128
                dma("sp", YS[r0:r0 + 128, :], ysb, [bysb], [B_YS])
        P.barrier()

        off[0] = g_mark
        L2 = view([128, 2048]); B_L2 = Buf()
        Y1 = [view([128, 1024]) for _ in range(2)]; B_Y1 = [Buf() for _ in range(2)]
        Y2 = [view([128, 1024]) for _ in range(2)]; B_Y2 = [Buf() for _ in range(2)]
        XA = [view([128, 1024]) for _ in range(2)]; B_XA = [Buf() for _ in range(2)]
        STATS5 = view([128, 2, 6]); B_ST5 = Buf()
        MV5 = view([128, 4]); B_MV5 = Buf()
        dma("sp", L2, rp[:, R_L2G:R_L2G + 2048].partition_broadcast(128), (), [B_L2])
        for ch in range(NTOK // 128):
            t0 = ch * 128
            y1 = Y1[ch % 2]; by1 = B_Y1[ch % 2]
            y2 = Y2[ch % 2]; by2 = B_Y2[ch % 2]
            xa = XA[ch % 2]; bxa = B_XA[ch % 2]
            dma("sp", xa, X1S[t0:t0 + 128, :], [B_X1S], [bxa])
            P.op("pool", lambda e, ix=POS[:, ch, 0:1], o=y1: e.indirect_dma_start(
                out=o, out_offset=None, in_=YS, in_offset=bass.IndirectOffsetOnAxis(ap=ix, axis=0)),
                [B_POS, B_YS], [by1], dma=True)
            P.op("pool", lambda e, ix=POS[:, ch, 1:2], o=y2: e.indirect_dma_start(
                out=o, out_offset=None, in_=YS, in_offset=bass.IndirectOffsetOnAxis(ap=ix, axis=0)),
                [B_POS, B_YS], [by2], dma=True)
            act(xa, xa, AF.Identity, [bxa], [bxa], scale=ALPHA)
            stt("dve", xa, y1, WTS[:, ch, 0:1], xa, ALU.mult, ALU.add, [by1, bxa, B_WTS], [bxa])
            stt("dve", xa, y2, WTS[:, ch, 1:2], xa, ALU.mult, ALU.add, [by2, bxa, B_WTS], [bxa])
            for half in range(2):
                P.op("dve", lambda e, o=STATS5[:, half, :], i=xa[:, half * 512:(half + 1) * 512]: e.bn_stats(out=o, in_=i),
                     [bxa], [B_ST5])
            P.op("dve", lambda e, o=MV5[:, 0:2], i=STATS5.rearrange("p a b -> p (a b)"): e.bn_aggr(out=o, in_=i),
                 [B_ST5], [B_MV5])
            act(MV5[:, 2:3], MV5[:, 1:2], AF.Sqrt, [B_MV5], [B_MV5], bias=1e-5)
            P.op("dve", lambda e: e.reciprocal(out=MV5[:, 2:3], in_=MV5[:, 2:3]), [B_MV5], [B_MV5])
            stt("dve", MV5[:, 3:4], MV5[:, 0:1], -1.0, MV5[:, 2:3], ALU.mult, ALU.mult, [B_MV5], [B_MV5])
            act(xa, xa, AF.Identity, [bxa, B_MV5], [bxa], scale=MV5[:, 2:3], bias=MV5[:, 3:4])
            tt("pool", xa, xa, L2[:, 0:1024], ALU.mult, [bxa, B_L2], [bxa])
            tt("pool", xa, xa, L2[:, 1024:2048], ALU.add, [bxa, B_L2], [bxa])
            dma("sp", out[t0:t0 + 128, :], xa, [bxa], [Buf()])
        P.barrier()

        with nc.Block() as block:
            P.emit(block)
    return nc


def _host_inputs(inputs):
    f = lambda k: np.ascontiguousarray(np.asarray(inputs[k], dtype=np.float32)[0])
    x = np.asarray(inputs["x"], dtype=np.float32)
    b_in = f("b_in")
    pp = np.zeros((128, 128), np.float32)

    def put(col, vec):
        n = vec.shape[0] // 128
        pp[:, col:col + n] = vec.reshape(n, 128).T

    put(P_BRX, b_in[C_RX:C_RX + 1024]); put(P_BRY, b_in[C_RY:C_RY + 1024]); put(P_BQ, b_in[C_Q:C_Q + 512])
    put(P_BK, b_in[C_K:C_K + 512]); put(P_BG, b_in[C_G:C_G + 1024])
    pp[0:16, P_BALR] = b_in[C_ALR:C_ALR + 16]
    put(P_BGA, b_in[C_GA:C_GA + 1024]); put(P_BGB, b_in[C_GB:C_GB + 1024])
    cw = f("conv_w")
    pp[:, P_CW:P_CW + 32] = cw.reshape(4, 8, 128).transpose(2, 1, 0).reshape(128, 32)
    put(P_CB, f("conv_b")); put(P_RBA, f("rg_b_a")); put(P_RBX, f("rg_b_x")); put(P_LAM, f("rg_lambda"))
    put(P_GBA, f("gla_b_a")); put(P_NG, f("gla_norm_g"))
    rp = np.zeros((1, NRP), np.float32)
    rp[0, R_BV:R_BV + 1024] = b_in[C_V:C_V + 1024]
    rp[0, R_BO:R_BO + 1024] = f("b_o"); rp[0, R_L1G:R_L1G + 1024] = f("ln1_g"); rp[0, R_L1B:R_L1B + 1024] = f("ln1_b")
    rp[0, R_L2G:R_L2G + 1024] = f("ln2_g"); rp[0, R_L2B:R_L2B + 1024] = f("ln2_b")
    rp[0, R_RB:R_RB + 4] = f("router_b_group"); rp[0, R_RB + 4:R_RB + 36] = f("router_b_expert")
    wr = np.ascontiguousarray(np.concatenate([f("router_w_group"), f("router_w_expert")], axis=1))
    rgw = np.ascontiguousarray(np.stack([f("rg_w_a"), f("rg_w_x")], axis=0))
    ii = np.arange(128)
    c_id = np.eye(128, dtype=np.float32)
    c_lt = (ii[:, None] < ii[None, :]).astype(np.float32)
    c_cm = np.tile((ii[None, :] >= ii[:, None]).astype(np.float32), (1, 4))
    c_mc = np.ones((128, 512), np.float32); c_mc[:, ::128] = 0.0
    c_eb = np.tile((np.arange(NEXP, dtype=np.float32) * CAP)[None, :], (128, 1))
    shared = {
        "w_in": f("w_in"), "pp": pp, "rp": rp, "wr": wr, "rgw": rgw, "wa2": f("gla_w_a2"),
        "wrnn": f("w_proj_rnn"), "wgla": f("w_proj_gla"), "wo": f("w_o"),
        "ew1": f("exp_w1"), "ew3": f("exp_w3"), "ew2": f("exp_w2"),
        "c_id": c_id, "c_lt": c_lt, "c_cm": c_cm, "c_mc": c_mc, "c_eb": c_eb,
    }
    in_maps = []
    for c in range(NCORES):
        xc = np.ascontiguousarray(x[2 * c:2 * c + 2].reshape(NTOK, 1024))
        m = dict(shared)
        m["xn"] = xc
        m["xT"] = np.ascontiguousarray(xc.T)
        in_maps.append(m)
    return in_maps


def kernel(**inputs):
    in_maps = _host_inputs(inputs)
    nc = build(debug=False)
    res = run_bass_kernel_spmd(nc, in_maps, core_ids=list(range(NCORES)))
    outs = [np.asarray(r["out"], dtype=np.float32).reshape(2, SEQ, 1024) for r in res.results]
    return np.concatenate(outs, axis=0)
```

```python
import numpy as np
from contextlib import ExitStack
import concourse.bass as bass
import concourse.mybir as mybir
from concourse.bass_utils import run_bass_kernel_spmd

F32 = mybir.dt.float32
BF16 = mybir.dt.bfloat16
I32 = mybir.dt.int32
AF = mybir.ActivationFunctionType
ALU = mybir.AluOpType
AX = mybir.AxisListType

SAME_ENGINE_SYNC = True
DMA_RING = 12
NCORES = 8
NTOK = 4096
SEQ = 2048
CAP = 384
NEXP = 32
ALPHA = 2.0 ** 0.25
SBUF_WORDS = 44 * 1024

C_RX, C_RY, C_Q, C_K, C_V, C_G, C_ALR, C_GA, C_GB = 0, 1024, 2048, 2560, 3072, 4096, 5120, 5136, 6160
P_BRX, P_BRY, P_BQ, P_BK, P_BG, P_BALR, P_BGA, P_BGB = 0, 8, 16, 20, 24, 32, 33, 41
P_CW, P_CB, P_RBA, P_RBX, P_LAM, P_GBA, P_NG = 49, 81, 89, 97, 105, 113, 117
R_BV, R_BO, R_L1G, R_L1B, R_L2G, R_L2B, R_RB = 0, 1024, 2048, 3072, 4096, 5120, 6144
NRP = 6180


class Buf:
    __slots__ = ("w", "r")

    def __init__(self):
        self.w = None
        self.r = {}


class _Eng:
    def __init__(self, name, sem, dma_sems):
        self.name = name
        self.sem = sem
        self.count = 0
        self.seen = {}
        self.ops = []
        self.dma_sems = dma_sems
        self.dma_n = 0


class Prog:
    def __init__(self, nc, stack):
        self.nc = nc
        self.E = {}
        for name in ("pe", "act", "dve", "pool", "sp"):
            sem = stack.enter_context(nc.semaphore("s_" + name))
            dsems = []
            if name in ("sp", "pool"):
                dsems = [stack.enter_context(nc.semaphore("d_%s%d" % (name, i))) for i in range(DMA_RING)]
            self.E[name] = _Eng(name, sem, dsems)

    def op(self, eng, fn, reads=(), writes=(), dma=False):
        E = self.E[eng]
        deps = {}

        def add(t):
            if t is None:
                return
            if deps.get(t[0], (None, 0))[1] < t[1]:
                deps[t[0]] = t

        for b in reads:
            add(b.w)
        for b in writes:
            add(b.w)
            for t in b.r.values():
                add(t)
        waits = []
        for s, v in deps.values():
            if E.seen.get(s, 0) >= v:
                continue
            if s is E.sem and (eng == "pe" or not SAME_ENGINE_SYNC):
                continue
            waits.append((s, v))
            E.seen[s] = v
        if dma:
            slot = E.dma_n % DMA_RING
            sem = E.dma_sems[slot]
            val = 16 * (E.dma_n // DMA_RING + 1)
            if val > 16 and E.seen.get(sem, 0) < val - 16:
                waits.append((sem, val - 16))
                E.seen[sem] = val - 16
            E.dma_n += 1
            tok = (sem, val)
            inc = 16
        else:
            E.count += 1
            tok = (E.sem, E.count)
            inc = 1
        E.ops.append((waits, fn, tok[0], inc))
        for b in reads:
            if b.r.get(tok[0], (None, 0))[1] < tok[1]:
                b.r[tok[0]] = tok
        for b in writes:
            b.w = tok
            b.r = {}
        return tok

    def barrier(self):
        toks = []
        for E in self.E.values():
            if E.count:
                toks.append((E.sem, E.count))
            for i, s in enumerate(E.dma_sems):
                n = (E.dma_n - 1 - i) // DMA_RING + 1 if E.dma_n > i else 0
                if n > 0:
                    toks.append((s, 16 * n))
        for E in self.E.values():
            waits = []
            for s, v in toks:
                if s is E.sem or E.seen.get(s, 0) >= v:
                    continue
                waits.append((s, v))
                E.seen[s] = v
            if waits:
                E.ops.append((waits, None, None, 0))

    def emit(self, block):
        def run(E):
            def body(eng):
                for waits, fn, sem, inc in E.ops:
                    for s, v in waits:
                        eng.wait_ge(s, v)
                    if fn is not None:
                        fn(eng).then_inc(sem, inc)
            return body

        block.tensor(run(self.E["pe"]))
        block.scalar(run(self.E["act"]))
        block.vector(run(self.E["dve"]))
        block.gpsimd(run(self.E["pool"]))
        block.sync(run(self.E["sp"]))


def build(debug=False):
    nc = bass.Bass("TRN2", target_bir_lowering=False)

    def din(name, shape, dt=F32):
        return nc.dram_tensor(name, list(shape), dt, kind="ExternalInput").ap()

    xT = din("xT", [1024, NTOK]); xn = din("xn", [NTOK, 1024]); w_in = din("w_in", [1024, 7184])
    pp = din("pp", [128, 128]); rp = din("rp", [1, NRP]); wr = din("wr", [1024, 36])
    rgw = din("rgw", [2, 8, 128, 128]); wa2 = din("wa2", [16, 512])
    wrnn = din("wrnn", [1024, 1024]); wgla = din("wgla", [1024, 1024]); wo = din("wo", [1024, 1024])
    ew1 = din("ew1", [NEXP, 1024, 512]); ew3 = din("ew3", [NEXP, 1024, 512]); ew2 = din("ew2", [NEXP, 512, 1024])
    c_id = din("c_id", [128, 128]); c_lt = din("c_lt", [128, 128]); c_cm = din("c_cm", [128, 512])
    c_mc = din("c_mc", [128, 512]); c_eb = din("c_eb", [128, 32])
    out = nc.dram_tensor("out", [NTOK, 1024], F32, kind="ExternalOutput").ap()
    sk = "ExternalOutput" if debug else "Internal"
    AG = nc.dram_tensor("AG", [1024, NTOK], BF16, kind=sk).ap()
    MG = nc.dram_tensor("MG", [1024, NTOK], BF16, kind=sk).ap()
    X1S = nc.dram_tensor("X1S", [NTOK, 1024], F32, kind=sk).ap()
    XS = nc.dram_tensor("XS", [NEXP * CAP, 1024], BF16, kind=sk).ap()
    YS = nc.dram_tensor("YS", [NEXP * CAP, 1024], F32, kind=sk).ap()
    if debug:
        d_pos = nc.dram_tensor("d_pos", [128, 64], I32, kind="ExternalOutput").ap()
        d_wts = nc.dram_tensor("d_wts", [128, 64], F32, kind="ExternalOutput").ap()

    st = ExitStack()
    with st:
        big = st.enter_context(nc.sbuf_tensor("big", [128, SBUF_WORDS], F32))
        PSB = [st.enter_context(nc.psum_tensor("ps%d" % i, [128, 512], F32)) for i in range(8)]
        PSBUF = [Buf() for _ in range(8)]
        P = Prog(nc, st)
        off = [0]
        psn = [0]

        def view(shape, dt=F32):
            n = 1
            for s in shape[1:]:
                n *= s
            nw = (n * (2 if dt == BF16 else 4) + 3) // 4
            nw = (nw + 7) // 8 * 8
            assert off[0] + nw <= SBUF_WORDS, ("SBUF overflow", off[0], nw)
            v = big[:, off[0]:off[0] + nw]
            off[0] += nw
            if dt != F32:
                v = v.bitcast(dt)
            if dt == BF16 and n % 2:
                v = v[:, 0:n]
            elif dt == BF16:
                v = v[:, 0:n]
            else:
                v = v[:, 0:n]
            if len(shape) > 2:
                names = " ".join("a%d" % i for i in range(len(shape) - 1))
                kw = {"a%d" % i: shape[i + 1] for i in range(len(shape) - 1)}
                v = v.rearrange("p (%s) -> p %s" % (names, names), **kw)
            if shape[0] != 128:
                v = v[0:shape[0]]
            return v

        def next_ps():
            i = psn[0] % 8
            psn[0] += 1
            return PSB[i], PSBUF[i]

        def mm(o, lhsT, rhs, start, stop, reads, writes):
            P.op("pe", lambda e: e.matmul(o, lhsT=lhsT, rhs=rhs, start=start, stop=stop), reads, writes)

        def tp(o, in_, ident, reads, writes):
            P.op("pe", lambda e: e.transpose(o, in_, ident), reads, writes)

        def act(o, in_, func, reads, writes, bias=None, scale=None, accum=None):
            kw = {}
            if bias is not None:
                kw["bias"] = bias
            if scale is not None:
                kw["scale"] = scale
            if accum is not None:
                kw["accum_out"] = accum
            P.op("act", lambda e: e.activation(out=o, in_=in_, func=func, **kw), reads, writes)

        def ts(eng, o, in0, s1, s2, op0, op1, reads, writes):
            if op1 is None:
                P.op(eng, lambda e: e.tensor_scalar(out=o, in0=in0, scalar1=s1, scalar2=None, op0=op0), reads, writes)
            else:
                P.op(eng, lambda e: e.tensor_scalar(out=o, in0=in0, scalar1=s1, scalar2=s2, op0=op0, op1=op1), reads, writes)

        def tt(eng, o, in0, in1, op, reads, writes):
            P.op(eng, lambda e: e.tensor_tensor(out=o, in0=in0, in1=in1, op=op), reads, writes)

        def stt(eng, o, in0, sc, in1, op0, op1, reads, writes):
            P.op(eng, lambda e: e.scalar_tensor_tensor(out=o, in0=in0, scalar=sc, in1=in1, op0=op0, op1=op1), reads, writes)

        def cp(eng, o, in_, reads, writes):
            P.op(eng, lambda e: e.tensor_copy(out=o, in_=in_), reads, writes)

        def ms(eng, o, val, writes):
            P.op(eng, lambda e: e.memset(o, val), (), writes)

        def dma(eng, o, in_, reads, writes):
            P.op(eng, lambda e: e.dma_start(out=o, in_=in_), reads, writes, dma=True)

        def wview(w, c0, c1):
            return w[:, c0:c1].rearrange("(kt p) n -> p kt n", p=128)

        PP = view([128, 128]); B_PP = Buf()
        DER = view([128, 32]); B_DER = Buf()
        TMP8 = view([128, 8]); B_TMP8 = Buf()
        IDB = view([128, 128], BF16); IDF = view([128, 128]); LTB = view([128, 128], BF16)
        ALL1 = view([128, 128], BF16); ONES256 = view([128, 128], BF16); ONE1 = view([128, 128], BF16)
        CMB = view([128, 512], BF16); MCF = view([128, 512]); CNTB = view([128, 32])
        POS = view([128, 32, 2], I32); WTS = view([128, 32, 2])
        B_CONST = Buf(); B_CNT = Buf(); B_POS = Buf(); B_WTS = Buf()
        dma("sp", PP, pp, (), [B_PP])
        dma("sp", IDF, c_id, (), [B_CONST])
        dma("sp", MCF, c_mc, (), [B_CONST])
        dma("sp", CNTB, c_eb, (), [B_CNT])
        dma("pool", IDB, c_id, (), [B_CONST])
        dma("pool", LTB, c_lt, (), [B_CONST])
        dma("pool", CMB, c_cm, (), [B_CONST])
        ms("dve", ALL1, 1.0, [B_CONST])
        ms("dve", ONES256, 1.0 / 256.0, [B_CONST])
        ms("dve", ONE1, 1.0, [B_CONST])
        act(TMP8, PP[:, P_LAM:P_LAM + 8], AF.Exp, [B_PP], [B_TMP8], scale=-1.0)
        act(TMP8, TMP8, AF.Ln, [B_TMP8], [B_TMP8], bias=1.0)
        ts("dve", DER[:, 0:8], TMP8, -8.0, None, ALU.mult, None, [B_TMP8], [B_DER])
        ts("dve", DER[:, 8:16], TMP8, -16.0, None, ALU.mult, None, [B_TMP8], [B_DER])
        ts("dve", DER[:, 16:20], PP[:, P_GBA:P_GBA + 4], -1.0, None, ALU.mult, None, [B_PP], [B_DER])
        g_mark = off[0]
        PR = [B_PP, B_DER]

        def pcol(c):
            return PP[:, c:c + 1]

        W1 = view([128, 8, 3072], BF16); B_W1 = [Buf() for _ in range(3)]
        WRNN = view([128, 8, 1024], BF16); B_WRNN = Buf()
        RGW = view([128, 2, 8, 128], BF16); B_RGW = Buf()
        XT = [view([128, 8, 512], BF16) for _ in range(2)]; B_XT = [Buf() for _ in range(2)]
        NS = 2
        RX = [view([128, 2, 516]) for _ in range(NS)]; B_RX = [[Buf(), Buf()] for _ in range(NS)]
        U = [view([128, 2, 512]) for _ in range(NS)]; B_U = [[Buf(), Buf()] for _ in range(NS)]
        UBF = [view([128, 2, 512], BF16) for _ in range(NS)]; B_UBF = [[Buf(), Buf()] for _ in range(NS)]
        THR = [view([128, 2, 512]) for _ in range(NS)]; B_THR = [[Buf(), Buf()] for _ in range(NS)]
        A2 = [view([128, 2, 512]) for _ in range(NS)]; B_A2 = [[Buf(), Buf()] for _ in range(NS)]
        THI = [view([128, 2, 512]) for _ in range(NS)]; B_THI = [[Buf(), Buf()] for _ in range(NS)]
        GY = [view([128, 2, 512], BF16) for _ in range(NS)]; B_GY = [[Buf(), Buf()] for _ in range(NS)]
        HG = [view([128, 8, 512], BF16) for _ in range(2)]; B_HG = [[Buf() for _ in range(8)] for _ in range(2)]
        HALO = view([128, 8, 4]); B_HALO = [Buf() for _ in range(8)]
        HST = view([128, 8]); B_HST = [Buf() for _ in range(8)]
        SGA = [view([128, 512]) for _ in range(2)]; B_SGA = [Buf() for _ in range(2)]
        AGO = [view([128, 512], BF16) for _ in range(2)]; B_AGO = [Buf() for _ in range(2)]
        B_AG = Buf()
        DER2 = view([128, 32]); B_DER2 = Buf()
        ts("dve", DER2[:, 0:8], DER[:, 0:8], 0.5, None, ALU.mult, None, [B_DER], [B_DER2])
        ts("dve", DER2[:, 8:16], PP[:, P_RBA:P_RBA + 8], 0.5, None, ALU.mult, None, [B_PP], [B_DER2])
        ts("dve", DER2[:, 16:24], PP[:, P_RBX:P_RBX + 8], 0.5, None, ALU.mult, None, [B_PP], [B_DER2])
        ts("dve", DER2[:, 24:32], PP[:, P_BGA:P_BGA + 8], 0.5, None, ALU.mult, None, [B_PP], [B_DER2])
        PR1 = PR + [B_DER2]

        for i, c0 in enumerate((C_RX, C_RY, C_GA)):
            dma("pool", W1[:, :, i * 1024:(i + 1) * 1024], wview(w_in, c0, c0 + 1024), (), [B_W1[i]])
        dma("pool", RGW, rgw.rearrange("g h i j -> i g h j"), (), [B_RGW])
        dma("pool", WRNN, wview(wrnn, 0, 1024), (), [B_WRNN])

        def p1_A(G):
            j, g = G // 4, G % 4
            t0 = j * 512
            first = (j % 4 == 0)
            xt = XT[j % 2]; bxt = B_XT[j % 2]
            sset = G % NS
            if g == 0:
                dma("pool", xt, xT[:, t0:t0 + 512].rearrange("(kt p) n -> p kt n", p=128), (), [bxt])
            for ci in range(2):
                c = g * 2 + ci
                rx = RX[sset]; brx = B_RX[sset][ci]
                u = U[sset]; bu = B_U[sset][ci]
                ps, bps = next_ps()
                for kt in range(8):
                    mm(ps[:, :], W1[:, kt, c * 128:(c + 1) * 128], xt[:, kt, :], kt == 0, kt == 7, [B_W1[0], bxt], [bps])
                if first:
                    ms("pool", rx[:, ci, 0:3], 0.0, [brx])
                else:
                    cp("pool", rx[:, ci, 0:3], HALO[:, c, 0:3], [B_HALO[c]], [brx])
                act(rx[:, ci, 3:515], ps[:, :], AF.Identity, [bps] + PR1, [brx], bias=pcol(P_BRX + c))
                cp("pool", HALO[:, c, 0:3], rx[:, ci, 512:515], [brx], [B_HALO[c]])
                act(u[:, ci, :], rx[:, ci, 3:515], AF.Identity, [brx] + PR1, [bu], scale=pcol(P_CW + c * 4 + 3),
                    bias=pcol(P_CB + c))
                for k in (0, 1, 2):
                    stt("dve", u[:, ci, :], rx[:, ci, k:k + 512], pcol(P_CW + c * 4 + k), u[:, ci, :],
                        ALU.mult, ALU.add, [brx, bu] + PR1, [bu])
                act(UBF[sset][:, ci, :], u[:, ci, :], AF.Copy, [bu], [B_UBF[sset][ci]])
            for ci in range(2):
                c = g * 2 + ci
                ps, bps = next_ps()
                for kt in range(8):
                    mm(ps[:, :], W1[:, kt, 1024 + c * 128:1024 + (c + 1) * 128], xt[:, kt, :], kt == 0, kt == 7,
                       [B_W1[1], bxt], [bps])
                act(GY[sset][:, ci, :], ps[:, :], AF.Gelu_apprx_tanh, [bps] + PR1, [B_GY[sset][ci]], bias=pcol(P_BRY + c))

        def p1_B(G):
            j, g = G // 4, G % 4
            first = (j % 4 == 0)
            sset = G % NS
            u = U[sset]; thr = THR[sset]; thi = THI[sset]; a2 = A2[sset]; ubf = UBF[sset]; gy = GY[sset]
            for ci in range(2):
                c = g * 2 + ci
                ps, bps = next_ps()
                mm(ps[:, :], RGW[:, 0, c, :], ubf[:, ci, :], True, True, [B_RGW, B_UBF[sset][ci]], [bps])
                act(thr[:, ci, :], ps[:, :], AF.Tanh, [bps] + PR1, [B_THR[sset][ci]], bias=DER2[:, 8 + c:9 + c], scale=0.5)
                ps, bps = next_ps()
                mm(ps[:, :], RGW[:, 1, c, :], ubf[:, ci, :], True, True, [B_RGW, B_UBF[sset][ci]], [bps])
                act(thi[:, ci, :], ps[:, :], AF.Tanh, [bps] + PR1, [B_THI[sset][ci]], bias=DER2[:, 16 + c:17 + c], scale=0.5)
            for ci in range(2):
                c = g * 2 + ci
                act(a2[:, ci, :], thr[:, ci, :], AF.Exp, [B_THR[sset][ci]] + PR1, [B_A2[sset][ci]],
                    scale=DER[:, c:c + 1], bias=DER[:, c:c + 1])
                act(thr[:, ci, :], thr[:, ci, :], AF.Exp, [B_THR[sset][ci]] + PR1, [B_THR[sset][ci]],
                    scale=DER2[:, c:c + 1], bias=DER2[:, c:c + 1])
            for ci in range(2):
                ts("dve", a2[:, ci, :], a2[:, ci, :], 0.99999994, -1.0, ALU.min, ALU.mult, [B_A2[sset][ci]], [B_A2[sset][ci]])
                stt("dve", thi[:, ci, :], thi[:, ci, :], 1.0, u[:, ci, :], ALU.add, ALU.mult,
                    [B_THI[sset][ci], B_U[sset][ci]], [B_THI[sset][ci]])
            for ci in range(2):
                act(a2[:, ci, :], a2[:, ci, :], AF.Sqrt, [B_A2[sset][ci]], [B_A2[sset][ci]], bias=0.25, scale=0.25)
            for ci in range(2):
                c = g * 2 + ci
                tt("dve", thi[:, ci, :], thi[:, ci, :], a2[:, ci, :], ALU.mult, [B_A2[sset][ci], B_THI[sset][ci]],
                   [B_THI[sset][ci]])
                init = 0.0 if first else HST[:, c:c + 1]
                P.op("dve", lambda e, o=u[:, ci, :], d0=thr[:, ci, :], d1=thi[:, ci, :], ini=init:
                     e.tensor_tensor_scan(out=o, data0=d0, data1=d1, initial=ini, op0=ALU.mult, op1=ALU.add),
                     [B_THR[sset][ci], B_THI[sset][ci], B_HST[c], B_U[sset][ci]], [B_U[sset][ci]])
                cp("dve", HST[:, c:c + 1], u[:, ci, 511:512], [B_U[sset][ci]], [B_HST[c]])
                stt("dve", HG[j % 2][:, c, :], u[:, ci, :], 0.5, gy[:, ci, :], ALU.mult, ALU.mult,
                    [B_U[sset][ci], B_GY[sset][ci]], [B_HG[j % 2][c]])

        n_ag = [0]

        def p1_C(j):
            t0 = j * 512
            xt = XT[j % 2]; bxt = B_XT[j % 2]
            for dt in range(8):
                ps, bps = next_ps()
                for kt in range(8):
                    mm(ps[:, :], W1[:, kt, 2048 + dt * 128:2048 + (dt + 1) * 128], xt[:, kt, :], kt == 0, kt == 7,
                       [B_W1[2], bxt], [bps])
                sga = SGA[n_ag[0] % 2]; bsga = B_SGA[n_ag[0] % 2]
                ago = AGO[n_ag[0] % 2]; bago = B_AGO[n_ag[0] % 2]
                n_ag[0] += 1
                act(sga, ps[:, :], AF.Tanh, [bps] + PR1, [bsga], bias=DER2[:, 24 + dt:25 + dt], scale=0.5)
                ps2, bps2 = next_ps()
                for c in range(8):
                    mm(ps2[:, :], WRNN[:, c, dt * 128:(dt + 1) * 128], HG[j % 2][:, c, :], c == 0, c == 7,
                       [B_WRNN, B_HG[j % 2][c]], [bps2])
                stt("dve", ago, sga, 1.0, ps2[:, :], ALU.add, ALU.mult, [bps2, bsga], [bago])
                dma("sp", AG[dt * 128:(dt + 1) * 128, t0:t0 + 512], ago, [bago], [B_AG])

        NG = (NTOK // 512) * 4
        p1_A(0)
        for G in range(NG):
            if G + 1 < NG:
                p1_A(G + 1)
            p1_B(G)
            if G % 4 == 0 and G >= 4:
                p1_C(G // 4 - 1)
        p1_C(NTOK // 512 - 1)
        P.barrier()

        off[0] = g_mark
        TB = 256
        NCH = TB // 128
        NW2 = 4112
        W2 = view([128, 8, NW2], BF16); B_W2 = Buf()
        o_q, o_k, o_v, o_g, o_alr, o_gb = 0, 512, 1024, 2048, 3072, 3088
        WGLA = view([128, 8, 1024], BF16); B_WGLA = Buf()
        WA2 = view([16, 512], BF16); B_WA2 = Buf()
        BVB = view([1, 1024], BF16); B_BVB = Buf()
        XT2 = [view([128, 8, TB], BF16) for _ in range(2)]; B_XT2 = [Buf() for _ in range(2)]
        ALRB = view([16, TB], BF16); B_ALRB = Buf()
        ECS = view([128, 4, TB]); B_ECS = [Buf() for _ in range(4)]
        CS = view([128, 4, TB]); B_CS = [Buf() for _ in range(4)]
        EB = [view([128, TB]) for _ in range(2)]; B_EB = [Buf() for _ in range(2)]
        EINV = [view([128, TB]) for _ in range(2)]; B_EINV = [Buf() for _ in range(2)]
        EBL = view([128, 4, NCH]); B_EBL = [Buf() for _ in range(4)]
        QD = view([128, 4, TB], BF16); B_QD = [Buf() for _ in range(4)]
        KI = view([128, 4, TB], BF16); B_KI = [Buf() for _ in range(4)]
        KT = view([128, NCH, 512], BF16); B_KT = [Buf() for _ in range(NCH)]
        VT = view([128, NCH, 1024], BF16); B_VT = [Buf() for _ in range(NCH)]
        SC = view([128, NCH, 512], BF16); B_SC = [Buf() for _ in range(NCH)]
        S = view([128, 4, 256]); B_S = [Buf() for _ in range(4)]
        SBF = view([128, 4, 256], BF16); B_SBF = [Buf() for _ in range(4)]
        T1 = view([128, 4, 256]); B_T1 = [Buf() for _ in range(4)]
        SQ = view([128, 2, 512], BF16); B_SQ = [Buf() for _ in range(2)]
        RSTD = view([128, 512]); B_RSTD = Buf()
        ON = view([128, 8, TB]); B_ON = [Buf() for _ in range(8)]
        SG = view([128, 8, TB]); B_SG = [Buf() for _ in range(8)]
        OFIN = view([128, 8, TB], BF16); B_OFIN = [Buf() for _ in range(8)]
        AGI = view([128, 8, TB], BF16); B_AGI = Buf()
        SGB = [view([128, TB]) for _ in range(2)]; B_SGB = [Buf() for _ in range(2)]
        TBB = [view([128, TB]) for _ in range(2)]; B_TBB = [Buf() for _ in range(2)]
        B_MG = Buf()

        for (o0, c0, n) in ((o_q, C_Q, 1024), (o_v, C_V, 1024), (o_g, C_G, 1024), (o_gb, C_GB, 1024)):
            dma("pool", W2[:, :, o0:o0 + n], wview(w_in, c0, c0 + n), (), [B_W2])
        dma("pool", W2[:, :, o_alr:o_alr + 16], wview(w_in, C_ALR, C_ALR + 16), (), [B_W2])
        dma("pool", WGLA, wview(wgla, 0, 1024), (), [B_WGLA])
        dma("pool", WA2, wa2, (), [B_WA2])
        dma("pool", BVB, rp[:, R_BV:R_BV + 1024], (), [B_BVB])

        n_mg = 0
        for j in range(NTOK // TB):
            t0 = j * TB
            first = (t0 % SEQ == 0)
            xt = XT2[j % 2]; bxt = B_XT2[j % 2]
            dma("pool", xt, xT[:, t0:t0 + TB].rearrange("(kt p) n -> p kt n", p=128), (), [bxt])
            dma("sp", AGI, AG[:, t0:t0 + TB].rearrange("(dt p) n -> p dt n", p=128), [B_AG], [B_AGI])
            ps, bps = next_ps()
            for kt in range(8):
                mm(ps[0:16, 0:TB], W2[:, kt, o_alr:o_alr + 16], xt[:, kt, :], kt == 0, kt == 7, [B_W2, bxt], [bps])
            act(ALRB, ps[0:16, 0:TB], AF.Identity, [bps] + PR, [B_ALRB], bias=PP[0:16, P_BALR:P_BALR + 1])
            for hd in range(4):
                psz, bpsz = next_ps()
                mm(psz[:, 0:TB], WA2[:, hd * 128:(hd + 1) * 128], ALRB, True, True, [B_WA2, B_ALRB], [bpsz])
                act(ECS[:, hd, :], psz[:, 0:TB], AF.Exp, [bpsz] + PR, [B_ECS[hd]], bias=DER[:, 16 + hd:17 + hd], scale=-1.0)
            for hd in range(4):
                act(ECS[:, hd, :], ECS[:, hd, :], AF.Ln, [B_ECS[hd]], [B_ECS[hd]], bias=1.0)
            for hd in range(4):
                P.op("dve", lambda e, o=CS[:, hd, :], d0=MCF[:, 0:TB], d1=ECS[:, hd, :]:
                     e.tensor_tensor_scan(out=o, data0=d0, data1=d1, initial=0.0, op0=ALU.mult, op1=ALU.add),
                     [B_ECS[hd], B_CONST], [B_CS[hd]])
            for hd in range(4):
                psq, bpsq = next_ps()
                for kt in range(8):
                    mm(psq[:, 0:TB], W2[:, kt, o_q + hd * 128:o_q + (hd + 1) * 128], xt[:, kt, :], kt == 0, kt == 7,
                       [B_W2, bxt], [bpsq])
                psk, bpsk = next_ps()
                for kt in range(8):
                    mm(psk[:, 0:TB], W2[:, kt, o_k + hd * 128:o_k + (hd + 1) * 128], xt[:, kt, :], kt == 0, kt == 7,
                       [B_W2, bxt], [bpsk])
                eb = EB[hd % 2]; beb = B_EB[hd % 2]
                einv = EINV[hd % 2]; beinv = B_EINV[hd % 2]
                act(eb, CS[:, hd, :], AF.Exp, [B_CS[hd]], [beb], scale=-1.0 / 16.0, bias=float(-0.5 * np.log(128.0)))
                act(einv, CS[:, hd, :], AF.Exp, [B_CS[hd]], [beinv], scale=1.0 / 16.0)
                act(EBL[:, hd, :], CS[:, hd, :].rearrange("p (c t) -> p c t", t=128)[:, :, 127], AF.Exp, [B_CS[hd]],
                    [B_EBL[hd]], scale=-1.0 / 16.0)
                stt("dve", QD[:, hd, :], psq[:, 0:TB], pcol(P_BQ + hd), eb, ALU.add, ALU.mult,
                    [bpsq, beb] + PR, [B_QD[hd]])
                stt("dve", KI[:, hd, :], psk[:, 0:TB], pcol(P_BK + hd), einv, ALU.add, ALU.mult,
                    [bpsk, beinv] + PR, [B_KI[hd]])
            for c in range(NCH):
                pst, bpst = next_ps()
                pstb = pst[:, :].bitcast(BF16)
                for hd in range(4):
                    tp(pstb[:, hd * 128:(hd + 1) * 128], KI[:, hd, c * 128:(c + 1) * 128], IDB,
                       [B_KI[hd], B_CONST], [bpst])
                act(KT[:, c, :], pstb[:, 0:512], AF.Copy, [bpst], [B_KT[c]])
                for half in range(2):
                    psv, bpsv = next_ps()
                    for kt in range(8):
                        mm(psv[:, :], xt[:, kt, c * 128:(c + 1) * 128],
                           W2[:, kt, o_v + half * 512:o_v + (half + 1) * 512], kt == 0, False, [B_W2, bxt], [bpsv])
                    mm(psv[:, :], ONE1[0:1, :], BVB[0:1, half * 512:(half + 1) * 512], False, True,
                       [B_CONST, B_BVB], [bpsv])
                    act(VT[:, c, half * 512:(half + 1) * 512], psv[:, :], AF.Copy, [bpsv], [B_VT[c]])
                pss, bpss = next_ps()
                for hd in range(4):
                    mm(pss[:, hd * 128:(hd + 1) * 128], KI[:, hd, c * 128:(c + 1) * 128],
                       QD[:, hd, c * 128:(c + 1) * 128], True, True, [B_KI[hd], B_QD[hd]], [bpss])
                tt("dve", SC[:, c, :], pss[:, :], CMB, ALU.mult, [bpss, B_CONST], [B_SC[c]])
            if first:
                for hd in range(4):
                    ms("dve", S[:, hd, :], 0.0, [B_S[hd]])
                    ms("dve", SBF[:, hd, :], 0.0, [B_SBF[hd]])
            for c in range(NCH):
                pso = [next_ps(), next_ps()]
                for hd in range(4):
                    po, bpo = pso[hd // 2]
                    for vh in range(2):
                        col = ((hd % 2) * 2 + vh) * 128
                        mm(po[:, col:col + 128], VT[:, c, hd * 256 + vh * 128:hd * 256 + (vh + 1) * 128],
                           SC[:, c, hd * 128:(hd + 1) * 128], True, False, [B_VT[c], B_SC[c]], [bpo])
                        mm(po[:, col:col + 128], SBF[:, hd, vh * 128:(vh + 1) * 128],
                           QD[:, hd, c * 128:(c + 1) * 128], False, True, [B_SBF[hd], B_QD[hd]], [bpo])
                    pkv, bpkv = next_ps()
                    mm(pkv[:, 0:256], KT[:, c, hd * 128:(hd + 1) * 128], VT[:, c, hd * 256:(hd + 1) * 256],
                       True, True, [B_KT[c], B_VT[c]], [bpkv])
                    act(T1[:, hd, :], S[:, hd, :], AF.Identity, [B_S[hd], B_EBL[hd]], [B_T1[hd]],
                        scale=EBL[:, hd, c:c + 1])
                    stt("dve", S[:, hd, :], pkv[:, 0:256], EBL[:, hd, c:c + 1], T1[:, hd, :], ALU.mult, ALU.add,
                        [bpkv, B_EBL[hd], B_T1[hd]], [B_S[hd]])
                    act(SBF[:, hd, :], S[:, hd, :], AF.Copy, [B_S[hd]], [B_SBF[hd]])
                for hh in range(2):
                    po, bpo = pso[hh]
                    act(SQ[:, hh, :], po[:, :], AF.Square, [bpo], [B_SQ[hh]])
                pn, bpn = next_ps()
                for hd in range(4):
                    for vh in range(2):
                        col = ((hd % 2) * 2 + vh) * 128
                        mm(pn[:, hd * 128:(hd + 1) * 128], ONES256, SQ[:, hd // 2, col:col + 128], vh == 0, vh == 1,
                           [B_CONST, B_SQ[hd // 2]], [bpn])
                act(RSTD, pn[:, :], AF.Sqrt, [bpn], [B_RSTD], bias=1e-6)
                P.op("dve", lambda e: e.reciprocal(out=RSTD, in_=RSTD), [B_RSTD], [B_RSTD])
                for hd in range(4):
                    po, bpo = pso[hd // 2]
                    for vh in range(2):
                        col = ((hd % 2) * 2 + vh) * 128
                        vt = hd * 2 + vh
                        tt("dve", ON[:, vt, c * 128:(c + 1) * 128], po[:, col:col + 128],
                           RSTD[:, hd * 128:(hd + 1) * 128], ALU.mult, [bpo, B_RSTD], [B_ON[vt]])
            for vt in range(8):
                ps, bps = next_ps()
                for kt in range(8):
                    mm(ps[:, 0:TB], W2[:, kt, o_g + vt * 128:o_g + (vt + 1) * 128], xt[:, kt, :], kt == 0, kt == 7,
                       [B_W2, bxt], [bps])
                act(SG[:, vt, :], ps[:, 0:TB], AF.Silu, [bps] + PR, [B_SG[vt]], bias=pcol(P_BG + vt))
                stt("dve", OFIN[:, vt, :], ON[:, vt, :], pcol(P_NG + vt), SG[:, vt, :], ALU.mult, ALU.mult,
                    [B_ON[vt], B_SG[vt]] + PR, [B_OFIN[vt]])
            for dt in range(8):
                ps, bps = next_ps()
                for kt in range(8):
                    mm(ps[:, 0:TB], W2[:, kt, o_gb + dt * 128:o_gb + (dt + 1) * 128], xt[:, kt, :], kt == 0, kt == 7,
                       [B_W2, bxt], [bps])
                sgb = SGB[n_mg % 2]; bsgb = B_SGB[n_mg % 2]
                tbb = TBB[n_mg % 2]; btbb = B_TBB[n_mg % 2]
                n_mg += 1
                act(sgb, ps[:, 0:TB], AF.Sigmoid, [bps] + PR, [bsgb], bias=pcol(P_BGB + dt))
                ps2, bps2 = next_ps()
                for vt in range(8):
                    mm(ps2[:, 0:TB], WGLA[:, vt, dt * 128:(dt + 1) * 128], OFIN[:, vt, :], vt == 0, vt == 7,
                       [B_WGLA, B_OFIN[vt]], [bps2])
                tt("dve", tbb, ps2[:, 0:TB], sgb, ALU.mult, [bps2, bsgb], [btbb])
                tt("dve", AGI[:, dt, :], tbb, AGI[:, dt, :], ALU.add, [btbb, B_AGI], [B_AGI])
            dma("sp", MG[:, t0:t0 + TB].rearrange("(dt p) n -> p dt n", p=128), AGI, [B_AGI], [B_MG])
        P.barrier()

        off[0] = g_mark
        WO = view([128, 8, 1024], BF16); B_WO = Buf()
        WRS = view([128, 8, 36]); B_WRS = Buf()
        RPB = view([128, 3 * 1024]); B_RPB = Buf()
        RBB = view([128, 36]); B_RBB = Buf()
        MGI = [view([128, 8, 512], BF16) for _ in range(2)]; B_MGI = [Buf() for _ in range(2)]
        NZ = 3
        XTM = [view([128, 1024]) for _ in range(NZ)]; B_XTM = [Buf() for _ in range(NZ)]
        Z = [view([128, 1024]) for _ in range(NZ)]; B_Z = [Buf() for _ in range(NZ)]
        X1B = [view([128, 1024], BF16) for _ in range(8)]; B_X1B = [Buf() for _ in range(8)]
        X1T = [view([128, 8, 128]) for _ in range(2)]; B_X1T = [Buf() for _ in range(2)]
        STATS = [view([128, 2, 6]) for _ in range(2)]; B_STATS = [Buf() for _ in range(2)]
        MV4 = [view([128, 4]) for _ in range(NZ)]; B_MV4 = [Buf() for _ in range(NZ)]
        LG4 = [view([128, 4, 36]) for _ in range(2)]; B_LG4 = [Buf() for _ in range(2)]
        RT = view([128, 1400]); B_RT = Buf()
        CBF = view([128, 4, 32], BF16); B_CBF = Buf()
        B_X1S = Buf(); B_XS = Buf()
        dma("pool", WO, wview(wo, 0, 1024), (), [B_WO])
        BOB = view([1, 1024], BF16); B_BOB = Buf()
        dma("pool", BOB, rp[:, R_BO:R_BO + 1024], (), [B_BOB])
        dma("sp", WRS, wr.rearrange("(kt p) n -> p kt n", p=128), (), [B_WRS])
        dma("sp", RPB, rp[:, R_BO:R_BO + 3072].partition_broadcast(128), (), [B_RPB])
        dma("sp", RBB, rp[:, R_RB:R_RB + 36].partition_broadcast(128), (), [B_RBB])
        _ro = [0]

        def rt(n, shape=None):
            v = RT[:, _ro[0]:_ro[0] + n]
            _ro[0] += n
            return v

        GMAX = rt(4); OHG = rt(16); DG = rt(16); SUMG = rt(4); PG = rt(4)
        T44 = rt(128); ESEL = rt(32); M1 = rt(4); OH1 = rt(32); E2 = rt(32); M2 = rt(4); OH2 = rt(32)
        DD = rt(4); W1c = rt(4); OH1F = rt(128); OH2F = rt(128); RKB = rt(128); RK = rt(128); TMPR = rt(128); PF = rt(8)
        v3 = lambda a, n: a.rearrange("p (c x) -> p c x", x=n)
        OHG3 = v3(OHG, 4); DG3 = v3(DG, 4); ESEL3 = v3(ESEL, 8); OH13 = v3(OH1, 8); E23 = v3(E2, 8); OH23 = v3(OH2, 8)
        T444 = T44.rearrange("p (c g e) -> p c g e", c=4, g=4)
        OH1F4 = OH1F.rearrange("p (c g e) -> p c g e", c=4, g=4); OH2F4 = OH2F.rearrange("p (c g e) -> p c g e", c=4, g=4)
        OH1F3 = v3(OH1F, 32); OH2F3 = v3(OH2F, 32); RKB3 = v3(RKB, 32); RK3 = v3(RK, 32); TMPR3 = v3(TMPR, 32)
        PF3 = v3(PF, 2)
        bc = lambda a, shp: a.broadcast_to(shp)
        R_ = [B_RT]

        def red(o, i, op):
            P.op("dve", lambda e: e.tensor_reduce(out=o, in_=i, axis=AX.X, op=op), R_, R_)

        def stage1(ch):
            j, cc = ch // 4, ch % 4
            t0 = ch * 128
            if cc == 0:
                dma("sp", MGI[j % 2], MG[:, j * 512:(j + 1) * 512].rearrange("(dt p) n -> p dt n", p=128), [B_MG],
                    [B_MGI[j % 2]])
            mgi = MGI[j % 2]; bmgi = B_MGI[j % 2]
            xtm = XTM[ch % NZ]; bxtm = B_XTM[ch % NZ]
            z = Z[ch % NZ]; bz = B_Z[ch % NZ]
            mv = MV4[ch % NZ]; bmv = B_MV4[ch % NZ]
            stt_ = STATS[ch % 2]; bst = B_STATS[ch % 2]
            x1b = X1B[ch % 8]; bx1b = B_X1B[ch % 8]
            dma("sp", xtm, xn[t0:t0 + 128, :], (), [bxtm])
            for half in range(2):
                ps, bps = next_ps()
                for jt in range(8):
                    mm(ps[:, :], mgi[:, jt, cc * 128:(cc + 1) * 128], WO[:, jt, half * 512:(half + 1) * 512],
                       jt == 0, False, [bmgi, B_WO], [bps])
                mm(ps[:, :], ONE1[0:1, :], BOB[0:1, half * 512:(half + 1) * 512], False, True, [B_CONST, B_BOB], [bps])
                stt("dve", z[:, half * 512:(half + 1) * 512], xtm[:, half * 512:(half + 1) * 512], ALPHA, ps[:, :],
                    ALU.mult, ALU.add, [bps, bxtm], [bz])
                P.op("dve", lambda e, o=stt_[:, half, :], i=z[:, half * 512:(half + 1) * 512]: e.bn_stats(out=o, in_=i),
                     [bz], [bst])
            P.op("dve", lambda e, o=mv[:, 0:2], i=stt_.rearrange("p a b -> p (a b)"): e.bn_aggr(out=o, in_=i),
                 [bst], [bmv])
            act(mv[:, 2:3], mv[:, 1:2], AF.Sqrt, [bmv], [bmv], bias=1e-5)
            P.op("dve", lambda e, o=mv[:, 2:3]: e.reciprocal(out=o, in_=o), [bmv], [bmv])
            stt("dve", mv[:, 3:4], mv[:, 0:1], -1.0, mv[:, 2:3], ALU.mult, ALU.mult, [bmv], [bmv])
            act(z, z, AF.Identity, [bz, bmv], [bz], scale=mv[:, 2:3], bias=mv[:, 3:4])
            tt("dve", z, z, RPB[:, 1024:2048], ALU.mult, [bz, B_RPB], [bz])
            tt("dve", z, z, RPB[:, 2048:3072], ALU.add, [bz, B_RPB], [bz])
            dma("sp", X1S[t0:t0 + 128, :], z, [bz], [B_X1S])
            act(x1b, z, AF.Copy, [bz], [bx1b])

        def stage2(ch):
            j, cc = ch // 4, ch % 4
            z = Z[ch % NZ]; bz = B_Z[ch % NZ]
            x1t = X1T[ch % 2]; bx1t = B_X1T[ch % 2]
            for half in range(2):
                ps, bps = next_ps()
                for q4 in range(4):
                    dtl = half * 4 + q4
                    tp(ps[:, q4 * 128:(q4 + 1) * 128], z[:, dtl * 128:(dtl + 1) * 128], IDF, [bz, B_CONST], [bps])
                act(x1t[:, half * 4:(half + 1) * 4, :], ps[:, :].rearrange("p (a b) -> p a b", b=128), AF.Copy,
                    [bps], [bx1t])
            ps, bps = next_ps()
            for dtl in range(8):
                mm(ps[:, 0:36], x1t[:, dtl, :], WRS[:, dtl, :], dtl == 0, dtl == 7, [bx1t, B_WRS], [bps])
            tt("dve", LG4[j % 2][:, cc, :], ps[:, 0:36], RBB, ALU.add, [bps, B_RBB], [B_LG4[j % 2]])

        def stageB(j):
            LG = LG4[j % 2]; blg = B_LG4[j % 2]
            ch0 = j * 4
            LGg = LG[:, :, 0:4]
            LGe = LG[:, :, 4:36].rearrange("p c (g e) -> p c g e", e=8)
            P.op("dve", lambda e: e.tensor_reduce(out=GMAX, in_=LGg, axis=AX.X, op=ALU.max), [blg] + R_, R_)
            tt("dve", OHG3, LGg, bc(GMAX.unsqueeze(2), [128, 4, 4]), ALU.is_equal, [blg] + R_, R_)
            tt("dve", DG3, LGg, bc(GMAX.unsqueeze(2), [128, 4, 4]), ALU.subtract, [blg] + R_, R_)
            act(DG, DG, AF.Exp, R_, R_)
            red(SUMG, DG3, ALU.add)
            P.op("dve", lambda e: e.reciprocal(out=PG, in_=SUMG), R_, R_)
            tt("dve", T444, LGe, bc(OHG3.unsqueeze(3), [128, 4, 4, 8]), ALU.mult, [blg] + R_, R_)
            red(ESEL3, T444.rearrange("p c g e -> p c e g"), ALU.add)
            red(M1, ESEL3, ALU.max)
            tt("dve", OH13, ESEL3, bc(M1.unsqueeze(2), [128, 4, 8]), ALU.is_equal, R_, R_)
            stt("dve", E2, OH1, -1e30, ESEL, ALU.mult, ALU.add, R_, R_)
            red(M2, E23, ALU.max)
            tt("dve", OH23, E23, bc(M2.unsqueeze(2), [128, 4, 8]), ALU.is_equal, R_, R_)
            tt("dve", DD, M2, M1, ALU.subtract, R_, R_)
            act(DD, DD, AF.Exp, R_, R_)
            ts("dve", DD, DD, 1.0, None, ALU.add, None, R_, R_)
            P.op("dve", lambda e: e.reciprocal(out=W1c, in_=DD), R_, R_)
            tt("dve", WTS[:, ch0:ch0 + 4, 0], W1c, PG, ALU.mult, R_, [B_WTS])
            tt("dve", WTS[:, ch0:ch0 + 4, 1], PG, WTS[:, ch0:ch0 + 4, 0], ALU.subtract, R_ + [B_WTS], [B_WTS])
            tt("dve", OH1F4, bc(OHG3.unsqueeze(3), [128, 4, 4, 8]), bc(OH13.unsqueeze(2), [128, 4, 4, 8]), ALU.mult, R_, R_)
            tt("dve", OH2F4, bc(OHG3.unsqueeze(3), [128, 4, 4, 8]), bc(OH23.unsqueeze(2), [128, 4, 4, 8]), ALU.mult, R_, R_)
            tt("dve", CBF, OH1F3, OH2F3, ALU.add, R_, [B_CBF])
            ps, bps = next_ps()
            for c in range(4):
                mm(ps[:, c * 64:c * 64 + 32], LTB, CBF[:, c, :], True, True, [B_CONST, B_CBF], [bps])
                mm(ps[:, c * 64 + 32:c * 64 + 64], ALL1, CBF[:, c, :], True, True, [B_CONST, B_CBF], [bps])
            psv = ps[:, 0:256].rearrange("p (c x) -> p c x", x=64)
            cp("dve", RKB3[:, 0, :], CNTB, [B_CNT] + R_, R_)
            for c in range(1, 4):
                tt("dve", RKB3[:, c, :], RKB3[:, c - 1, :], psv[:, c - 1, 32:64], ALU.add, [bps] + R_, R_)
            tt("dve", CNTB, RKB3[:, 3, :], psv[:, 3, 32:64], ALU.add, [bps] + R_, [B_CNT])
            tt("dve", RK3, psv[:, :, 0:32], RKB3, ALU.add, [bps] + R_, R_)
            tt("dve", TMPR3, OH1F3, RK3, ALU.mult, R_, R_)
            red(PF3[:, :, 0], TMPR3, ALU.add)
            tt("dve", TMPR3, OH2F3, RK3, ALU.mult, R_, R_)
            red(PF3[:, :, 1], TMPR3, ALU.add)
            cp("dve", POS[:, ch0:ch0 + 4, :], PF3, R_, [B_POS])
            for c in range(4):
                ch = ch0 + c
                for k in range(2):
                    P.op("pool", lambda e, ix=POS[:, ch, k:k + 1], src=X1B[ch % 8]: e.indirect_dma_start(
                        out=XS, out_offset=bass.IndirectOffsetOnAxis(ap=ix, axis=0), in_=src, in_offset=None),
                        [B_POS, B_X1B[ch % 8]], [B_XS], dma=True)

        NCHK = NTOK // 128
        for ch in range(NCHK + 1):
            if ch < NCHK:
                stage1(ch)
            if ch >= 1:
                stage2(ch - 1)
                if (ch - 1) % 4 == 3:
                    stageB((ch - 1) // 4)
        if debug:
            dma("sp", d_pos, POS.rearrange("p a b -> p (a b)"), [B_POS], [Buf()])
            dma("sp", d_wts, WTS.rearrange("p a b -> p (a b)"), [B_WTS], [Buf()])
        P.barrier()

        off[0] = g_mark
        NST = CAP // 128
        NWB = 3
        EW1 = [view([128, 8, 512], BF16) for _ in range(NWB)]
        EW3 = [view([128, 8, 512], BF16) for _ in range(NWB)]
        EW2 = [view([128, 4, 1024], BF16) for _ in range(NWB)]
        B_EW = [[Buf(), Buf(), Buf()] for _ in range(NWB)]
        XSL = [view([128, NST, 1024], BF16) for _ in range(2)]; B_XSL = [Buf() for _ in range(2)]
        XST = [view([128, 8, CAP], BF16) for _ in range(2)]; B_XST = [Buf() for _ in range(2)]
        HT = [view([128, 4, CAP], BF16) for _ in range(2)]; B_HT = [[Buf() for _ in range(4)] for _ in range(2)]
        S1 = [view([128, CAP]) for _ in range(2)]; B_S1 = [Buf() for _ in range(2)]
        YSB = [view([128, 1024]) for _ in range(3)]; B_YSB = [Buf() for _ in range(3)]
        B_YS = Buf()
        n_s1 = [0]
        n_y = [0]

        def p4_load(e):
            w = e % NWB
            dma("pool", EW1[w], ew1[e].rearrange("(kt p) n -> p kt n", p=128), (), [B_EW[w][0]])
            dma("pool", EW3[w], ew3[e].rearrange("(kt p) n -> p kt n", p=128), (), [B_EW[w][1]])
            dma("pool", EW2[w], ew2[e].rearrange("(kt p) n -> p kt n", p=128), (), [B_EW[w][2]])
            dma("sp", XSL[e % 2], XS[e * CAP:(e + 1) * CAP, :].rearrange("(s p) n -> p s n", p=128), [B_XS], [B_XSL[e % 2]])

        def p4_T(e):
            p = e % 2
            for s_ in range(NST):
                pst, bpst = next_ps()
                pstb = pst[:, :].bitcast(BF16)
                for dtl in range(8):
                    tp(pstb[:, dtl * 128:(dtl + 1) * 128], XSL[p][:, s_, dtl * 128:(dtl + 1) * 128], IDB,
                       [B_XSL[p], B_CONST], [bpst])
                act(XST[p][:, :, s_ * 128:(s_ + 1) * 128], pstb.rearrange("p (a b) -> p a b", b=128), AF.Copy,
                    [bpst], [B_XST[p]])

        def p4_H(e):
            p = e % 2; w = e % NWB
            for ft in range(4):
                ps1, bps1 = next_ps()
                for kt in range(8):
                    mm(ps1[:, 0:CAP], EW1[w][:, kt, ft * 128:(ft + 1) * 128], XST[p][:, kt, :], kt == 0, kt == 7,
                       [B_EW[w][0], B_XST[p]], [bps1])
                ps3, bps3 = next_ps()
                for kt in range(8):
                    mm(ps3[:, 0:CAP], EW3[w][:, kt, ft * 128:(ft + 1) * 128], XST[p][:, kt, :], kt == 0, kt == 7,
                       [B_EW[w][1], B_XST[p]], [bps3])
                s1 = S1[n_s1[0] % 2]; bs1 = B_S1[n_s1[0] % 2]
                n_s1[0] += 1
                act(s1, ps1[:, 0:CAP], AF.Silu, [bps1], [bs1])
                tt("dve", HT[p][:, ft, :], ps3[:, 0:CAP], s1, ALU.mult, [bps3, bs1], [B_HT[p][ft]])

        def p4_Y(e):
            p = e % 2; w = e % NWB
            for s_ in range(NST):
                ysb = YSB[n_y[0] % 3]; bysb = B_YSB[n_y[0] % 3]
                n_y[0] += 1
                for half in range(2):
                    ps, bps = next_ps()
                    for ft in range(4):
                        mm(ps[:, :], HT[p][:, ft, s_ * 128:(s_ + 1) * 128], EW2[w][:, ft, half * 512:(half + 1) * 512],
                           ft == 0, ft == 3, [B_HT[p][ft], B_EW[w][2]], [bps])
                    if half == 0:
                        act(ysb[:, 0:512], ps[:, :], AF.Copy, [bps], [bysb])
                    else:
                        cp("dve", ysb[:, 512:1024], ps[:, :], [bps], [bysb])
                r0 = e * CAP + s_ * 128
                dma("sp", YS[r0:r0 + 128, :], ysb, [bysb], [B_YS])

        p4_load(0)
        p4_load(1)
        p4_T(0)
        for e in range(NEXP):
            if e + 2 < NEXP:
                p4_load(e + 2)
            p4_H(e)
            if e + 1 < NEXP:
                p4_T(e + 1)
            p4_Y(e)
        P.barrier()

        off[0] = g_mark
        NR5 = 3
        L2 = view([128, 2048]); B_L2 = Buf()
        Y1 = [view([128, 1024]) for _ in range(NR5)]; B_Y1 = [Buf() for _ in range(NR5)]
        Y2 = [view([128, 1024]) for _ in range(NR5)]; B_Y2 = [Buf() for _ in range(NR5)]
        XA = [view([128, 1024]) for _ in range(NR5)]; B_XA = [Buf() for _ in range(NR5)]
        STATS5 = [view([128, 2, 6]) for _ in range(2)]; B_ST5 = [Buf() for _ in range(2)]
        MV5 = [view([128, 4]) for _ in range(2)]; B_MV5 = [Buf() for _ in range(2)]
        dma("sp", L2, rp[:, R_L2G:R_L2G + 2048].partition_broadcast(128), (), [B_L2])

        def p5_load(ch):
            t0 = ch * 128
            r = ch % NR5
            dma("sp", XA[r], X1S[t0:t0 + 128, :], [B_X1S], [B_XA[r]])
            P.op("pool", lambda e, ix=POS[:, ch, 0:1], o=Y1[r]: e.indirect_dma_start(
                out=o, out_offset=None, in_=YS, in_offset=bass.IndirectOffsetOnAxis(ap=ix, axis=0)),
                [B_POS, B_YS], [B_Y1[r]], dma=True)
            P.op("pool", lambda e, ix=POS[:, ch, 1:2], o=Y2[r]: e.indirect_dma_start(
                out=o, out_offset=None, in_=YS, in_offset=bass.IndirectOffsetOnAxis(ap=ix, axis=0)),
                [B_POS, B_YS], [B_Y2[r]], dma=True)

        def p5_comp(ch):
            t0 = ch * 128
            r = ch % NR5
            xa = XA[r]; bxa = B_XA[r]; y1 = Y1[r]; by1 = B_Y1[r]; y2 = Y2[r]; by2 = B_Y2[r]
            st5 = STATS5[ch % 2]; bst5 = B_ST5[ch % 2]; mv5 = MV5[ch % 2]; bmv5 = B_MV5[ch % 2]
            act(xa, xa, AF.Identity, [bxa], [bxa], scale=ALPHA)
            stt("dve", xa, y1, WTS[:, ch, 0:1], xa, ALU.mult, ALU.add, [by1, bxa, B_WTS], [bxa])
            stt("dve", xa, y2, WTS[:, ch, 1:2], xa, ALU.mult, ALU.add, [by2, bxa, B_WTS], [bxa])
            for half in range(2):
                P.op("dve", lambda e, o=st5[:, half, :], i=xa[:, half * 512:(half + 1) * 512]: e.bn_stats(out=o, in_=i),
                     [bxa], [bst5])
            P.op("dve", lambda e, o=mv5[:, 0:2], i=st5.rearrange("p a b -> p (a b)"): e.bn_aggr(out=o, in_=i),
                 [bst5], [bmv5])
            act(mv5[:, 2:3], mv5[:, 1:2], AF.Sqrt, [bmv5], [bmv5], bias=1e-5)
            P.op("dve", lambda e, o=mv5[:, 2:3]: e.reciprocal(out=o, in_=o), [bmv5], [bmv5])
            stt("dve", mv5[:, 3:4], mv5[:, 0:1], -1.0, mv5[:, 2:3], ALU.mult, ALU.mult, [bmv5], [bmv5])
            act(xa, xa, AF.Identity, [bxa, bmv5], [bxa], scale=mv5[:, 2:3], bias=mv5[:, 3:4])
            tt("dve", xa, xa, L2[:, 0:1024], ALU.mult, [bxa, B_L2], [bxa])
            tt("dve", xa, xa, L2[:, 1024:2048], ALU.add, [bxa, B_L2], [bxa])
            dma("sp", out[t0:t0 + 128, :], xa, [bxa], [Buf()])

        NCH5 = NTOK // 128
        p5_load(0)
        p5_load(1)
        for ch in range(NCH5):
            if ch + 2 < NCH5:
                p5_load(ch + 2)
            p5_comp(ch)
        P.barrier()

        with nc.Block() as block:
            P.emit(block)
    return nc


def _host_inputs(inputs):
    f = lambda k: np.ascontiguousarray(np.asarray(inputs[k], dtype=np.float32)[0])
    x = np.asarray(inputs["x"], dtype=np.float32)
    b_in = f("b_in")
    pp = np.zeros((128, 128), np.float32)

    def put(col, vec):
        n = vec.shape[0] // 128
        pp[:, col:col + n] = vec.reshape(n, 128).T

    put(P_BRX, b_in[C_RX:C_RX + 1024]); put(P_BRY, b_in[C_RY:C_RY + 1024]); put(P_BQ, b_in[C_Q:C_Q + 512])
    put(P_BK, b_in[C_K:C_K + 512]); put(P_BG, b_in[C_G:C_G + 1024])
    pp[0:16, P_BALR] = b_in[C_ALR:C_ALR + 16]
    put(P_BGA, b_in[C_GA:C_GA + 1024]); put(P_BGB, b_in[C_GB:C_GB + 1024])
    cw = f("conv_w")
    pp[:, P_CW:P_CW + 32] = cw.reshape(4, 8, 128).transpose(2, 1, 0).reshape(128, 32)
    put(P_CB, f("conv_b")); put(P_RBA, f("rg_b_a")); put(P_RBX, f("rg_b_x")); put(P_LAM, f("rg_lambda"))
    put(P_GBA, f("gla_b_a")); put(P_NG, f("gla_norm_g"))
    rp = np.zeros((1, NRP), np.float32)
    rp[0, R_BV:R_BV + 1024] = b_in[C_V:C_V + 1024]
    rp[0, R_BO:R_BO + 1024] = f("b_o"); rp[0, R_L1G:R_L1G + 1024] = f("ln1_g"); rp[0, R_L1B:R_L1B + 1024] = f("ln1_b")
    rp[0, R_L2G:R_L2G + 1024] = f("ln2_g"); rp[0, R_L2B:R_L2B + 1024] = f("ln2_b")
    rp[0, R_RB:R_RB + 4] = f("router_b_group"); rp[0, R_RB + 4:R_RB + 36] = f("router_b_expert")
    wr = np.ascontiguousarray(np.concatenate([f("router_w_group"), f("router_w_expert")], axis=1))
    rgw = np.ascontiguousarray(np.stack([f("rg_w_a"), f("rg_w_x")], axis=0))
    ii = np.arange(128)
    c_id = np.eye(128, dtype=np.float32)
    c_lt = (ii[:, None] < ii[None, :]).astype(np.float32)
    c_cm = np.tile((ii[None, :] >= ii[:, None]).astype(np.float32), (1, 4))
    c_mc = np.ones((128, 512), np.float32); c_mc[:, ::128] = 0.0
    c_eb = np.tile((np.arange(NEXP, dtype=np.float32) * CAP)[None, :], (128, 1))
    shared = {
        "w_in": f("w_in"), "pp": pp, "rp": rp, "wr": wr, "rgw": rgw, "wa2": f("gla_w_a2"),
        "wrnn": f("w_proj_rnn"), "wgla": f("w_proj_gla"), "wo": f("w_o"),
        "ew1": f("exp_w1"), "ew3": f("exp_w3"), "ew2": f("exp_w2"),
        "c_id": c_id, "c_lt": c_lt, "c_cm": c_cm, "c_mc": c_mc, "c_eb": c_eb,
    }
    in_maps = []
    for c in range(NCORES):
        xc = np.ascontiguousarray(x[2 * c:2 * c + 2].reshape(NTOK, 1024))
        m = dict(shared)
        m["xn"] = xc
        m["xT"] = np.ascontiguousarray(xc.T)
        in_maps.append(m)
    return in_maps


def kernel(**inputs):
    in_maps = _host_inputs(inputs)
    nc = build(debug=False)
    res = run_bass_kernel_spmd(nc, in_maps, core_ids=list(range(NCORES)))
    outs = [np.asarray(r["out"], dtype=np.float32).reshape(2, SEQ, 1024) for r in res.results]
    return np.concatenate(outs, axis=0)
```

```python
import numpy as np
from contextlib import ExitStack
import concourse.bass as bass
import concourse.mybir as mybir
from concourse.bass_utils import run_bass_kernel_spmd

F32 = mybir.dt.float32
BF16 = mybir.dt.bfloat16
I32 = mybir.dt.int32
AF = mybir.ActivationFunctionType
ALU = mybir.AluOpType
AX = mybir.AxisListType

SAME_ENGINE_SYNC = True
DMA_RING = 12
NCORES = 8
NTOK = 4096
SEQ = 2048
CAP = 384
NEXP = 32
ALPHA = 2.0 ** 0.25
SBUF_WORDS = 44 * 1024

C_RX, C_RY, C_Q, C_K, C_V, C_G, C_ALR, C_GA, C_GB = 0, 1024, 2048, 2560, 3072, 4096, 5120, 5136, 6160
P_BRX, P_BRY, P_BQ, P_BK, P_BG, P_BALR, P_BGA, P_BGB = 0, 8, 16, 20, 24, 32, 33, 41
P_CW, P_CB, P_RBA, P_RBX, P_LAM, P_GBA, P_NG = 49, 81, 89, 97, 105, 113, 117
R_BV, R_BO, R_L1G, R_L1B, R_L2G, R_L2B, R_RB = 0, 1024, 2048, 3072, 4096, 5120, 6144
NRP = 6180


class Buf:
    __slots__ = ("w", "r")

    def __init__(self):
        self.w = None
        self.r = {}


class _Eng:
    def __init__(self, name, sem, dma_sems):
        self.name = name
        self.sem = sem
        self.count = 0
        self.seen = {}
        self.ops = []
        self.dma_sems = dma_sems
        self.dma_n = 0


class Prog:
    def __init__(self, nc, stack):
        self.nc = nc
        self.E = {}
        for name in ("pe", "act", "dve", "pool", "sp"):
            sem = stack.enter_context(nc.semaphore("s_" + name))
            dsems = []
            if name in ("sp", "pool"):
                dsems = [stack.enter_context(nc.semaphore("d_%s%d" % (name, i))) for i in range(DMA_RING)]
            self.E[name] = _Eng(name, sem, dsems)

    def op(self, eng, fn, reads=(), writes=(), dma=False):
        E = self.E[eng]
        deps = {}

        def add(t):
            if t is None:
                return
            if deps.get(t[0], (None, 0))[1] < t[1]:
                deps[t[0]] = t

        for b in reads:
            add(b.w)
        for b in writes:
            add(b.w)
            for t in b.r.values():
                add(t)
        waits = []
        for s, v in deps.values():
            if E.seen.get(s, 0) >= v:
                continue
            if s is E.sem and (eng == "pe" or not SAME_ENGINE_SYNC):
                continue
            waits.append((s, v))
            E.seen[s] = v
        if dma:
            slot = E.dma_n % DMA_RING
            sem = E.dma_sems[slot]
            val = 16 * (E.dma_n // DMA_RING + 1)
            if val > 16 and E.seen.get(sem, 0) < val - 16:
                waits.append((sem, val - 16))
                E.seen[sem] = val - 16
            E.dma_n += 1
            tok = (sem, val)
            inc = 16
        else:
            E.count += 1
            tok = (E.sem, E.count)
            inc = 1
        E.ops.append((waits, fn, tok[0], inc))
        for b in reads:
            if b.r.get(tok[0], (None, 0))[1] < tok[1]:
                b.r[tok[0]] = tok
        for b in writes:
            b.w = tok
            b.r = {}
        return tok

    def barrier(self):
        toks = []
        for E in self.E.values():
            if E.count:
                toks.append((E.sem, E.count))
            for i, s in enumerate(E.dma_sems):
                n = (E.dma_n - 1 - i) // DMA_RING + 1 if E.dma_n > i else 0
                if n > 0:
                    toks.append((s, 16 * n))
        for E in self.E.values():
            waits = []
            for s, v in toks:
                if s is E.sem or E.seen.get(s, 0) >= v:
                    continue
                waits.append((s, v))
                E.seen[s] = v
            if waits:
                E.ops.append((waits, None, None, 0))

    def emit(self, block):
        def run(E):
            def body(eng):
                for waits, fn, sem, inc in E.ops:
                    for s, v in waits:
                        eng.wait_ge(s, v)
                    if fn is not None:
                        fn(eng).then_inc(sem, inc)
            return body

        block.tensor(run(self.E["pe"]))
        block.scalar(run(self.E["act"]))
        block.vector(run(self.E["dve"]))
        block.gpsimd(run(self.E["pool"]))
        block.sync(run(self.E["sp"]))


def build(debug=False):
    nc = bass.Bass("TRN2", target_bir_lowering=False)

    def din(name, shape, dt=F32):
        return nc.dram_tensor(name, list(shape), dt, kind="ExternalInput").ap()

    xT = din("xT", [1024, NTOK]); xn = din("xn", [NTOK, 1024]); w_in = din("w_in", [1024, 7184])
    pp = din("pp", [128, 128]); rp = din("rp", [1, NRP]); wr = din("wr", [1024, 36])
    rgw = din("rgw", [2, 8, 128, 128]); wa2 = din("wa2", [16, 512])
    wrnn = din("wrnn", [1024, 1024]); wgla = din("wgla", [1024, 1024]); wo = din("wo", [1024, 1024])
    ew1 = din("ew1", [NEXP, 1024, 512]); ew3 = din("ew3", [NEXP, 1024, 512]); ew2 = din("ew2", [NEXP, 512, 1024])
    c_id = din("c_id", [128, 128]); c_lt = din("c_lt", [128, 128]); c_cm = din("c_cm", [128, 512])
    c_mc = din("c_mc", [128, 512]); c_eb = din("c_eb", [128, 32])
    out = nc.dram_tensor("out", [NTOK, 1024], F32, kind="ExternalOutput").ap()
    sk = "ExternalOutput" if debug else "Internal"
    AG = nc.dram_tensor("AG", [1024, NTOK], BF16, kind=sk).ap()
    MG = nc.dram_tensor("MG", [1024, NTOK], BF16, kind=sk).ap()
    X1S = nc.dram_tensor("X1S", [NTOK, 1024], F32, kind=sk).ap()
    XS = nc.dram_tensor("XS", [NEXP * CAP, 1024], BF16, kind=sk).ap()
    YS = nc.dram_tensor("YS", [NEXP * CAP, 1024], F32, kind=sk).ap()
    if debug:
        d_pos = nc.dram_tensor("d_pos", [128, 64], I32, kind="ExternalOutput").ap()
        d_wts = nc.dram_tensor("d_wts", [128, 64], F32, kind="ExternalOutput").ap()

    st = ExitStack()
    with st:
        big = st.enter_context(nc.sbuf_tensor("big", [128, SBUF_WORDS], F32))
        PSB = [st.enter_context(nc.psum_tensor("ps%d" % i, [128, 512], F32)) for i in range(8)]
        PSBUF = [Buf() for _ in range(8)]
        P = Prog(nc, st)
        off = [0]
        psn = [0]

        def view(shape, dt=F32):
            n = 1
            for s in shape[1:]:
                n *= s
            nw = (n * (2 if dt == BF16 else 4) + 3) // 4
            nw = (nw + 7) // 8 * 8
            assert off[0] + nw <= SBUF_WORDS, ("SBUF overflow", off[0], nw)
            v = big[:, off[0]:off[0] + nw]
            off[0] += nw
            if dt != F32:
                v = v.bitcast(dt)
            if dt == BF16 and n % 2:
                v = v[:, 0:n]
            elif dt == BF16:
                v = v[:, 0:n]
            else:
                v = v[:, 0:n]
            if len(shape) > 2:
                names = " ".join("a%d" % i for i in range(len(shape) - 1))
                kw = {"a%d" % i: shape[i + 1] for i in range(len(shape) - 1)}
                v = v.rearrange("p (%s) -> p %s" % (names, names), **kw)
            if shape[0] != 128:
                v = v[0:shape[0]]
            return v

        def next_ps():
            i = psn[0] % 8
            psn[0] += 1
            return PSB[i], PSBUF[i]

        def mm(o, lhsT, rhs, start, stop, reads, writes):
            P.op("pe", lambda e: e.matmul(o, lhsT=lhsT, rhs=rhs, start=start, stop=stop), reads, writes)

        def tp(o, in_, ident, reads, writes):
            P.op("pe", lambda e: e.transpose(o, in_, ident), reads, writes)

        def act(o, in_, func, reads, writes, bias=None, scale=None, accum=None):
            kw = {}
            if bias is not None:
                kw["bias"] = bias
            if scale is not None:
                kw["scale"] = scale
            if accum is not None:
                kw["accum_out"] = accum
            P.op("act", lambda e: e.activation(out=o, in_=in_, func=func, **kw), reads, writes)

        def ts(eng, o, in0, s1, s2, op0, op1, reads, writes):
            if op1 is None:
                P.op(eng, lambda e: e.tensor_scalar(out=o, in0=in0, scalar1=s1, scalar2=None, op0=op0), reads, writes)
            else:
                P.op(eng, lambda e: e.tensor_scalar(out=o, in0=in0, scalar1=s1, scalar2=s2, op0=op0, op1=op1), reads, writes)

        def tt(eng, o, in0, in1, op, reads, writes):
            P.op(eng, lambda e: e.tensor_tensor(out=o, in0=in0, in1=in1, op=op), reads, writes)

        def stt(eng, o, in0, sc, in1, op0, op1, reads, writes):
            P.op(eng, lambda e: e.scalar_tensor_tensor(out=o, in0=in0, scalar=sc, in1=in1, op0=op0, op1=op1), reads, writes)

        def cp(eng, o, in_, reads, writes):
            P.op(eng, lambda e: e.tensor_copy(out=o, in_=in_), reads, writes)

        def ms(eng, o, val, writes):
            P.op(eng, lambda e: e.memset(o, val), (), writes)

        def dma(eng, o, in_, reads, writes):
            P.op(eng, lambda e: e.dma_start(out=o, in_=in_), reads, writes, dma=True)

        def wview(w, c0, c1):
            return w[:, c0:c1].rearrange("(kt p) n -> p kt n", p=128)

        PP = view([128, 128]); B_PP = Buf()
        DER = view([128, 32]); B_DER = Buf()
        TMP8 = view([128, 8]); B_TMP8 = Buf()
        IDB = view([128, 128], BF16); IDF = view([128, 128]); LTB = view([128, 128], BF16)
        ALL1 = view([128, 128], BF16); ONES256 = view([128, 128], BF16); ONE1 = view([128, 128], BF16)
        CMB = view([128, 512], BF16); MCF = view([128, 512]); CNTB = view([128, 32])
        POS = view([128, 32, 2], I32); WTS = view([128, 32, 2])
        B_CONST = Buf(); B_CNT = Buf(); B_POS = Buf(); B_WTS = Buf()
        dma("sp", PP, pp, (), [B_PP])
        dma("sp", IDF, c_id, (), [B_CONST])
        dma("sp", MCF, c_mc, (), [B_CONST])
        dma("sp", CNTB, c_eb, (), [B_CNT])
        dma("pool", IDB, c_id, (), [B_CONST])
        dma("pool", LTB, c_lt, (), [B_CONST])
        dma("pool", CMB, c_cm, (), [B_CONST])
        ms("dve", ALL1, 1.0, [B_CONST])
        ms("dve", ONES256, 1.0 / 256.0, [B_CONST])
        ms("dve", ONE1, 1.0, [B_CONST])
        act(TMP8, PP[:, P_LAM:P_LAM + 8], AF.Exp, [B_PP], [B_TMP8], scale=-1.0)
        act(TMP8, TMP8, AF.Ln, [B_TMP8], [B_TMP8], bias=1.0)
        ts("dve", DER[:, 0:8], TMP8, -8.0, None, ALU.mult, None, [B_TMP8], [B_DER])
        ts("dve", DER[:, 8:16], TMP8, -16.0, None, ALU.mult, None, [B_TMP8], [B_DER])
        ts("dve", DER[:, 16:20], PP[:, P_GBA:P_GBA + 4], -1.0, None, ALU.mult, None, [B_PP], [B_DER])
        g_mark = off[0]
        PR = [B_PP, B_DER]

        def pcol(c):
            return PP[:, c:c + 1]

        W1 = view([128, 8, 3072], BF16); B_W1 = [Buf() for _ in range(3)]
        WRNN = view([128, 8, 1024], BF16); B_WRNN = Buf()
        RGW = view([128, 2, 8, 128], BF16); B_RGW = Buf()
        XT = [view([128, 8, 512], BF16) for _ in range(2)]; B_XT = [Buf() for _ in range(2)]
        NS = 2
        RX = [view([128, 2, 516]) for _ in range(NS)]; B_RX = [[Buf(), Buf()] for _ in range(NS)]
        U = [view([128, 2, 512]) for _ in range(NS)]; B_U = [[Buf(), Buf()] for _ in range(NS)]
        UBF = [view([128, 2, 512], BF16) for _ in range(NS)]; B_UBF = [[Buf(), Buf()] for _ in range(NS)]
        THR = [view([128, 2, 512]) for _ in range(NS)]; B_THR = [[Buf(), Buf()] for _ in range(NS)]
        A2 = [view([128, 2, 512]) for _ in range(NS)]; B_A2 = [[Buf(), Buf()] for _ in range(NS)]
        THI = [view([128, 2, 512]) for _ in range(NS)]; B_THI = [[Buf(), Buf()] for _ in range(NS)]
        GY = [view([128, 2, 512], BF16) for _ in range(NS)]; B_GY = [[Buf(), Buf()] for _ in range(NS)]
        HG = [view([128, 8, 512], BF16) for _ in range(2)]; B_HG = [[Buf() for _ in range(8)] for _ in range(2)]
        HALO = view([128, 8, 4]); B_HALO = [Buf() for _ in range(8)]
        HST = view([128, 8]); B_HST = [Buf() for _ in range(8)]
        SGA = [view([128, 512]) for _ in range(2)]; B_SGA = [Buf() for _ in range(2)]
        AGO = [view([128, 512], BF16) for _ in range(2)]; B_AGO = [Buf() for _ in range(2)]
        B_AG = Buf()
        DER2 = view([128, 32]); B_DER2 = Buf()
        ts("dve", DER2[:, 0:8], DER[:, 0:8], 0.5, None, ALU.mult, None, [B_DER], [B_DER2])
        ts("dve", DER2[:, 8:16], PP[:, P_RBA:P_RBA + 8], 0.5, None, ALU.mult, None, [B_PP], [B_DER2])
        ts("dve", DER2[:, 16:24], PP[:, P_RBX:P_RBX + 8], 0.5, None, ALU.mult, None, [B_PP], [B_DER2])
        ts("dve", DER2[:, 24:32], PP[:, P_BGA:P_BGA + 8], 0.5, None, ALU.mult, None, [B_PP], [B_DER2])
        PR1 = PR + [B_DER2]

        for i, c0 in enumerate((C_RX, C_RY, C_GA)):
            dma("pool", W1[:, :, i * 1024:(i + 1) * 1024], wview(w_in, c0, c0 + 1024), (), [B_W1[i]])
        dma("pool", RGW, rgw.rearrange("g h i j -> i g h j"), (), [B_RGW])
        dma("pool", WRNN, wview(wrnn, 0, 1024), (), [B_WRNN])

        def p1_A(G):
            j, g = G // 4, G % 4
            t0 = j * 512
            first = (j % 4 == 0)
            xt = XT[j % 2]; bxt = B_XT[j % 2]
            sset = G % NS
            if g == 0:
                dma("pool", xt, xT[:, t0:t0 + 512].rearrange("(kt p) n -> p kt n", p=128), (), [bxt])
            for ci in range(2):
                c = g * 2 + ci
                rx = RX[sset]; brx = B_RX[sset][ci]
                u = U[sset]; bu = B_U[sset][ci]
                ps, bps = next_ps()
                for kt in range(8):
                    mm(ps[:, :], W1[:, kt, c * 128:(c + 1) * 128], xt[:, kt, :], kt == 0, kt == 7, [B_W1[0], bxt], [bps])
                if first:
                    ms("pool", rx[:, ci, 0:3], 0.0, [brx])
                else:
                    cp("pool", rx[:, ci, 0:3], HALO[:, c, 0:3], [B_HALO[c]], [brx])
                act(rx[:, ci, 3:515], ps[:, :], AF.Identity, [bps] + PR1, [brx], bias=pcol(P_BRX + c))
                cp("pool", HALO[:, c, 0:3], rx[:, ci, 512:515], [brx], [B_HALO[c]])
                act(u[:, ci, :], rx[:, ci, 3:515], AF.Identity, [brx] + PR1, [bu], scale=pcol(P_CW + c * 4 + 3),
                    bias=pcol(P_CB + c))
                for k in (0, 1, 2):
                    stt("dve", u[:, ci, :], rx[:, ci, k:k + 512], pcol(P_CW + c * 4 + k), u[:, ci, :],
                        ALU.mult, ALU.add, [brx, bu] + PR1, [bu])
            for ci in range(2):
                c = g * 2 + ci
                ps, bps = next_ps()
                for kt in range(8):
                    mm(ps[:, :], W1[:, kt, 1024 + c * 128:1024 + (c + 1) * 128], xt[:, kt, :], kt == 0, kt == 7,
                       [B_W1[1], bxt], [bps])
                act(GY[sset][:, ci, :], ps[:, :], AF.Gelu_apprx_tanh, [bps] + PR1, [B_GY[sset][ci]], bias=pcol(P_BRY + c))
            for ci in range(2):
                cp("pool", UBF[sset][:, ci, :], U[sset][:, ci, :], [B_U[sset][ci]], [B_UBF[sset][ci]])

        def p1_B(G):
            j, g = G // 4, G % 4
            first = (j % 4 == 0)
            sset = G % NS
            u = U[sset]; thr = THR[sset]; thi = THI[sset]; a2 = A2[sset]; ubf = UBF[sset]; gy = GY[sset]
            for ci in range(2):
                c = g * 2 + ci
                ps, bps = next_ps()
                mm(ps[:, :], RGW[:, 0, c, :], ubf[:, ci, :], True, True, [B_RGW, B_UBF[sset][ci]], [bps])
                act(thr[:, ci, :], ps[:, :], AF.Tanh, [bps] + PR1, [B_THR[sset][ci]], bias=DER2[:, 8 + c:9 + c], scale=0.5)
                ps, bps = next_ps()
                mm(ps[:, :], RGW[:, 1, c, :], ubf[:, ci, :], True, True, [B_RGW, B_UBF[sset][ci]], [bps])
                act(thi[:, ci, :], ps[:, :], AF.Tanh, [bps] + PR1, [B_THI[sset][ci]], bias=DER2[:, 16 + c:17 + c], scale=0.5)
            for ci in range(2):
                c = g * 2 + ci
                act(thr[:, ci, :], thr[:, ci, :], AF.Exp, [B_THR[sset][ci]] + PR1, [B_THR[sset][ci]],
                    scale=DER2[:, c:c + 1], bias=DER2[:, c:c + 1])
                tt("pool", a2[:, ci, :], thr[:, ci, :], thr[:, ci, :], ALU.mult, [B_THR[sset][ci]], [B_A2[sset][ci]])
            for ci in range(2):
                ts("dve", a2[:, ci, :], a2[:, ci, :], 0.99999994, -1.0, ALU.min, ALU.mult, [B_A2[sset][ci]], [B_A2[sset][ci]])
                stt("dve", thi[:, ci, :], thi[:, ci, :], 1.0, u[:, ci, :], ALU.add, ALU.mult,
                    [B_THI[sset][ci], B_U[sset][ci]], [B_THI[sset][ci]])
            for ci in range(2):
                act(a2[:, ci, :], a2[:, ci, :], AF.Sqrt, [B_A2[sset][ci]], [B_A2[sset][ci]], bias=0.25, scale=0.25)
            for ci in range(2):
                c = g * 2 + ci
                tt("dve", thi[:, ci, :], thi[:, ci, :], a2[:, ci, :], ALU.mult, [B_A2[sset][ci], B_THI[sset][ci]],
                   [B_THI[sset][ci]])
                init = 0.0 if first else HST[:, c:c + 1]
                P.op("dve", lambda e, o=u[:, ci, :], d0=thr[:, ci, :], d1=thi[:, ci, :], ini=init:
                     e.tensor_tensor_scan(out=o, data0=d0, data1=d1, initial=ini, op0=ALU.mult, op1=ALU.add),
                     [B_THR[sset][ci], B_THI[sset][ci], B_HST[c], B_U[sset][ci]], [B_U[sset][ci]])
                cp("dve", HST[:, c:c + 1], u[:, ci, 511:512], [B_U[sset][ci]], [B_HST[c]])
                stt("dve", HG[j % 2][:, c, :], u[:, ci, :], 0.5, gy[:, ci, :], ALU.mult, ALU.mult,
                    [B_U[sset][ci], B_GY[sset][ci]], [B_HG[j % 2][c]])

        n_ag = [0]

        def p1_C(j):
            t0 = j * 512
            xt = XT[j % 2]; bxt = B_XT[j % 2]
            for dt in range(8):
                ps, bps = next_ps()
                for kt in range(8):
                    mm(ps[:, :], W1[:, kt, 2048 + dt * 128:2048 + (dt + 1) * 128], xt[:, kt, :], kt == 0, kt == 7,
                       [B_W1[2], bxt], [bps])
                sga = SGA[n_ag[0] % 2]; bsga = B_SGA[n_ag[0] % 2]
                ago = AGO[n_ag[0] % 2]; bago = B_AGO[n_ag[0] % 2]
                n_ag[0] += 1
                act(sga, ps[:, :], AF.Tanh, [bps] + PR1, [bsga], bias=DER2[:, 24 + dt:25 + dt], scale=0.5)
                ps2, bps2 = next_ps()
                for c in range(8):
                    mm(ps2[:, :], WRNN[:, c, dt * 128:(dt + 1) * 128], HG[j % 2][:, c, :], c == 0, c == 7,
                       [B_WRNN, B_HG[j % 2][c]], [bps2])
                stt("dve", ago, sga, 1.0, ps2[:, :], ALU.add, ALU.mult, [bps2, bsga], [bago])
                dma("sp", AG[dt * 128:(dt + 1) * 128, t0:t0 + 512], ago, [bago], [B_AG])

        NG = (NTOK // 512) * 4
        p1_A(0)
        for G in range(NG):
            if G + 1 < NG:
                p1_A(G + 1)
            p1_B(G)
            if G % 4 == 0 and G >= 4:
                p1_C(G // 4 - 1)
        p1_C(NTOK // 512 - 1)
        P.barrier()

        off[0] = g_mark
        TB = 256
        NCH = TB // 128
        NW2 = 4112
        W2 = view([128, 8, NW2], BF16); B_W2 = Buf()
        o_q, o_k, o_v, o_g, o_alr, o_gb = 0, 512, 1024, 2048, 3072, 3088
        WGLA = view([128, 8, 1024], BF16); B_WGLA = Buf()
        WA2 = view([16, 512], BF16); B_WA2 = Buf()
        BVB = view([1, 1024], BF16); B_BVB = Buf()
        XT2 = [view([128, 8, TB], BF16) for _ in range(2)]; B_XT2 = [Buf() for _ in range(2)]
        ALRB = view([16, TB], BF16); B_ALRB = Buf()
        ECS = view([128, 4, TB]); B_ECS = [Buf() for _ in range(4)]
        CS = view([128, 4, TB]); B_CS = [Buf() for _ in range(4)]
        EINV = [view([128, TB]) for _ in range(2)]; B_EINV = [Buf() for _ in range(2)]
        EBL = [view([128, 4, NCH]) for _ in range(2)]; B_EBL = [[Buf() for _ in range(4)] for _ in range(2)]
        QD = [view([128, 4, TB], BF16) for _ in range(2)]; B_QD = [[Buf() for _ in range(4)] for _ in range(2)]
        KI = [view([128, 4, TB], BF16) for _ in range(2)]; B_KI = [[Buf() for _ in range(4)] for _ in range(2)]
        KT = [view([128, NCH, 512], BF16) for _ in range(2)]; B_KT = [[Buf() for _ in range(NCH)] for _ in range(2)]
        VT = [view([128, NCH, 1024], BF16) for _ in range(2)]; B_VT = [[Buf() for _ in range(NCH)] for _ in range(2)]
        SC = [view([128, NCH, 512], BF16) for _ in range(2)]; B_SC = [[Buf() for _ in range(NCH)] for _ in range(2)]
        S = view([128, 4, 256]); B_S = [Buf() for _ in range(4)]
        SBF = view([128, 4, 256], BF16); B_SBF = [Buf() for _ in range(4)]
        T1 = [view([128, 256]) for _ in range(2)]; B_T1 = [Buf() for _ in range(2)]
        SQ = view([128, 2, 512], BF16); B_SQ = [Buf() for _ in range(2)]
        RSTD = [view([128, 512]) for _ in range(2)]; B_RSTD = [Buf() for _ in range(2)]
        EPS6 = view([128, 8]); B_EPS6 = Buf()
        ms("dve", EPS6, 1e-6, [B_EPS6])
        ON = view([128, 8, TB]); B_ON = [Buf() for _ in range(8)]
        SG = view([128, 8, TB], BF16); B_SG = [Buf() for _ in range(8)]
        OFIN = view([128, 8, TB], BF16); B_OFIN = [Buf() for _ in range(8)]
        AGI = view([128, 8, TB], BF16); B_AGI = Buf()
        SGB = view([128, 8, TB], BF16); B_SGB = [Buf() for _ in range(8)]
        TBB = [view([128, TB]) for _ in range(2)]; B_TBB = [Buf() for _ in range(2)]
        B_MG = Buf()
        pso_n = [0]; psg_n = [0]

        def ps_o():
            i = pso_n[0] % 4
            pso_n[0] += 1
            return PSB[i], PSBUF[i]

        def ps_g():
            i = 4 + psg_n[0] % 4
            psg_n[0] += 1
            return PSB[i], PSBUF[i]

        for (o0, c0, n) in ((o_q, C_Q, 1024), (o_v, C_V, 1024), (o_g, C_G, 1024), (o_gb, C_GB, 1024)):
            dma("pool", W2[:, :, o0:o0 + n], wview(w_in, c0, c0 + n), (), [B_W2])
        dma("pool", W2[:, :, o_alr:o_alr + 16], wview(w_in, C_ALR, C_ALR + 16), (), [B_W2])
        dma("pool", WGLA, wview(wgla, 0, 1024), (), [B_WGLA])
        dma("pool", WA2, wa2, (), [B_WA2])
        dma("pool", BVB, rp[:, R_BV:R_BV + 1024], (), [B_BVB])

        def p2_ab(j):
            par = j % 2
            t0 = j * TB
            xt = XT2[par]; bxt = B_XT2[par]
            dma("pool", xt, xT[:, t0:t0 + TB].rearrange("(kt p) n -> p kt n", p=128), (), [bxt])
            ps, bps = ps_g()
            for kt in range(8):
                mm(ps[0:16, 0:TB], W2[:, kt, o_alr:o_alr + 16], xt[:, kt, :], kt == 0, kt == 7, [B_W2, bxt], [bps])
            act(ALRB, ps[0:16, 0:TB], AF.Identity, [bps] + PR, [B_ALRB], bias=PP[0:16, P_BALR:P_BALR + 1])
            for hd in range(4):
                psz, bpsz = ps_g()
                mm(psz[:, 0:TB], WA2[:, hd * 128:(hd + 1) * 128], ALRB, True, True, [B_WA2, B_ALRB], [bpsz])
                act(ECS[:, hd, :], psz[:, 0:TB], AF.Exp, [bpsz] + PR, [B_ECS[hd]], bias=DER[:, 16 + hd:17 + hd], scale=-1.0)
            for hd in range(4):
                act(ECS[:, hd, :], ECS[:, hd, :], AF.Ln, [B_ECS[hd]], [B_ECS[hd]], bias=1.0)
            for hd in range(4):
                P.op("dve", lambda e, o=CS[:, hd, :], d0=MCF[:, 0:TB], d1=ECS[:, hd, :]:
                     e.tensor_tensor_scan(out=o, data0=d0, data1=d1, initial=0.0, op0=ALU.mult, op1=ALU.add),
                     [B_ECS[hd], B_CONST], [B_CS[hd]])
            for hd in range(4):
                psq, bpsq = ps_g()
                for kt in range(8):
                    mm(psq[:, 0:TB], W2[:, kt, o_q + hd * 128:o_q + (hd + 1) * 128], xt[:, kt, :], kt == 0, kt == 7,
                       [B_W2, bxt], [bpsq])
                psk, bpsk = ps_g()
                for kt in range(8):
                    mm(psk[:, 0:TB], W2[:, kt, o_k + hd * 128:o_k + (hd + 1) * 128], xt[:, kt, :], kt == 0, kt == 7,
                       [B_W2, bxt], [bpsk])
                eb = ECS[:, hd, :]; beb = B_ECS[hd]
                einv = EINV[hd % 2]; beinv = B_EINV[hd % 2]
                act(eb, CS[:, hd, :], AF.Exp, [B_CS[hd]], [beb], scale=-1.0 / 16.0, bias=float(-0.5 * np.log(128.0)))
                act(einv, CS[:, hd, :], AF.Exp, [B_CS[hd]], [beinv], scale=1.0 / 16.0)
                act(EBL[par][:, hd, :], CS[:, hd, :].rearrange("p (c t) -> p c t", t=128)[:, :, 127], AF.Exp, [B_CS[hd]],
                    [B_EBL[par][hd]], scale=-1.0 / 16.0)
                stt("dve", QD[par][:, hd, :], psq[:, 0:TB], pcol(P_BQ + hd), eb, ALU.add, ALU.mult,
                    [bpsq, beb] + PR, [B_QD[par][hd]])
                stt("dve", KI[par][:, hd, :], psk[:, 0:TB], pcol(P_BK + hd), einv, ALU.add, ALU.mult,
                    [bpsk, beinv] + PR, [B_KI[par][hd]])
            for c in range(NCH):
                pst, bpst = ps_g()
                pstb = pst[:, :].bitcast(BF16)
                for hd in range(4):
                    tp(pstb[:, hd * 128:(hd + 1) * 128], KI[par][:, hd, c * 128:(c + 1) * 128], IDB,
                       [B_KI[par][hd], B_CONST], [bpst])
                act(KT[par][:, c, :], pstb[:, 0:512], AF.Copy, [bpst], [B_KT[par][c]])
                for half in range(2):
                    psv, bpsv = ps_g()
                    for kt in range(8):
                        mm(psv[:, :], xt[:, kt, c * 128:(c + 1) * 128],
                           W2[:, kt, o_v + half * 512:o_v + (half + 1) * 512], kt == 0, False, [B_W2, bxt], [bpsv])
                    mm(psv[:, :], ONE1[0:1, :], BVB[0:1, half * 512:(half + 1) * 512], False, True,
                       [B_CONST, B_BVB], [bpsv])
                    act(VT[par][:, c, half * 512:(half + 1) * 512], psv[:, :], AF.Copy, [bpsv], [B_VT[par][c]])
                pss, bpss = ps_g()
                for hd in range(4):
                    mm(pss[:, hd * 128:(hd + 1) * 128], KI[par][:, hd, c * 128:(c + 1) * 128],
                       QD[par][:, hd, c * 128:(c + 1) * 128], True, True, [B_KI[par][hd], B_QD[par][hd]], [bpss])
                tt("dve", SC[par][:, c, :], pss[:, :], CMB, ALU.mult, [bpss, B_CONST], [B_SC[par][c]])

        n_t1 = [0]

        def p2_rms(j, c, pso):
            for hh in range(2):
                po, bpo = pso[hh]
                act(SQ[:, hh, :], po[:, :], AF.Square, [bpo], [B_SQ[hh]])
            pn, bpn = ps_g()
            for hd in range(4):
                for vh in range(2):
                    col = ((hd % 2) * 2 + vh) * 128
                    mm(pn[:, hd * 128:(hd + 1) * 128], ONES256, SQ[:, hd // 2, col:col + 128], vh == 0, vh == 1,
                       [B_CONST, B_SQ[hd // 2]], [bpn])
            rstd = RSTD[c % 2]; brstd = B_RSTD[c % 2]
            act(rstd, pn[:, :], AF.Ln, [bpn, B_EPS6], [brstd], bias=EPS6[:, 0:1])
            return rstd, brstd

        def p2_rms2(j, c, pso, rstd, brstd):
            act(rstd, rstd, AF.Exp, [brstd], [brstd], scale=-0.5)
            for hd in range(4):
                po, bpo = pso[hd // 2]
                for vh in range(2):
                    col = ((hd % 2) * 2 + vh) * 128
                    vt = hd * 2 + vh
                    tt("dve", ON[:, vt, c * 128:(c + 1) * 128], po[:, col:col + 128],
                       rstd[:, hd * 128:(hd + 1) * 128], ALU.mult, [bpo, brstd], [B_ON[vt]])

        def p2_c(j):
            par = j % 2
            t0 = j * TB
            first = (t0 % SEQ == 0)
            xt = XT2[par]; bxt = B_XT2[par]
            if first:
                for hd in range(4):
                    ms("dve", S[:, hd, :], 0.0, [B_S[hd]])
                    ms("dve", SBF[:, hd, :], 0.0, [B_SBF[hd]])
            pend = []
            for c in range(NCH):
                pso = [ps_o(), ps_o()]
                for hd in range(4):
                    po, bpo = pso[hd // 2]
                    for vh in range(2):
                        col = ((hd % 2) * 2 + vh) * 128
                        mm(po[:, col:col + 128], VT[par][:, c, hd * 256 + vh * 128:hd * 256 + (vh + 1) * 128],
                           SC[par][:, c, hd * 128:(hd + 1) * 128], True, False, [B_VT[par][c], B_SC[par][c]], [bpo])
                        mm(po[:, col:col + 128], SBF[:, hd, vh * 128:(vh + 1) * 128],
                           QD[par][:, hd, c * 128:(c + 1) * 128], False, True, [B_SBF[hd], B_QD[par][hd]], [bpo])
                    pkv, bpkv = ps_g()
                    mm(pkv[:, 0:256], KT[par][:, c, hd * 128:(hd + 1) * 128], VT[par][:, c, hd * 256:(hd + 1) * 256],
                       True, True, [B_KT[par][c], B_VT[par][c]], [bpkv])
                    t1 = T1[n_t1[0] % 2]; bt1 = B_T1[n_t1[0] % 2]
                    n_t1[0] += 1
                    act(t1, S[:, hd, :], AF.Identity, [B_S[hd], B_EBL[par][hd]], [bt1], scale=EBL[par][:, hd, c:c + 1])
                    stt("dve", S[:, hd, :], pkv[:, 0:256], EBL[par][:, hd, c:c + 1], t1, ALU.mult, ALU.add,
                        [bpkv, B_EBL[par][hd], bt1], [B_S[hd]])
                    act(SBF[:, hd, :], S[:, hd, :], AF.Copy, [B_S[hd]], [B_SBF[hd]])
                    vt = c * 4 + hd
                    if vt < 8:
                        ps, bps = ps_g()
                        for kt in range(8):
                            mm(ps[:, 0:TB], W2[:, kt, o_g + vt * 128:o_g + (vt + 1) * 128], xt[:, kt, :], kt == 0,
                               kt == 7, [B_W2, bxt], [bps])
                        act(SG[:, vt, :], ps[:, 0:TB], AF.Silu, [bps] + PR, [B_SG[vt]], bias=pcol(P_BG + vt))
                pend.append((c, pso))
            rs = [p2_rms(j, c_, pso_) for (c_, pso_) in pend]
            p2_e1(j)
            for (c_, pso_), (rstd, brstd) in zip(pend, rs):
                p2_rms2(j, c_, pso_, rstd, brstd)
            for vt in range(8):
                stt("dve", OFIN[:, vt, :], ON[:, vt, :], pcol(P_NG + vt), SG[:, vt, :], ALU.mult, ALU.mult,
                    [B_ON[vt], B_SG[vt]] + PR, [B_OFIN[vt]])

        n_mg = [0]

        def p2_e1(j):
            par = j % 2
            xt = XT2[par]; bxt = B_XT2[par]
            for dt in range(8):
                ps, bps = ps_g()
                for kt in range(8):
                    mm(ps[:, 0:TB], W2[:, kt, o_gb + dt * 128:o_gb + (dt + 1) * 128], xt[:, kt, :], kt == 0, kt == 7,
                       [B_W2, bxt], [bps])
                act(SGB[:, dt, :], ps[:, 0:TB], AF.Sigmoid, [bps] + PR, [B_SGB[dt]], bias=pcol(P_BGB + dt))

        def p2_e(j):
            par = j % 2
            t0 = j * TB
            dma("sp", AGI, AG[:, t0:t0 + TB].rearrange("(dt p) n -> p dt n", p=128), [B_AG], [B_AGI])
            for dt in range(8):
                sgb = SGB[:, dt, :]; bsgb = B_SGB[dt]
                tbb = TBB[n_mg[0] % 2]; btbb = B_TBB[n_mg[0] % 2]
                n_mg[0] += 1
                ps2, bps2 = ps_g()
                for vt in range(8):
                    mm(ps2[:, 0:TB], WGLA[:, vt, dt * 128:(dt + 1) * 128], OFIN[:, vt, :], vt == 0, vt == 7,
                       [B_WGLA, B_OFIN[vt]], [bps2])
                tt("dve", tbb, ps2[:, 0:TB], sgb, ALU.mult, [bps2, bsgb], [btbb])
                tt("dve", AGI[:, dt, :], tbb, AGI[:, dt, :], ALU.add, [btbb, B_AGI], [B_AGI])
            dma("sp", MG[:, t0:t0 + TB].rearrange("(dt p) n -> p dt n", p=128), AGI, [B_AGI], [B_MG])

        NB2 = NTOK // TB
        p2_ab(0)
        for j in range(NB2):
            if j + 1 < NB2:
                p2_ab(j + 1)
            p2_c(j)
            p2_e(j)
        P.barrier()

        off[0] = g_mark
        WO = view([128, 8, 1024], BF16); B_WO = Buf()
        WRS = view([128, 8, 36]); B_WRS = Buf()
        RPB = view([128, 3 * 1024]); B_RPB = Buf()
        RBB = view([128, 36]); B_RBB = Buf()
        MGI = [view([128, 8, 512], BF16) for _ in range(2)]; B_MGI = [Buf() for _ in range(2)]
        NZ = 4
        XTM = [view([128, 1024]) for _ in range(NZ)]; B_XTM = [Buf() for _ in range(NZ)]
        Z = [view([128, 1024]) for _ in range(NZ)]; B_Z = [Buf() for _ in range(NZ)]
        X1B = [view([128, 1024], BF16) for _ in range(8)]; B_X1B = [Buf() for _ in range(8)]
        X1T = [view([128, 8, 128]) for _ in range(2)]; B_X1T = [Buf() for _ in range(2)]
        STATS = [view([128, 2, 6]) for _ in range(2)]; B_STATS = [Buf() for _ in range(2)]
        MV4 = [view([128, 4]) for _ in range(NZ)]; B_MV4 = [Buf() for _ in range(NZ)]
        LG4 = [view([128, 4, 36]) for _ in range(2)]; B_LG4 = [Buf() for _ in range(2)]
        RT = view([128, 1400]); B_RT = Buf()
        CBF = view([128, 4, 32], BF16); B_CBF = Buf()
        B_X1S = Buf(); B_XS = Buf()
        dma("pool", WO, wview(wo, 0, 1024), (), [B_WO])
        BOB = view([1, 1024], BF16); B_BOB = Buf()
        dma("pool", BOB, rp[:, R_BO:R_BO + 1024], (), [B_BOB])
        dma("sp", WRS, wr.rearrange("(kt p) n -> p kt n", p=128), (), [B_WRS])
        dma("sp", RPB, rp[:, R_BO:R_BO + 3072].partition_broadcast(128), (), [B_RPB])
        dma("sp", RBB, rp[:, R_RB:R_RB + 36].partition_broadcast(128), (), [B_RBB])
        _ro = [0]

        def rt(n, shape=None):
            v = RT[:, _ro[0]:_ro[0] + n]
            _ro[0] += n
            return v

        GMAX = rt(4); OHG = rt(16); DG = rt(16); SUMG = rt(4); PG = rt(4)
        T44 = rt(128); ESEL = rt(32); M1 = rt(4); OH1 = rt(32); E2 = rt(32); M2 = rt(4); OH2 = rt(32)
        DD = rt(4); W1c = rt(4); OH1F = rt(128); OH2F = rt(128); RKB = rt(128); RK = rt(128); TMPR = rt(128); PF = rt(8)
        v3 = lambda a, n: a.rearrange("p (c x) -> p c x", x=n)
        OHG3 = v3(OHG, 4); DG3 = v3(DG, 4); ESEL3 = v3(ESEL, 8); OH13 = v3(OH1, 8); E23 = v3(E2, 8); OH23 = v3(OH2, 8)
        T444 = T44.rearrange("p (c g e) -> p c g e", c=4, g=4)
        OH1F4 = OH1F.rearrange("p (c g e) -> p c g e", c=4, g=4); OH2F4 = OH2F.rearrange("p (c g e) -> p c g e", c=4, g=4)
        OH1F3 = v3(OH1F, 32); OH2F3 = v3(OH2F, 32); RKB3 = v3(RKB, 32); RK3 = v3(RK, 32); TMPR3 = v3(TMPR, 32)
        PF3 = v3(PF, 2)
        bc = lambda a, shp: a.broadcast_to(shp)
        R_ = [B_RT]

        def red(o, i, op):
            P.op("dve", lambda e: e.tensor_reduce(out=o, in_=i, axis=AX.X, op=op), R_, R_)

        def stage1(ch):
            j, cc = ch // 4, ch % 4
            t0 = ch * 128
            if cc == 0:
                dma("sp", MGI[j % 2], MG[:, j * 512:(j + 1) * 512].rearrange("(dt p) n -> p dt n", p=128), [B_MG],
                    [B_MGI[j % 2]])
            mgi = MGI[j % 2]; bmgi = B_MGI[j % 2]
            xtm = XTM[ch % NZ]; bxtm = B_XTM[ch % NZ]
            z = Z[ch % NZ]; bz = B_Z[ch % NZ]
            mv = MV4[ch % NZ]; bmv = B_MV4[ch % NZ]
            stt_ = STATS[ch % 2]; bst = B_STATS[ch % 2]
            x1b = X1B[ch % 8]; bx1b = B_X1B[ch % 8]
            dma("sp", xtm, xn[t0:t0 + 128, :], (), [bxtm])
            for half in range(2):
                ps, bps = next_ps()
                for jt in range(8):
                    mm(ps[:, :], mgi[:, jt, cc * 128:(cc + 1) * 128], WO[:, jt, half * 512:(half + 1) * 512],
                       jt == 0, False, [bmgi, B_WO], [bps])
                mm(ps[:, :], ONE1[0:1, :], BOB[0:1, half * 512:(half + 1) * 512], False, True, [B_CONST, B_BOB], [bps])
                stt("dve", z[:, half * 512:(half + 1) * 512], xtm[:, half * 512:(half + 1) * 512], ALPHA, ps[:, :],
                    ALU.mult, ALU.add, [bps, bxtm], [bz])
                P.op("dve", lambda e, o=stt_[:, half, :], i=z[:, half * 512:(half + 1) * 512]: e.bn_stats(out=o, in_=i),
                     [bz], [bst])
            P.op("dve", lambda e, o=mv[:, 0:2], i=stt_.rearrange("p a b -> p (a b)"): e.bn_aggr(out=o, in_=i),
                 [bst], [bmv])
            act(mv[:, 2:3], mv[:, 1:2], AF.Sqrt, [bmv], [bmv], bias=1e-5)
            P.op("dve", lambda e, o=mv[:, 2:3]: e.reciprocal(out=o, in_=o), [bmv], [bmv])
            stt("dve", mv[:, 3:4], mv[:, 0:1], -1.0, mv[:, 2:3], ALU.mult, ALU.mult, [bmv], [bmv])

        def stage1b(ch):
            t0 = ch * 128
            z = Z[ch % NZ]; bz = B_Z[ch % NZ]
            mv = MV4[ch % NZ]; bmv = B_MV4[ch % NZ]
            x1b = X1B[ch % 8]; bx1b = B_X1B[ch % 8]
            act(z, z, AF.Identity, [bz, bmv], [bz], scale=mv[:, 2:3], bias=mv[:, 3:4])
            tt("dve", z, z, RPB[:, 1024:2048], ALU.mult, [bz, B_RPB], [bz])
            tt("dve", z, z, RPB[:, 2048:3072], ALU.add, [bz, B_RPB], [bz])
            dma("sp", X1S[t0:t0 + 128, :], z, [bz], [B_X1S])
            act(x1b, z, AF.Copy, [bz], [bx1b])

        def stage2(ch):
            j, cc = ch // 4, ch % 4
            z = Z[ch % NZ]; bz = B_Z[ch % NZ]
            x1t = X1T[ch % 2]; bx1t = B_X1T[ch % 2]
            for half in range(2):
                ps, bps = next_ps()
                for q4 in range(4):
                    dtl = half * 4 + q4
                    tp(ps[:, q4 * 128:(q4 + 1) * 128], z[:, dtl * 128:(dtl + 1) * 128], IDF, [bz, B_CONST], [bps])
                act(x1t[:, half * 4:(half + 1) * 4, :], ps[:, :].rearrange("p (a b) -> p a b", b=128), AF.Copy,
                    [bps], [bx1t])

        def stage2b(ch):
            j, cc = ch // 4, ch % 4
            x1t = X1T[ch % 2]; bx1t = B_X1T[ch % 2]
            ps, bps = next_ps()
            for dtl in range(8):
                mm(ps[:, 0:36], x1t[:, dtl, :], WRS[:, dtl, :], dtl == 0, dtl == 7, [bx1t, B_WRS], [bps])
            tt("dve", LG4[j % 2][:, cc, :], ps[:, 0:36], RBB, ALU.add, [bps, B_RBB], [B_LG4[j % 2]])

        def stageB(j):
            LG = LG4[j % 2]; blg = B_LG4[j % 2]
            ch0 = j * 4
            LGg = LG[:, :, 0:4]
            LGe = LG[:, :, 4:36].rearrange("p c (g e) -> p c g e", e=8)
            P.op("dve", lambda e: e.tensor_reduce(out=GMAX, in_=LGg, axis=AX.X, op=ALU.max), [blg] + R_, R_)
            tt("dve", OHG3, LGg, bc(GMAX.unsqueeze(2), [128, 4, 4]), ALU.is_equal, [blg] + R_, R_)
            tt("dve", DG3, LGg, bc(GMAX.unsqueeze(2), [128, 4, 4]), ALU.subtract, [blg] + R_, R_)
            act(DG, DG, AF.Exp, R_, R_)
            red(SUMG, DG3, ALU.add)
            P.op("dve", lambda e: e.reciprocal(out=PG, in_=SUMG), R_, R_)
            tt("dve", T444, LGe, bc(OHG3.unsqueeze(3), [128, 4, 4, 8]), ALU.mult, [blg] + R_, R_)
            red(ESEL3, T444.rearrange("p c g e -> p c e g"), ALU.add)
            red(M1, ESEL3, ALU.max)
            tt("dve", OH13, ESEL3, bc(M1.unsqueeze(2), [128, 4, 8]), ALU.is_equal, R_, R_)
            stt("dve", E2, OH1, -1e30, ESEL, ALU.mult, ALU.add, R_, R_)
            red(M2, E23, ALU.max)
            tt("dve", OH23, E23, bc(M2.unsqueeze(2), [128, 4, 8]), ALU.is_equal, R_, R_)
            tt("dve", DD, M2, M1, ALU.subtract, R_, R_)
            act(DD, DD, AF.Exp, R_, R_)
            ts("dve", DD, DD, 1.0, None, ALU.add, None, R_, R_)
            P.op("dve", lambda e: e.reciprocal(out=W1c, in_=DD), R_, R_)
            tt("dve", WTS[:, ch0:ch0 + 4, 0], W1c, PG, ALU.mult, R_, [B_WTS])
            tt("dve", WTS[:, ch0:ch0 + 4, 1], PG, WTS[:, ch0:ch0 + 4, 0], ALU.subtract, R_ + [B_WTS], [B_WTS])
            tt("dve", OH1F4, bc(OHG3.unsqueeze(3), [128, 4, 4, 8]), bc(OH13.unsqueeze(2), [128, 4, 4, 8]), ALU.mult, R_, R_)
            tt("dve", OH2F4, bc(OHG3.unsqueeze(3), [128, 4, 4, 8]), bc(OH23.unsqueeze(2), [128, 4, 4, 8]), ALU.mult, R_, R_)
            tt("dve", CBF, OH1F3, OH2F3, ALU.add, R_, [B_CBF])
            ps, bps = next_ps()
            for c in range(4):
                mm(ps[:, c * 64:c * 64 + 32], LTB, CBF[:, c, :], True, True, [B_CONST, B_CBF], [bps])
                mm(ps[:, c * 64 + 32:c * 64 + 64], ALL1, CBF[:, c, :], True, True, [B_CONST, B_CBF], [bps])
            psv = ps[:, 0:256].rearrange("p (c x) -> p c x", x=64)
            cp("dve", RKB3[:, 0, :], CNTB, [B_CNT] + R_, R_)
            for c in range(1, 4):
                tt("dve", RKB3[:, c, :], RKB3[:, c - 1, :], psv[:, c - 1, 32:64], ALU.add, [bps] + R_, R_)
            tt("dve", CNTB, RKB3[:, 3, :], psv[:, 3, 32:64], ALU.add, [bps] + R_, [B_CNT])
            tt("dve", RK3, psv[:, :, 0:32], RKB3, ALU.add, [bps] + R_, R_)
            tt("dve", TMPR3, OH1F3, RK3, ALU.mult, R_, R_)
            red(PF3[:, :, 0], TMPR3, ALU.add)
            tt("dve", TMPR3, OH2F3, RK3, ALU.mult, R_, R_)
            red(PF3[:, :, 1], TMPR3, ALU.add)
            cp("dve", POS[:, ch0:ch0 + 4, :], PF3, R_, [B_POS])
            for c in range(4):
                ch = ch0 + c
                for k in range(2):
                    P.op("pool", lambda e, ix=POS[:, ch, k:k + 1], src=X1B[ch % 8]: e.indirect_dma_start(
                        out=XS, out_offset=bass.IndirectOffsetOnAxis(ap=ix, axis=0), in_=src, in_offset=None),
                        [B_POS, B_X1B[ch % 8]], [B_XS], dma=True)

        NCHK = NTOK // 128
        for step in range(NCHK + 2):
            if step - 2 >= 0:
                stage2(step - 2)
            if 0 <= step - 1 < NCHK:
                stage1b(step - 1)
            if step < NCHK:
                stage1(step)
            if step - 2 >= 0:
                stage2b(step - 2)
                if (step - 2) % 4 == 3:
                    stageB((step - 2) // 4)
        if debug:
            dma("sp", d_pos, POS.rearrange("p a b -> p (a b)"), [B_POS], [Buf()])
            dma("sp", d_wts, WTS.rearrange("p a b -> p (a b)"), [B_WTS], [Buf()])
        P.barrier()

        off[0] = g_mark
        NST = CAP // 128
        NWB = 3
        EW1 = [view([128, 8, 512], BF16) for _ in range(NWB)]
        EW3 = [view([128, 8, 512], BF16) for _ in range(NWB)]
        EW2 = [view([128, 4, 1024], BF16) for _ in range(NWB)]
        B_EW = [[Buf(), Buf(), Buf()] for _ in range(NWB)]
        XSL = [view([128, NST, 1024], BF16) for _ in range(2)]; B_XSL = [Buf() for _ in range(2)]
        XST = [view([128, 8, CAP], BF16) for _ in range(2)]; B_XST = [Buf() for _ in range(2)]
        HT = [view([128, 4, CAP], BF16) for _ in range(2)]; B_HT = [[Buf() for _ in range(4)] for _ in range(2)]
        S1 = [view([128, CAP]) for _ in range(2)]; B_S1 = [Buf() for _ in range(2)]
        YSB = [view([128, 1024]) for _ in range(3)]; B_YSB = [Buf() for _ in range(3)]
        B_YS = Buf()
        n_s1 = [0]
        n_y = [0]

        def p4_load(e):
            w = e % NWB
            dma("pool", EW1[w], ew1[e].rearrange("(kt p) n -> p kt n", p=128), (), [B_EW[w][0]])
            dma("pool", EW3[w], ew3[e].rearrange("(kt p) n -> p kt n", p=128), (), [B_EW[w][1]])
            dma("pool", EW2[w], ew2[e].rearrange("(kt p) n -> p kt n", p=128), (), [B_EW[w][2]])
            dma("sp", XSL[e % 2], XS[e * CAP:(e + 1) * CAP, :].rearrange("(s p) n -> p s n", p=128), [B_XS], [B_XSL[e % 2]])

        def p4_T(e):
            p = e % 2
            for s_ in range(NST):
                pst, bpst = next_ps()
                pstb = pst[:, :].bitcast(BF16)
                for dtl in range(8):
                    tp(pstb[:, dtl * 128:(dtl + 1) * 128], XSL[p][:, s_, dtl * 128:(dtl + 1) * 128], IDB,
                       [B_XSL[p], B_CONST], [bpst])
                act(XST[p][:, :, s_ * 128:(s_ + 1) * 128], pstb.rearrange("p (a b) -> p a b", b=128), AF.Copy,
                    [bpst], [B_XST[p]])

        def p4_H(e):
            p = e % 2; w = e % NWB
            for ft in range(4):
                ps1, bps1 = next_ps()
                for kt in range(8):
                    mm(ps1[:, 0:CAP], EW1[w][:, kt, ft * 128:(ft + 1) * 128], XST[p][:, kt, :], kt == 0, kt == 7,
                       [B_EW[w][0], B_XST[p]], [bps1])
                ps3, bps3 = next_ps()
                for kt in range(8):
                    mm(ps3[:, 0:CAP], EW3[w][:, kt, ft * 128:(ft + 1) * 128], XST[p][:, kt, :], kt == 0, kt == 7,
                       [B_EW[w][1], B_XST[p]], [bps3])
                s1 = S1[n_s1[0] % 2]; bs1 = B_S1[n_s1[0] % 2]
                n_s1[0] += 1
                act(s1, ps1[:, 0:CAP], AF.Silu, [bps1], [bs1])
                tt("dve", HT[p][:, ft, :], ps3[:, 0:CAP], s1, ALU.mult, [bps3, bs1], [B_HT[p][ft]])

        def p4_Y(e):
            p = e % 2; w = e % NWB
            for s_ in range(NST):
                ysb = YSB[n_y[0] % 3]; bysb = B_YSB[n_y[0] % 3]
                n_y[0] += 1
                for half in range(2):
                    ps, bps = next_ps()
                    for ft in range(4):
                        mm(ps[:, :], HT[p][:, ft, s_ * 128:(s_ + 1) * 128], EW2[w][:, ft, half * 512:(half + 1) * 512],
                           ft == 0, ft == 3, [B_HT[p][ft], B_EW[w][2]], [bps])
                    if half == 0:
                        act(ysb[:, 0:512], ps[:, :], AF.Copy, [bps], [bysb])
                    else:
                        cp("dve", ysb[:, 512:1024], ps[:, :], [bps], [bysb])
                r0 = e * CAP + s_ * 128
                dma("sp", YS[r0:r0 + 128, :], ysb, [bysb], [B_YS])

        p4_load(0)
        p4_load(1)
        p4_T(0)
        for e in range(NEXP):
            if e + 2 < NEXP:
                p4_load(e + 2)
            p4_H(e)
            if e + 1 < NEXP:
                p4_T(e + 1)
            p4_Y(e)
        P.barrier()

        off[0] = g_mark
        NR5 = 4
        L2 = view([128, 2048]); B_L2 = Buf()
        Y1 = [view([128, 1024]) for _ in range(NR5)]; B_Y1 = [Buf() for _ in range(NR5)]
        Y2 = [view([128, 1024]) for _ in range(NR5)]; B_Y2 = [Buf() for _ in range(NR5)]
        XA = [view([128, 1024]) for _ in range(NR5)]; B_XA = [Buf() for _ in range(NR5)]
        STATS5 = [view([128, 2, 6]) for _ in range(2)]; B_ST5 = [Buf() for _ in range(2)]
        MV5 = [view([128, 4]) for _ in range(2)]; B_MV5 = [Buf() for _ in range(2)]
        dma("sp", L2, rp[:, R_L2G:R_L2G + 2048].partition_broadcast(128), (), [B_L2])

        def p5_load(ch):
            t0 = ch * 128
            r = ch % NR5
            dma("sp", XA[r], X1S[t0:t0 + 128, :], [B_X1S], [B_XA[r]])
            P.op("pool", lambda e, ix=POS[:, ch, 0:1], o=Y1[r]: e.indirect_dma_start(
                out=o, out_offset=None, in_=YS, in_offset=bass.IndirectOffsetOnAxis(ap=ix, axis=0)),
                [B_POS, B_YS], [B_Y1[r]], dma=True)
            P.op("pool", lambda e, ix=POS[:, ch, 1:2], o=Y2[r]: e.indirect_dma_start(
                out=o, out_offset=None, in_=YS, in_offset=bass.IndirectOffsetOnAxis(ap=ix, axis=0)),
                [B_POS, B_YS], [B_Y2[r]], dma=True)

        def p5_comp(ch):
            t0 = ch * 128
            r = ch % NR5
            xa = XA[r]; bxa = B_XA[r]; y1 = Y1[r]; by1 = B_Y1[r]; y2 = Y2[r]; by2 = B_Y2[r]
            st5 = STATS5[ch % 2]; bst5 = B_ST5[ch % 2]; mv5 = MV5[ch % 2]; bmv5 = B_MV5[ch % 2]
            act(xa, xa, AF.Identity, [bxa], [bxa], scale=ALPHA)
            stt("dve", xa, y1, WTS[:, ch, 0:1], xa, ALU.mult, ALU.add, [by1, bxa, B_WTS], [bxa])
            stt("dve", xa, y2, WTS[:, ch, 1:2], xa, ALU.mult, ALU.add, [by2, bxa, B_WTS], [bxa])
            for half in range(2):
                P.op("dve", lambda e, o=st5[:, half, :], i=xa[:, half * 512:(half + 1) * 512]: e.bn_stats(out=o, in_=i),
                     [bxa], [bst5])
            P.op("dve", lambda e, o=mv5[:, 0:2], i=st5.rearrange("p a b -> p (a b)"): e.bn_aggr(out=o, in_=i),
                 [bst5], [bmv5])
            act(mv5[:, 2:3], mv5[:, 1:2], AF.Sqrt, [bmv5], [bmv5], bias=1e-5)
            P.op("dve", lambda e, o=mv5[:, 2:3]: e.reciprocal(out=o, in_=o), [bmv5], [bmv5])
            stt("dve", mv5[:, 3:4], mv5[:, 0:1], -1.0, mv5[:, 2:3], ALU.mult, ALU.mult, [bmv5], [bmv5])
            act(xa, xa, AF.Identity, [bxa, bmv5], [bxa], scale=mv5[:, 2:3], bias=mv5[:, 3:4])
            tt("dve", xa, xa, L2[:, 0:1024], ALU.mult, [bxa, B_L2], [bxa])
            tt("pool", xa, xa, L2[:, 1024:2048], ALU.add, [bxa, B_L2], [bxa])
            dma("sp", out[t0:t0 + 128, :], xa, [bxa], [Buf()])

        NCH5 = NTOK // 128
        p5_load(0)
        p5_load(1)
        for ch in range(NCH5):
            if ch + 2 < NCH5:
                p5_load(ch + 2)
            p5_comp(ch)
        P.barrier()

        with nc.Block() as block:
            P.emit(block)
    return nc


def _host_inputs(inputs):
    f = lambda k: np.ascontiguousarray(np.asarray(inputs[k], dtype=np.float32)[0])
    x = np.asarray(inputs["x"], dtype=np.float32)
    b_in = f("b_in")
    pp = np.zeros((128, 128), np.float32)

    def put(col, vec):
        n = vec.shape[0] // 128
        pp[:, col:col + n] = vec.reshape(n, 128).T

    put(P_BRX, b_in[C_RX:C_RX + 1024]); put(P_BRY, b_in[C_RY:C_RY + 1024]); put(P_BQ, b_in[C_Q:C_Q + 512])
    put(P_BK, b_in[C_K:C_K + 512]); put(P_BG, b_in[C_G:C_G + 1024])
    pp[0:16, P_BALR] = b_in[C_ALR:C_ALR + 16]
    put(P_BGA, b_in[C_GA:C_GA + 1024]); put(P_BGB, b_in[C_GB:C_GB + 1024])
    cw = f("conv_w")
    pp[:, P_CW:P_CW + 32] = cw.reshape(4, 8, 128).transpose(2, 1, 0).reshape(128, 32)
    put(P_CB, f("conv_b")); put(P_RBA, f("rg_b_a")); put(P_RBX, f("rg_b_x")); put(P_LAM, f("rg_lambda"))
    put(P_GBA, f("gla_b_a")); put(P_NG, f("gla_norm_g"))
    rp = np.zeros((1, NRP), np.float32)
    rp[0, R_BV:R_BV + 1024] = b_in[C_V:C_V + 1024]
    rp[0, R_BO:R_BO + 1024] = f("b_o"); rp[0, R_L1G:R_L1G + 1024] = f("ln1_g"); rp[0, R_L1B:R_L1B + 1024] = f("ln1_b")
    rp[0, R_L2G:R_L2G + 1024] = f("ln2_g"); rp[0, R_L2B:R_L2B + 1024] = f("ln2_b")
    rp[0, R_RB:R_RB + 4] = f("router_b_group"); rp[0, R_RB + 4:R_RB + 36] = f("router_b_expert")
    wr = np.ascontiguousarray(np.concatenate([f("router_w_group"), f("router_w_expert")], axis=1))
    rgw = np.ascontiguousarray(np.stack([f("rg_w_a"), f("rg_w_x")], axis=0))
    ii = np.arange(128)
    c_id = np.eye(128, dtype=np.float32)
    c_lt = (ii[:, None] < ii[None, :]).astype(np.float32)
    c_cm = np.tile((ii[None, :] >= ii[:, None]).astype(np.float32), (1, 4))
    c_mc = np.ones((128, 512), np.float32); c_mc[:, ::128] = 0.0
    c_eb = np.tile((np.arange(NEXP, dtype=np.float32) * CAP)[None, :], (128, 1))
    shared = {
        "w_in": f("w_in"), "pp": pp, "rp": rp, "wr": wr, "rgw": rgw, "wa2": f("gla_w_a2"),
        "wrnn": f("w_proj_rnn"), "wgla": f("w_proj_gla"), "wo": f("w_o"),
        "ew1": f("exp_w1"), "ew3": f("exp_w3"), "ew2": f("exp_w2"),
        "c_id": c_id, "c_lt": c_lt, "c_cm": c_cm, "c_mc": c_mc, "c_eb": c_eb,
    }
    in_maps = []
    for c in range(NCORES):
        xc = np.ascontiguousarray(x[2 * c:2 * c + 2].reshape(NTOK, 1024))
        m = dict(shared)
        m["xn"] = xc
        m["xT"] = np.ascontiguousarray(xc.T)
        in_maps.append(m)
    return in_maps


def kernel(**inputs):
    in_maps = _host_inputs(inputs)
    nc = build(debug=False)
    res = run_bass_kernel_spmd(nc, in_maps, core_ids=list(range(NCORES)))
    outs = [np.asarray(r["out"], dtype=np.float32).reshape(2, SEQ, 1024) for r in res.results]
    return np.concatenate(outs, axis=0)
```

```python
import numpy as np
from contextlib import ExitStack
import concourse.bass as bass
import concourse.mybir as mybir
from concourse.bass_utils import run_bass_kernel_spmd

F32 = mybir.dt.float32
BF16 = mybir.dt.bfloat16
I32 = mybir.dt.int32
AF = mybir.ActivationFunctionType
ALU = mybir.AluOpType
AX = mybir.AxisListType

SAME_ENGINE_SYNC = True
DMA_RING = 12
NCORES = 8
NTOK = 4096
SEQ = 2048
CAP = 384
NEXP = 32
ALPHA = 2.0 ** 0.25
SBUF_WORDS = 44 * 1024

C_RX, C_RY, C_Q, C_K, C_V, C_G, C_ALR, C_GA, C_GB = 0, 1024, 2048, 2560, 3072, 4096, 5120, 5136, 6160
P_BRX, P_BRY, P_BQ, P_BK, P_BG, P_BALR, P_BGA, P_BGB = 0, 8, 16, 20, 24, 32, 33, 41
P_CW, P_CB, P_RBA, P_RBX, P_LAM, P_GBA, P_NG = 49, 81, 89, 97, 105, 113, 117
R_BV, R_BO, R_L1G, R_L1B, R_L2G, R_L2B, R_RB = 0, 1024, 2048, 3072, 4096, 5120, 6144
NRP = 6180


class Buf:
    __slots__ = ("w", "r")

    def __init__(self):
        self.w = None
        self.r = {}


class _Eng:
    def __init__(self, name, sem, dma_sems):
        self.name = name
        self.sem = sem
        self.count = 0
        self.seen = {}
        self.ops = []
        self.dma_sems = dma_sems
        self.dma_n = 0


class Prog:
    def __init__(self, nc, stack):
        self.nc = nc
        self.E = {}
        for name in ("pe", "act", "dve", "pool", "sp"):
            sem = stack.enter_context(nc.semaphore("s_" + name))
            dsems = []
            if name in ("sp", "pool"):
                dsems = [stack.enter_context(nc.semaphore("d_%s%d" % (name, i))) for i in range(DMA_RING)]
            self.E[name] = _Eng(name, sem, dsems)

    def op(self, eng, fn, reads=(), writes=(), dma=False):
        E = self.E[eng]
        deps = {}

        def add(t):
            if t is None:
                return
            if deps.get(t[0], (None, 0))[1] < t[1]:
                deps[t[0]] = t

        for b in reads:
            add(b.w)
        for b in writes:
            add(b.w)
            for t in b.r.values():
                add(t)
        waits = []
        for s, v in deps.values():
            if E.seen.get(s, 0) >= v:
                continue
            if s is E.sem and (eng == "pe" or not SAME_ENGINE_SYNC):
                continue
            waits.append((s, v))
            E.seen[s] = v
        if dma:
            slot = E.dma_n % DMA_RING
            sem = E.dma_sems[slot]
            val = 16 * (E.dma_n // DMA_RING + 1)
            if val > 16 and E.seen.get(sem, 0) < val - 16:
                waits.append((sem, val - 16))
                E.seen[sem] = val - 16
            E.dma_n += 1
            tok = (sem, val)
            inc = 16
        else:
            E.count += 1
            tok = (E.sem, E.count)
            inc = 1
        E.ops.append((waits, fn, tok[0], inc))
        for b in reads:
            if b.r.get(tok[0], (None, 0))[1] < tok[1]:
                b.r[tok[0]] = tok
        for b in writes:
            b.w = tok
            b.r = {}
        return tok

    def barrier(self):
        toks = []
        for E in self.E.values():
            if E.count:
                toks.append((E.sem, E.count))
            for i, s in enumerate(E.dma_sems):
                n = (E.dma_n - 1 - i) // DMA_RING + 1 if E.dma_n > i else 0
                if n > 0:
                    toks.append((s, 16 * n))
        for E in self.E.values():
            waits = []
            for s, v in toks:
                if s is E.sem or E.seen.get(s, 0) >= v:
                    continue
                waits.append((s, v))
                E.seen[s] = v
            if waits:
                E.ops.append((waits, None, None, 0))

    def emit(self, block):
        def run(E):
            def body(eng):
                for waits, fn, sem, inc in E.ops:
                    for s, v in waits:
                        eng.wait_ge(s, v)
                    if fn is not None:
                        fn(eng).then_inc(sem, inc)
            return body

        block.tensor(run(self.E["pe"]))
        block.scalar(run(self.E["act"]))
        block.vector(run(self.E["dve"]))
        block.gpsimd(run(self.E["pool"]))
        block.sync(run(self.E["sp"]))


def build(debug=False):
    nc = bass.Bass("TRN2", target_bir_lowering=False)

    def din(name, shape, dt=F32):
        return nc.dram_tensor(name, list(shape), dt, kind="ExternalInput").ap()

    xT = din("xT", [1024, NTOK]); xn = din("xn", [NTOK, 1024]); w_in = din("w_in", [1024, 7184])
    pp = din("pp", [128, 128]); rp = din("rp", [1, NRP]); wr = din("wr", [1024, 36])
    rgw = din("rgw", [2, 8, 128, 128]); wa2 = din("wa2", [16, 512])
    wrnn = din("wrnn", [1024, 1024]); wgla = din("wgla", [1024, 1024]); wo = din("wo", [1024, 1024])
    ew1 = din("ew1", [NEXP, 1024, 512]); ew3 = din("ew3", [NEXP, 1024, 512]); ew2 = din("ew2", [NEXP, 512, 1024])
    c_id = din("c_id", [128, 128]); c_lt = din("c_lt", [128, 128]); c_cm = din("c_cm", [128, 512])
    c_mc = din("c_mc", [128, 512]); c_eb = din("c_eb", [128, 32])
    out = nc.dram_tensor("out", [NTOK, 1024], F32, kind="ExternalOutput").ap()
    sk = "ExternalOutput" if debug else "Internal"
    AG = nc.dram_tensor("AG", [1024, NTOK], BF16, kind=sk).ap()
    MG = nc.dram_tensor("MG", [1024, NTOK], BF16, kind=sk).ap()
    X1S = nc.dram_tensor("X1S", [NTOK, 1024], F32, kind=sk).ap()
    XS = nc.dram_tensor("XS", [NEXP * CAP, 1024], BF16, kind=sk).ap()
    YS = nc.dram_tensor("YS", [NEXP * CAP, 1024], F32, kind=sk).ap()
    if debug:
        d_pos = nc.dram_tensor("d_pos", [128, 64], I32, kind="ExternalOutput").ap()
        d_wts = nc.dram_tensor("d_wts", [128, 64], F32, kind="ExternalOutput").ap()

    st = ExitStack()
    with st:
        big = st.enter_context(nc.sbuf_tensor("big", [128, SBUF_WORDS], F32))
        PSB = [st.enter_context(nc.psum_tensor("ps%d" % i, [128, 512], F32)) for i in range(8)]
        PSBUF = [Buf() for _ in range(8)]
        P = Prog(nc, st)
        off = [0]
        psn = [0]
        _hw = []

        def view(shape, dt=F32):
            n = 1
            for s in shape[1:]:
                n *= s
            nw = (n * (2 if dt == BF16 else 4) + 3) // 4
            nw = (nw + 7) // 8 * 8
            assert off[0] + nw <= SBUF_WORDS, ("SBUF overflow", off[0], nw)
            v = big[:, off[0]:off[0] + nw]
            off[0] += nw
            if dt != F32:
                v = v.bitcast(dt)
            if dt == BF16 and n % 2:
                v = v[:, 0:n]
            elif dt == BF16:
                v = v[:, 0:n]
            else:
                v = v[:, 0:n]
            if len(shape) > 2:
                names = " ".join("a%d" % i for i in range(len(shape) - 1))
                kw = {"a%d" % i: shape[i + 1] for i in range(len(shape) - 1)}
                v = v.rearrange("p (%s) -> p %s" % (names, names), **kw)
            if shape[0] != 128:
                v = v[0:shape[0]]
            return v

        def next_ps():
            i = psn[0] % 8
            psn[0] += 1
            return PSB[i], PSBUF[i]

        def mm(o, lhsT, rhs, start, stop, reads, writes):
            P.op("pe", lambda e: e.matmul(o, lhsT=lhsT, rhs=rhs, start=start, stop=stop), reads, writes)

        def tp(o, in_, ident, reads, writes):
            P.op("pe", lambda e: e.transpose(o, in_, ident), reads, writes)

        def act(o, in_, func, reads, writes, bias=None, scale=None, accum=None):
            kw = {}
            if bias is not None:
                kw["bias"] = bias
            if scale is not None:
                kw["scale"] = scale
            if accum is not None:
                kw["accum_out"] = accum
            P.op("act", lambda e: e.activation(out=o, in_=in_, func=func, **kw), reads, writes)

        def ts(eng, o, in0, s1, s2, op0, op1, reads, writes):
            if op1 is None:
                P.op(eng, lambda e: e.tensor_scalar(out=o, in0=in0, scalar1=s1, scalar2=None, op0=op0), reads, writes)
            else:
                P.op(eng, lambda e: e.tensor_scalar(out=o, in0=in0, scalar1=s1, scalar2=s2, op0=op0, op1=op1), reads, writes)

        def tt(eng, o, in0, in1, op, reads, writes):
            P.op(eng, lambda e: e.tensor_tensor(out=o, in0=in0, in1=in1, op=op), reads, writes)

        def stt(eng, o, in0, sc, in1, op0, op1, reads, writes):
            P.op(eng, lambda e: e.scalar_tensor_tensor(out=o, in0=in0, scalar=sc, in1=in1, op0=op0, op1=op1), reads, writes)

        def cp(eng, o, in_, reads, writes):
            P.op(eng, lambda e: e.tensor_copy(out=o, in_=in_), reads, writes)

        def ms(eng, o, val, writes):
            P.op(eng, lambda e: e.memset(o, val), (), writes)

        def dma(eng, o, in_, reads, writes):
            P.op(eng, lambda e: e.dma_start(out=o, in_=in_), reads, writes, dma=True)

        def wview(w, c0, c1):
            return w[:, c0:c1].rearrange("(kt p) n -> p kt n", p=128)

        PP = view([128, 128]); B_PP = Buf()
        DER = view([128, 32]); B_DER = Buf()
        TMP8 = view([128, 8]); B_TMP8 = Buf()
        IDB = view([128, 128], BF16); IDF = view([128, 128]); LTB = view([128, 128], BF16)
        ALL1 = view([128, 128], BF16); ONES256 = view([128, 128], BF16); ONE1 = view([128, 128], BF16)
        CMB = view([128, 512], BF16); MCF = view([128, 512]); CNTB = view([128, 32])
        POS = view([128, 32, 2], I32); WTS = view([128, 32, 2])
        B_CONST = Buf(); B_CNT = Buf(); B_POS = Buf(); B_WTS = Buf()
        dma("sp", PP, pp, (), [B_PP])
        dma("sp", IDF, c_id, (), [B_CONST])
        dma("sp", MCF, c_mc, (), [B_CONST])
        dma("sp", CNTB, c_eb, (), [B_CNT])
        dma("pool", IDB, c_id, (), [B_CONST])
        dma("pool", LTB, c_lt, (), [B_CONST])
        dma("pool", CMB, c_cm, (), [B_CONST])
        ms("dve", ALL1, 1.0, [B_CONST])
        ms("dve", ONES256, 1.0 / 256.0, [B_CONST])
        ms("dve", ONE1, 1.0, [B_CONST])
        act(TMP8, PP[:, P_LAM:P_LAM + 8], AF.Exp, [B_PP], [B_TMP8], scale=-1.0)
        act(TMP8, TMP8, AF.Ln, [B_TMP8], [B_TMP8], bias=1.0)
        ts("dve", DER[:, 0:8], TMP8, -8.0, None, ALU.mult, None, [B_TMP8], [B_DER])
        ts("dve", DER[:, 8:16], TMP8, -16.0, None, ALU.mult, None, [B_TMP8], [B_DER])
        ts("dve", DER[:, 16:20], PP[:, P_GBA:P_GBA + 4], -1.0, None, ALU.mult, None, [B_PP], [B_DER])
        g_mark = off[0]
        PR = [B_PP, B_DER]

        def pcol(c):
            return PP[:, c:c + 1]

        W1 = view([128, 8, 3072], BF16); B_W1 = [Buf() for _ in range(3)]
        WRNN = view([128, 8, 1024], BF16); B_WRNN = Buf()
        RGW = view([128, 2, 8, 128], BF16); B_RGW = Buf()
        XT = [view([128, 8, 512], BF16) for _ in range(2)]; B_XT = [Buf() for _ in range(2)]
        NS = 2
        RX = [view([128, 2, 516]) for _ in range(NS)]; B_RX = [[Buf(), Buf()] for _ in range(NS)]
        U = [view([128, 2, 512]) for _ in range(NS)]; B_U = [[Buf(), Buf()] for _ in range(NS)]
        UBF = [view([128, 2, 512], BF16) for _ in range(NS)]; B_UBF = [[Buf(), Buf()] for _ in range(NS)]
        THR = [view([128, 2, 512]) for _ in range(NS)]; B_THR = [[Buf(), Buf()] for _ in range(NS)]
        A2 = [view([128, 2, 512]) for _ in range(NS)]; B_A2 = [[Buf(), Buf()] for _ in range(NS)]
        THI = [view([128, 2, 512]) for _ in range(NS)]; B_THI = [[Buf(), Buf()] for _ in range(NS)]
        GY = [view([128, 2, 512], BF16) for _ in range(NS)]; B_GY = [[Buf(), Buf()] for _ in range(NS)]
        HG = [view([128, 8, 512], BF16) for _ in range(2)]; B_HG = [[Buf() for _ in range(8)] for _ in range(2)]
        HALO = view([128, 8, 4]); B_HALO = [Buf() for _ in range(8)]
        HST = view([128, 8]); B_HST = [Buf() for _ in range(8)]
        SGA = [view([128, 512]) for _ in range(2)]; B_SGA = [Buf() for _ in range(2)]
        AGO = [view([128, 512], BF16) for _ in range(2)]; B_AGO = [Buf() for _ in range(2)]
        B_AG = Buf()
        DER2 = view([128, 32]); B_DER2 = Buf()
        ts("dve", DER2[:, 0:8], DER[:, 0:8], 0.5, None, ALU.mult, None, [B_DER], [B_DER2])
        ts("dve", DER2[:, 8:16], PP[:, P_RBA:P_RBA + 8], 0.5, None, ALU.mult, None, [B_PP], [B_DER2])
        ts("dve", DER2[:, 16:24], PP[:, P_RBX:P_RBX + 8], 0.5, None, ALU.mult, None, [B_PP], [B_DER2])
        ts("dve", DER2[:, 24:32], PP[:, P_BGA:P_BGA + 8], 0.5, None, ALU.mult, None, [B_PP], [B_DER2])
        PR1 = PR + [B_DER2]

        for i, c0 in enumerate((C_RX, C_RY, C_GA)):
            dma("pool", W1[:, :, i * 1024:(i + 1) * 1024], wview(w_in, c0, c0 + 1024), (), [B_W1[i]])
        dma("pool", RGW, rgw.rearrange("g h i j -> i g h j"), (), [B_RGW])
        dma("pool", WRNN, wview(wrnn, 0, 1024), (), [B_WRNN])

        def p1_A(G):
            j, g = G // 4, G % 4
            t0 = j * 512
            first = (j % 4 == 0)
            xt = XT[j % 2]; bxt = B_XT[j % 2]
            sset = G % NS
            if g == 0:
                dma("pool", xt, xT[:, t0:t0 + 512].rearrange("(kt p) n -> p kt n", p=128), (), [bxt])
            for ci in range(2):
                c = g * 2 + ci
                rx = RX[sset]; brx = B_RX[sset][ci]
                u = U[sset]; bu = B_U[sset][ci]
                ps, bps = next_ps()
                for kt in range(8):
                    mm(ps[:, :], W1[:, kt, c * 128:(c + 1) * 128], xt[:, kt, :], kt == 0, kt == 7, [B_W1[0], bxt], [bps])
                if first:
                    ms("pool", rx[:, ci, 0:3], 0.0, [brx])
                else:
                    cp("pool", rx[:, ci, 0:3], HALO[:, c, 0:3], [B_HALO[c]], [brx])
                act(rx[:, ci, 3:515], ps[:, :], AF.Identity, [bps] + PR1, [brx], bias=pcol(P_BRX + c))
                cp("pool", HALO[:, c, 0:3], rx[:, ci, 512:515], [brx], [B_HALO[c]])
                act(u[:, ci, :], rx[:, ci, 3:515], AF.Identity, [brx] + PR1, [bu], scale=pcol(P_CW + c * 4 + 3),
                    bias=pcol(P_CB + c))
                for k in (0, 1, 2):
                    stt("dve", u[:, ci, :], rx[:, ci, k:k + 512], pcol(P_CW + c * 4 + k), u[:, ci, :],
                        ALU.mult, ALU.add, [brx, bu] + PR1, [bu])
            for ci in range(2):
                c = g * 2 + ci
                ps, bps = next_ps()
                for kt in range(8):
                    mm(ps[:, :], W1[:, kt, 1024 + c * 128:1024 + (c + 1) * 128], xt[:, kt, :], kt == 0, kt == 7,
                       [B_W1[1], bxt], [bps])
                act(GY[sset][:, ci, :], ps[:, :], AF.Gelu_apprx_tanh, [bps] + PR1, [B_GY[sset][ci]], bias=pcol(P_BRY + c))
            for ci in range(2):
                cp("pool", UBF[sset][:, ci, :], U[sset][:, ci, :], [B_U[sset][ci]], [B_UBF[sset][ci]])

        def p1_B(G):
            j, g = G // 4, G % 4
            first = (j % 4 == 0)
            sset = G % NS
            u = U[sset]; thr = THR[sset]; thi = THI[sset]; a2 = A2[sset]; ubf = UBF[sset]; gy = GY[sset]
            for ci in range(2):
                c = g * 2 + ci
                ps, bps = next_ps()
                mm(ps[:, :], RGW[:, 0, c, :], ubf[:, ci, :], True, True, [B_RGW, B_UBF[sset][ci]], [bps])
                act(thr[:, ci, :], ps[:, :], AF.Tanh, [bps] + PR1, [B_THR[sset][ci]], bias=DER2[:, 8 + c:9 + c], scale=0.5)
                ps, bps = next_ps()
                mm(ps[:, :], RGW[:, 1, c, :], ubf[:, ci, :], True, True, [B_RGW, B_UBF[sset][ci]], [bps])
                act(thi[:, ci, :], ps[:, :], AF.Tanh, [bps] + PR1, [B_THI[sset][ci]], bias=DER2[:, 16 + c:17 + c], scale=0.5)
            for ci in range(2):
                c = g * 2 + ci
                act(thr[:, ci, :], thr[:, ci, :], AF.Exp, [B_THR[sset][ci]] + PR1, [B_THR[sset][ci]],
                    scale=DER2[:, c:c + 1], bias=DER2[:, c:c + 1])
                tt("pool", a2[:, ci, :], thr[:, ci, :], thr[:, ci, :], ALU.mult, [B_THR[sset][ci]], [B_A2[sset][ci]])
            for ci in range(2):
                ts("dve", a2[:, ci, :], a2[:, ci, :], 0.99999994, -1.0, ALU.min, ALU.mult, [B_A2[sset][ci]], [B_A2[sset][ci]])
                stt("dve", thi[:, ci, :], thi[:, ci, :], 1.0, u[:, ci, :], ALU.add, ALU.mult,
                    [B_THI[sset][ci], B_U[sset][ci]], [B_THI[sset][ci]])
            for ci in range(2):
                act(a2[:, ci, :], a2[:, ci, :], AF.Sqrt, [B_A2[sset][ci]], [B_A2[sset][ci]], bias=0.25, scale=0.25)
            for ci in range(2):
                c = g * 2 + ci
                tt("dve", thi[:, ci, :], thi[:, ci, :], a2[:, ci, :], ALU.mult, [B_A2[sset][ci], B_THI[sset][ci]],
                   [B_THI[sset][ci]])
                init = 0.0 if first else HST[:, c:c + 1]
                P.op("dve", lambda e, o=u[:, ci, :], d0=thr[:, ci, :], d1=thi[:, ci, :], ini=init:
                     e.tensor_tensor_scan(out=o, data0=d0, data1=d1, initial=ini, op0=ALU.mult, op1=ALU.add),
                     [B_THR[sset][ci], B_THI[sset][ci], B_HST[c], B_U[sset][ci]], [B_U[sset][ci]])
                cp("dve", HST[:, c:c + 1], u[:, ci, 511:512], [B_U[sset][ci]], [B_HST[c]])
                stt("dve", HG[j % 2][:, c, :], u[:, ci, :], 0.5, gy[:, ci, :], ALU.mult, ALU.mult,
                    [B_U[sset][ci], B_GY[sset][ci]], [B_HG[j % 2][c]])

        n_ag = [0]

        def p1_C(j):
            t0 = j * 512
            xt = XT[j % 2]; bxt = B_XT[j % 2]
            for dt in range(8):
                ps, bps = next_ps()
                for kt in range(8):
                    mm(ps[:, :], W1[:, kt, 2048 + dt * 128:2048 + (dt + 1) * 128], xt[:, kt, :], kt == 0, kt == 7,
                       [B_W1[2], bxt], [bps])
                sga = SGA[n_ag[0] % 2]; bsga = B_SGA[n_ag[0] % 2]
                ago = AGO[n_ag[0] % 2]; bago = B_AGO[n_ag[0] % 2]
                n_ag[0] += 1
                act(sga, ps[:, :], AF.Tanh, [bps] + PR1, [bsga], bias=DER2[:, 24 + dt:25 + dt], scale=0.5)
                ps2, bps2 = next_ps()
                for c in range(8):
                    mm(ps2[:, :], WRNN[:, c, dt * 128:(dt + 1) * 128], HG[j % 2][:, c, :], c == 0, c == 7,
                       [B_WRNN, B_HG[j % 2][c]], [bps2])
                stt("dve", ago, sga, 1.0, ps2[:, :], ALU.add, ALU.mult, [bps2, bsga], [bago])
                dma("sp", AG[dt * 128:(dt + 1) * 128, t0:t0 + 512], ago, [bago], [B_AG])

        NG = (NTOK // 512) * 4
        p1_A(0)
        for G in range(NG):
            if G + 1 < NG:
                p1_A(G + 1)
            p1_B(G)
            if G % 4 == 0 and G >= 4:
                p1_C(G // 4 - 1)
        p1_C(NTOK // 512 - 1)
        P.barrier()

        _hw.append(off[0])
        off[0] = g_mark
        TB = 256
        NCH = TB // 128
        NW2 = 4112
        W2 = view([128, 8, NW2], BF16); B_W2 = Buf()
        o_q, o_k, o_v, o_g, o_alr, o_gb = 0, 512, 1024, 2048, 3072, 3088
        WGLA = view([128, 8, 1024], BF16); B_WGLA = Buf()
        WA2 = view([16, 512], BF16); B_WA2 = Buf()
        BVB = view([1, 1024], BF16); B_BVB = Buf()
        XT2 = [view([128, 8, TB], BF16) for _ in range(3)]; B_XT2 = [Buf() for _ in range(3)]
        ALRB = view([16, TB], BF16); B_ALRB = Buf()
        ECS = view([128, 4, TB]); B_ECS = [Buf() for _ in range(4)]
        CS = view([128, 4, TB]); B_CS = [Buf() for _ in range(4)]
        EINV = [view([128, TB]) for _ in range(2)]; B_EINV = [Buf() for _ in range(2)]
        EBL = [view([128, 4, NCH]) for _ in range(2)]; B_EBL = [[Buf() for _ in range(4)] for _ in range(2)]
        QD = [view([128, 4, TB], BF16) for _ in range(2)]; B_QD = [[Buf() for _ in range(4)] for _ in range(2)]
        KI = [view([128, 4, TB], BF16) for _ in range(2)]; B_KI = [[Buf() for _ in range(4)] for _ in range(2)]
        KT = [view([128, NCH, 512], BF16) for _ in range(2)]; B_KT = [[Buf() for _ in range(NCH)] for _ in range(2)]
        VT = [view([128, NCH, 1024], BF16) for _ in range(2)]; B_VT = [[Buf() for _ in range(NCH)] for _ in range(2)]
        SC = [view([128, NCH, 512], BF16) for _ in range(2)]; B_SC = [[Buf() for _ in range(NCH)] for _ in range(2)]
        S = view([128, 4, 256]); B_S = [Buf() for _ in range(4)]
        SBF = view([128, 4, 256], BF16); B_SBF = [Buf() for _ in range(4)]
        T1 = [view([128, 256]) for _ in range(2)]; B_T1 = [Buf() for _ in range(2)]
        SQ = view([128, 2, 512], BF16); B_SQ = [Buf() for _ in range(2)]
        RSTD = [view([128, 512]) for _ in range(2)]; B_RSTD = [Buf() for _ in range(2)]
        EPS6 = view([128, 8]); B_EPS6 = Buf()
        ms("dve", EPS6, 1e-6, [B_EPS6])
        ON = view([128, 8, TB]); B_ON = [Buf() for _ in range(8)]
        SG = view([128, 8, TB], BF16); B_SG = [Buf() for _ in range(8)]
        OFIN = view([128, 8, TB], BF16); B_OFIN = [Buf() for _ in range(8)]
        AGI = view([128, 8, TB], BF16); B_AGI = Buf()
        SGB = view([128, 8, TB], BF16); B_SGB = [Buf() for _ in range(8)]
        TBB = [view([128, TB]) for _ in range(2)]; B_TBB = [Buf() for _ in range(2)]
        B_MG = Buf()
        pso_n = [0]; psg_n = [0]

        def ps_o():
            i = pso_n[0] % 4
            pso_n[0] += 1
            return PSB[i], PSBUF[i]

        def ps_g():
            i = 4 + psg_n[0] % 4
            psg_n[0] += 1
            return PSB[i], PSBUF[i]

        for (o0, c0, n) in ((o_q, C_Q, 1024), (o_v, C_V, 1024), (o_g, C_G, 1024), (o_gb, C_GB, 1024)):
            dma("pool", W2[:, :, o0:o0 + n], wview(w_in, c0, c0 + n), (), [B_W2])
        dma("pool", W2[:, :, o_alr:o_alr + 16], wview(w_in, C_ALR, C_ALR + 16), (), [B_W2])
        dma("pool", WGLA, wview(wgla, 0, 1024), (), [B_WGLA])
        dma("pool", WA2, wa2, (), [B_WA2])
        dma("pool", BVB, rp[:, R_BV:R_BV + 1024], (), [B_BVB])

        def p2_ab(j):
            par = j % 2
            t0 = j * TB
            xt = XT2[j % 3]; bxt = B_XT2[j % 3]
            dma("pool", xt, xT[:, t0:t0 + TB].rearrange("(kt p) n -> p kt n", p=128), (), [bxt])
            ps, bps = ps_g()
            for kt in range(8):
                mm(ps[0:16, 0:TB], W2[:, kt, o_alr:o_alr + 16], xt[:, kt, :], kt == 0, kt == 7, [B_W2, bxt], [bps])
            act(ALRB, ps[0:16, 0:TB], AF.Identity, [bps] + PR, [B_ALRB], bias=PP[0:16, P_BALR:P_BALR + 1])
            for hd in range(4):
                psz, bpsz = ps_g()
                mm(psz[:, 0:TB], WA2[:, hd * 128:(hd + 1) * 128], ALRB, True, True, [B_WA2, B_ALRB], [bpsz])
                act(ECS[:, hd, :], psz[:, 0:TB], AF.Exp, [bpsz] + PR, [B_ECS[hd]], bias=DER[:, 16 + hd:17 + hd], scale=-1.0)
            for hd in range(4):
                act(ECS[:, hd, :], ECS[:, hd, :], AF.Ln, [B_ECS[hd]], [B_ECS[hd]], bias=1.0)
            for hd in range(4):
                P.op("dve", lambda e, o=CS[:, hd, :], d0=MCF[:, 0:TB], d1=ECS[:, hd, :]:
                     e.tensor_tensor_scan(out=o, data0=d0, data1=d1, initial=0.0, op0=ALU.mult, op1=ALU.add),
                     [B_ECS[hd], B_CONST], [B_CS[hd]])
            for hd in range(4):
                psq, bpsq = ps_g()
                for kt in range(8):
                    mm(psq[:, 0:TB], W2[:, kt, o_q + hd * 128:o_q + (hd + 1) * 128], xt[:, kt, :], kt == 0, kt == 7,
                       [B_W2, bxt], [bpsq])
                psk, bpsk = ps_g()
                for kt in range(8):
                    mm(psk[:, 0:TB], W2[:, kt, o_k + hd * 128:o_k + (hd + 1) * 128], xt[:, kt, :], kt == 0, kt == 7,
                       [B_W2, bxt], [bpsk])
                eb = ECS[:, hd, :]; beb = B_ECS[hd]
                einv = EINV[hd % 2]; beinv = B_EINV[hd % 2]
                act(eb, CS[:, hd, :], AF.Exp, [B_CS[hd]], [beb], scale=-1.0 / 16.0, bias=float(-0.5 * np.log(128.0)))
                act(einv, CS[:, hd, :], AF.Exp, [B_CS[hd]], [beinv], scale=1.0 / 16.0)
                act(EBL[par][:, hd, :], CS[:, hd, :].rearrange("p (c t) -> p c t", t=128)[:, :, 127], AF.Exp, [B_CS[hd]],
                    [B_EBL[par][hd]], scale=-1.0 / 16.0)
                stt("dve", QD[par][:, hd, :], psq[:, 0:TB], pcol(P_BQ + hd), eb, ALU.add, ALU.mult,
                    [bpsq, beb] + PR, [B_QD[par][hd]])
                stt("dve", KI[par][:, hd, :], psk[:, 0:TB], pcol(P_BK + hd), einv, ALU.add, ALU.mult,
                    [bpsk, beinv] + PR, [B_KI[par][hd]])
            for c in range(NCH):
                pst, bpst = ps_g()
                pstb = pst[:, :].bitcast(BF16)
                for hd in range(4):
                    tp(pstb[:, hd * 128:(hd + 1) * 128], KI[par][:, hd, c * 128:(c + 1) * 128], IDB,
                       [B_KI[par][hd], B_CONST], [bpst])
                act(KT[par][:, c, :], pstb[:, 0:512], AF.Copy, [bpst], [B_KT[par][c]])
                for half in range(2):
                    psv, bpsv = ps_g()
                    for kt in range(8):
                        mm(psv[:, :], xt[:, kt, c * 128:(c + 1) * 128],
                           W2[:, kt, o_v + half * 512:o_v + (half + 1) * 512], kt == 0, False, [B_W2, bxt], [bpsv])
                    mm(psv[:, :], ONE1[0:1, :], BVB[0:1, half * 512:(half + 1) * 512], False, True,
                       [B_CONST, B_BVB], [bpsv])
                    act(VT[par][:, c, half * 512:(half + 1) * 512], psv[:, :], AF.Copy, [bpsv], [B_VT[par][c]])
                pss, bpss = ps_g()
                for hd in range(4):
                    mm(pss[:, hd * 128:(hd + 1) * 128], KI[par][:, hd, c * 128:(c + 1) * 128],
                       QD[par][:, hd, c * 128:(c + 1) * 128], True, True, [B_KI[par][hd], B_QD[par][hd]], [bpss])
                tt("dve", SC[par][:, c, :], pss[:, :], CMB, ALU.mult, [bpss, B_CONST], [B_SC[par][c]])

        n_t1 = [0]

        def p2_rms(j, c, pso):
            for hh in range(2):
                po, bpo = pso[hh]
                act(SQ[:, hh, :], po[:, :], AF.Square, [bpo], [B_SQ[hh]])
            pn, bpn = ps_g()
            for hd in range(4):
                for vh in range(2):
                    col = ((hd % 2) * 2 + vh) * 128
                    mm(pn[:, hd * 128:(hd + 1) * 128], ONES256, SQ[:, hd // 2, col:col + 128], vh == 0, vh == 1,
                       [B_CONST, B_SQ[hd // 2]], [bpn])
            rstd = RSTD[c % 2]; brstd = B_RSTD[c % 2]
            act(rstd, pn[:, :], AF.Ln, [bpn, B_EPS6], [brstd], bias=EPS6[:, 0:1])
            return rstd, brstd

        def p2_rms2(j, c, pso, rstd, brstd):
            for hd in range(4):
                po, bpo = pso[hd // 2]
                for vh in range(2):
                    col = ((hd % 2) * 2 + vh) * 128
                    vt = hd * 2 + vh
                    tt("dve", ON[:, vt, c * 128:(c + 1) * 128], po[:, col:col + 128],
                       rstd[:, hd * 128:(hd + 1) * 128], ALU.mult, [bpo, brstd], [B_ON[vt]])

        def p2_c(j):
            par = j % 2
            t0 = j * TB
            first = (t0 % SEQ == 0)
            xt = XT2[j % 3]; bxt = B_XT2[j % 3]
            if first:
                for hd in range(4):
                    ms("dve", S[:, hd, :], 0.0, [B_S[hd]])
                    ms("dve", SBF[:, hd, :], 0.0, [B_SBF[hd]])
            pend = []
            for c in range(NCH):
                pso = [ps_o(), ps_o()]
                for hd in range(4):
                    po, bpo = pso[hd // 2]
                    for vh in range(2):
                        col = ((hd % 2) * 2 + vh) * 128
                        mm(po[:, col:col + 128], VT[par][:, c, hd * 256 + vh * 128:hd * 256 + (vh + 1) * 128],
                           SC[par][:, c, hd * 128:(hd + 1) * 128], True, False, [B_VT[par][c], B_SC[par][c]], [bpo])
                        mm(po[:, col:col + 128], SBF[:, hd, vh * 128:(vh + 1) * 128],
                           QD[par][:, hd, c * 128:(c + 1) * 128], False, True, [B_SBF[hd], B_QD[par][hd]], [bpo])
                    pkv, bpkv = ps_g()
                    mm(pkv[:, 0:256], KT[par][:, c, hd * 128:(hd + 1) * 128], VT[par][:, c, hd * 256:(hd + 1) * 256],
                       True, True, [B_KT[par][c], B_VT[par][c]], [bpkv])
                    t1 = T1[n_t1[0] % 2]; bt1 = B_T1[n_t1[0] % 2]
                    n_t1[0] += 1
                    act(t1, S[:, hd, :], AF.Identity, [B_S[hd], B_EBL[par][hd]], [bt1], scale=EBL[par][:, hd, c:c + 1])
                    stt("dve", S[:, hd, :], pkv[:, 0:256], EBL[par][:, hd, c:c + 1], t1, ALU.mult, ALU.add,
                        [bpkv, B_EBL[par][hd], bt1], [B_S[hd]])
                    act(SBF[:, hd, :], S[:, hd, :], AF.Copy, [B_S[hd]], [B_SBF[hd]])
                    vt = c * 4 + hd
                    if vt < 8:
                        ps, bps = ps_g()
                        for kt in range(8):
                            mm(ps[:, 0:TB], W2[:, kt, o_g + vt * 128:o_g + (vt + 1) * 128], xt[:, kt, :], kt == 0,
                               kt == 7, [B_W2, bxt], [bps])
                        act(SG[:, vt, :], ps[:, 0:TB], AF.Silu, [bps] + PR, [B_SG[vt]], bias=pcol(P_BG + vt))
                pend.append((c, pso))
            rs = [p2_rms(j, c_, pso_) for (c_, pso_) in pend]
            for (rstd, brstd) in rs:
                act(rstd, rstd, AF.Exp, [brstd], [brstd], scale=-0.5)
            p2_e1(j)
            for (c_, pso_), (rstd, brstd) in zip(pend, rs):
                p2_rms2(j, c_, pso_, rstd, brstd)
            for vt in range(8):
                stt("dve", OFIN[:, vt, :], ON[:, vt, :], pcol(P_NG + vt), SG[:, vt, :], ALU.mult, ALU.mult,
                    [B_ON[vt], B_SG[vt]] + PR, [B_OFIN[vt]])

        n_mg = [0]

        def p2_e1(j):
            par = j % 2
            xt = XT2[j % 3]; bxt = B_XT2[j % 3]
            for dt in range(8):
                ps, bps = ps_g()
                for kt in range(8):
                    mm(ps[:, 0:TB], W2[:, kt, o_gb + dt * 128:o_gb + (dt + 1) * 128], xt[:, kt, :], kt == 0, kt == 7,
                       [B_W2, bxt], [bps])
                act(SGB[:, dt, :], ps[:, 0:TB], AF.Sigmoid, [bps] + PR, [B_SGB[dt]], bias=pcol(P_BGB + dt))

        def p2_e(j):
            par = j % 2
            t0 = j * TB
            dma("sp", AGI, AG[:, t0:t0 + TB].rearrange("(dt p) n -> p dt n", p=128), [B_AG], [B_AGI])
            for dt in range(8):
                sgb = SGB[:, dt, :]; bsgb = B_SGB[dt]
                tbb = TBB[n_mg[0] % 2]; btbb = B_TBB[n_mg[0] % 2]
                n_mg[0] += 1
                ps2, bps2 = ps_g()
                for vt in range(8):
                    mm(ps2[:, 0:TB], WGLA[:, vt, dt * 128:(dt + 1) * 128], OFIN[:, vt, :], vt == 0, vt == 7,
                       [B_WGLA, B_OFIN[vt]], [bps2])
                tt("dve", tbb, ps2[:, 0:TB], sgb, ALU.mult, [bps2, bsgb], [btbb])
                tt("dve", AGI[:, dt, :], tbb, AGI[:, dt, :], ALU.add, [btbb, B_AGI], [B_AGI])
            dma("sp", MG[:, t0:t0 + TB].rearrange("(dt p) n -> p dt n", p=128), AGI, [B_AGI], [B_MG])

        NB2 = NTOK // TB
        p2_ab(0)
        for j in range(NB2):
            if j + 1 < NB2:
                p2_ab(j + 1)
            p2_c(j)
            p2_e(j)
        P.barrier()

        _hw.append(off[0])
        off[0] = g_mark
        WO = view([128, 8, 1024], BF16); B_WO = Buf()
        WRS = view([128, 8, 36]); B_WRS = Buf()
        RPB = view([128, 3 * 1024]); B_RPB = Buf()
        RBB = view([128, 36]); B_RBB = Buf()
        MGI = [view([128, 8, 512], BF16) for _ in range(2)]; B_MGI = [Buf() for _ in range(2)]
        NZ = 4
        XTM = [view([128, 1024]) for _ in range(NZ)]; B_XTM = [Buf() for _ in range(NZ)]
        Z = [view([128, 1024]) for _ in range(NZ)]; B_Z = [Buf() for _ in range(NZ)]
        X1B = [view([128, 1024], BF16) for _ in range(8)]; B_X1B = [Buf() for _ in range(8)]
        X1T = [view([128, 8, 128]) for _ in range(2)]; B_X1T = [Buf() for _ in range(2)]
        STATS = [view([128, 2, 6]) for _ in range(2)]; B_STATS = [Buf() for _ in range(2)]
        MV4 = [view([128, 4]) for _ in range(NZ)]; B_MV4 = [Buf() for _ in range(NZ)]
        LG4 = [view([128, 4, 36]) for _ in range(2)]; B_LG4 = [Buf() for _ in range(2)]
        RT = view([128, 1400]); B_RT = Buf()
        CBF = view([128, 4, 32], BF16); B_CBF = Buf()
        B_X1S = Buf(); B_XS = Buf()
        dma("pool", WO, wview(wo, 0, 1024), (), [B_WO])
        BOB = view([1, 1024], BF16); B_BOB = Buf()
        dma("pool", BOB, rp[:, R_BO:R_BO + 1024], (), [B_BOB])
        dma("sp", WRS, wr.rearrange("(kt p) n -> p kt n", p=128), (), [B_WRS])
        dma("sp", RPB, rp[:, R_BO:R_BO + 3072].partition_broadcast(128), (), [B_RPB])
        dma("sp", RBB, rp[:, R_RB:R_RB + 36].partition_broadcast(128), (), [B_RBB])
        _ro = [0]

        def rt(n, shape=None):
            v = RT[:, _ro[0]:_ro[0] + n]
            _ro[0] += n
            return v

        GMAX = rt(4); OHG = rt(16); DG = rt(16); SUMG = rt(4); PG = rt(4)
        T44 = rt(128); ESEL = rt(32); M1 = rt(4); OH1 = rt(32); E2 = rt(32); M2 = rt(4); OH2 = rt(32)
        DD = rt(4); W1c = rt(4); OH1F = rt(128); OH2F = rt(128); RKB = rt(128); RK = rt(128); TMPR = rt(128); PF = rt(8)
        v3 = lambda a, n: a.rearrange("p (c x) -> p c x", x=n)
        OHG3 = v3(OHG, 4); DG3 = v3(DG, 4); ESEL3 = v3(ESEL, 8); OH13 = v3(OH1, 8); E23 = v3(E2, 8); OH23 = v3(OH2, 8)
        T444 = T44.rearrange("p (c g e) -> p c g e", c=4, g=4)
        OH1F4 = OH1F.rearrange("p (c g e) -> p c g e", c=4, g=4); OH2F4 = OH2F.rearrange("p (c g e) -> p c g e", c=4, g=4)
        OH1F3 = v3(OH1F, 32); OH2F3 = v3(OH2F, 32); RKB3 = v3(RKB, 32); RK3 = v3(RK, 32); TMPR3 = v3(TMPR, 32)
        PF3 = v3(PF, 2)
        bc = lambda a, shp: a.broadcast_to(shp)
        R_ = [B_RT]

        def red(o, i, op):
            P.op("dve", lambda e: e.tensor_reduce(out=o, in_=i, axis=AX.X, op=op), R_, R_)

        def ld_mgi(j):
            dma("sp", MGI[j % 2], MG[:, j * 512:(j + 1) * 512].rearrange("(dt p) n -> p dt n", p=128), [B_MG],
                [B_MGI[j % 2]])

        def ld_x(ch):
            dma("sp", XTM[ch % NZ], xn[ch * 128:(ch + 1) * 128, :], (), [B_XTM[ch % NZ]])

        def stage1(ch):
            j, cc = ch // 4, ch % 4
            t0 = ch * 128
            if cc == 0 and j >= 1 and j + 1 < NTOK // 512:
                ld_mgi(j + 1)
            if ch + 3 < NTOK // 128:
                ld_x(ch + 3)
            mgi = MGI[j % 2]; bmgi = B_MGI[j % 2]
            xtm = XTM[ch % NZ]; bxtm = B_XTM[ch % NZ]
            z = Z[ch % NZ]; bz = B_Z[ch % NZ]
            mv = MV4[ch % NZ]; bmv = B_MV4[ch % NZ]
            stt_ = STATS[ch % 2]; bst = B_STATS[ch % 2]
            x1b = X1B[ch % 8]; bx1b = B_X1B[ch % 8]
            for half in range(2):
                ps, bps = next_ps()
                for jt in range(8):
                    mm(ps[:, :], mgi[:, jt, cc * 128:(cc + 1) * 128], WO[:, jt, half * 512:(half + 1) * 512],
                       jt == 0, False, [bmgi, B_WO], [bps])
                mm(ps[:, :], ONE1[0:1, :], BOB[0:1, half * 512:(half + 1) * 512], False, True, [B_CONST, B_BOB], [bps])
                stt("dve", z[:, half * 512:(half + 1) * 512], xtm[:, half * 512:(half + 1) * 512], ALPHA, ps[:, :],
                    ALU.mult, ALU.add, [bps, bxtm], [bz])
                P.op("dve", lambda e, o=stt_[:, half, :], i=z[:, half * 512:(half + 1) * 512]: e.bn_stats(out=o, in_=i),
                     [bz], [bst])
            P.op("dve", lambda e, o=mv[:, 0:2], i=stt_.rearrange("p a b -> p (a b)"): e.bn_aggr(out=o, in_=i),
                 [bst], [bmv])
            act(mv[:, 2:3], mv[:, 1:2], AF.Sqrt, [bmv], [bmv], bias=1e-5)
            P.op("dve", lambda e, o=mv[:, 2:3]: e.reciprocal(out=o, in_=o), [bmv], [bmv])
            stt("dve", mv[:, 3:4], mv[:, 0:1], -1.0, mv[:, 2:3], ALU.mult, ALU.mult, [bmv], [bmv])

        def stage1b(ch):
            t0 = ch * 128
            z = Z[ch % NZ]; bz = B_Z[ch % NZ]
            mv = MV4[ch % NZ]; bmv = B_MV4[ch % NZ]
            x1b = X1B[ch % 8]; bx1b = B_X1B[ch % 8]
            act(z, z, AF.Identity, [bz, bmv], [bz], scale=mv[:, 2:3], bias=mv[:, 3:4])
            tt("dve", z, z, RPB[:, 1024:2048], ALU.mult, [bz, B_RPB], [bz])
            tt("dve", z, z, RPB[:, 2048:3072], ALU.add, [bz, B_RPB], [bz])
            dma("sp", X1S[t0:t0 + 128, :], z, [bz], [B_X1S])
            act(x1b, z, AF.Copy, [bz], [bx1b])

        def stage2(ch):
            j, cc = ch // 4, ch % 4
            z = Z[ch % NZ]; bz = B_Z[ch % NZ]
            x1t = X1T[ch % 2]; bx1t = B_X1T[ch % 2]
            for half in range(2):
                ps, bps = next_ps()
                for q4 in range(4):
                    dtl = half * 4 + q4
                    tp(ps[:, q4 * 128:(q4 + 1) * 128], z[:, dtl * 128:(dtl + 1) * 128], IDF, [bz, B_CONST], [bps])
                act(x1t[:, half * 4:(half + 1) * 4, :], ps[:, :].rearrange("p (a b) -> p a b", b=128), AF.Copy,
                    [bps], [bx1t])

        def stage2b(ch):
            j, cc = ch // 4, ch % 4
            x1t = X1T[ch % 2]; bx1t = B_X1T[ch % 2]
            ps, bps = next_ps()
            for dtl in range(8):
                mm(ps[:, 0:36], x1t[:, dtl, :], WRS[:, dtl, :], dtl == 0, dtl == 7, [bx1t, B_WRS], [bps])
            tt("dve", LG4[j % 2][:, cc, :], ps[:, 0:36], RBB, ALU.add, [bps, B_RBB], [B_LG4[j % 2]])

        def stageB(j):
            LG = LG4[j % 2]; blg = B_LG4[j % 2]
            ch0 = j * 4
            LGg = LG[:, :, 0:4]
            LGe = LG[:, :, 4:36].rearrange("p c (g e) -> p c g e", e=8)
            P.op("dve", lambda e: e.tensor_reduce(out=GMAX, in_=LGg, axis=AX.X, op=ALU.max), [blg] + R_, R_)
            tt("dve", OHG3, LGg, bc(GMAX.unsqueeze(2), [128, 4, 4]), ALU.is_equal, [blg] + R_, R_)
            tt("dve", DG3, LGg, bc(GMAX.unsqueeze(2), [128, 4, 4]), ALU.subtract, [blg] + R_, R_)
            act(DG, DG, AF.Exp, R_, R_)
            red(SUMG, DG3, ALU.add)
            P.op("dve", lambda e: e.reciprocal(out=PG, in_=SUMG), R_, R_)
            tt("dve", T444, LGe, bc(OHG3.unsqueeze(3), [128, 4, 4, 8]), ALU.mult, [blg] + R_, R_)
            red(ESEL3, T444.rearrange("p c g e -> p c e g"), ALU.add)
            red(M1, ESEL3, ALU.max)
            tt("dve", OH13, ESEL3, bc(M1.unsqueeze(2), [128, 4, 8]), ALU.is_equal, R_, R_)
            stt("dve", E2, OH1, -1e30, ESEL, ALU.mult, ALU.add, R_, R_)
            red(M2, E23, ALU.max)
            tt("dve", OH23, E23, bc(M2.unsqueeze(2), [128, 4, 8]), ALU.is_equal, R_, R_)
            tt("dve", DD, M2, M1, ALU.subtract, R_, R_)
            act(DD, DD, AF.Exp, R_, R_)
            ts("dve", DD, DD, 1.0, None, ALU.add, None, R_, R_)
            P.op("dve", lambda e: e.reciprocal(out=W1c, in_=DD), R_, R_)
            tt("dve", WTS[:, ch0:ch0 + 4, 0], W1c, PG, ALU.mult, R_, [B_WTS])
            tt("dve", WTS[:, ch0:ch0 + 4, 1], PG, WTS[:, ch0:ch0 + 4, 0], ALU.subtract, R_ + [B_WTS], [B_WTS])
            tt("dve", OH1F4, bc(OHG3.unsqueeze(3), [128, 4, 4, 8]), bc(OH13.unsqueeze(2), [128, 4, 4, 8]), ALU.mult, R_, R_)
            tt("dve", OH2F4, bc(OHG3.unsqueeze(3), [128, 4, 4, 8]), bc(OH23.unsqueeze(2), [128, 4, 4, 8]), ALU.mult, R_, R_)
            tt("dve", CBF, OH1F3, OH2F3, ALU.add, R_, [B_CBF])
            ps, bps = next_ps()
            for c in range(4):
                mm(ps[:, c * 64:c * 64 + 32], LTB, CBF[:, c, :], True, True, [B_CONST, B_CBF], [bps])
                mm(ps[:, c * 64 + 32:c * 64 + 64], ALL1, CBF[:, c, :], True, True, [B_CONST, B_CBF], [bps])
            psv = ps[:, 0:256].rearrange("p (c x) -> p c x", x=64)
            cp("dve", RKB3[:, 0, :], CNTB, [B_CNT] + R_, R_)
            for c in range(1, 4):
                tt("dve", RKB3[:, c, :], RKB3[:, c - 1, :], psv[:, c - 1, 32:64], ALU.add, [bps] + R_, R_)
            tt("dve", CNTB, RKB3[:, 3, :], psv[:, 3, 32:64], ALU.add, [bps] + R_, [B_CNT])
            tt("dve", RK3, psv[:, :, 0:32], RKB3, ALU.add, [bps] + R_, R_)
            tt("dve", TMPR3, OH1F3, RK3, ALU.mult, R_, R_)
            red(PF3[:, :, 0], TMPR3, ALU.add)
            tt("dve", TMPR3, OH2F3, RK3, ALU.mult, R_, R_)
            red(PF3[:, :, 1], TMPR3, ALU.add)
            cp("dve", POS[:, ch0:ch0 + 4, :], PF3, R_, [B_POS])
            for c in range(4):
                ch = ch0 + c
                for k in range(2):
                    P.op("pool", lambda e, ix=POS[:, ch, k:k + 1], src=X1B[ch % 8]: e.indirect_dma_start(
                        out=XS, out_offset=bass.IndirectOffsetOnAxis(ap=ix, axis=0), in_=src, in_offset=None),
                        [B_POS, B_X1B[ch % 8]], [B_XS], dma=True)

        NCHK = NTOK // 128
        ld_mgi(0)
        ld_mgi(1)
        for ch in range(3):
            ld_x(ch)
        for step in range(NCHK + 2):
            if step - 2 >= 0:
                stage2(step - 2)
            if 0 <= step - 1 < NCHK:
                stage1b(step - 1)
            if step < NCHK:
                stage1(step)
            if step - 2 >= 0:
                stage2b(step - 2)
                if (step - 2) % 4 == 3:
                    stageB((step - 2) // 4)
        if debug:
            dma("sp", d_pos, POS.rearrange("p a b -> p (a b)"), [B_POS], [Buf()])
            dma("sp", d_wts, WTS.rearrange("p a b -> p (a b)"), [B_WTS], [Buf()])
        P.barrier()

        _hw.append(off[0])
        off[0] = g_mark
        NST = CAP // 128
        NWB = 3
        EW1 = [view([128, 8, 512], BF16) for _ in range(NWB)]
        EW3 = [view([128, 8, 512], BF16) for _ in range(NWB)]
        EW2 = [view([128, 4, 1024], BF16) for _ in range(NWB)]
        B_EW = [[Buf(), Buf(), Buf()] for _ in range(NWB)]
        XSL = [view([128, NST, 1024], BF16) for _ in range(2)]; B_XSL = [Buf() for _ in range(2)]
        XST = [view([128, 8, CAP], BF16) for _ in range(2)]; B_XST = [Buf() for _ in range(2)]
        HT = [view([128, 4, CAP], BF16) for _ in range(2)]; B_HT = [[Buf() for _ in range(4)] for _ in range(2)]
        S1 = [view([128, CAP]) for _ in range(2)]; B_S1 = [Buf() for _ in range(2)]
        YSB = [view([128, 1024]) for _ in range(3)]; B_YSB = [Buf() for _ in range(3)]
        B_YS = Buf()
        n_s1 = [0]
        n_y = [0]

        def p4_load(e):
            w = e % NWB
            dma("pool", EW1[w], ew1[e].rearrange("(kt p) n -> p kt n", p=128), (), [B_EW[w][0]])
            dma("pool", EW3[w], ew3[e].rearrange("(kt p) n -> p kt n", p=128), (), [B_EW[w][1]])
            dma("pool", EW2[w], ew2[e].rearrange("(kt p) n -> p kt n", p=128), (), [B_EW[w][2]])
            dma("sp", XSL[e % 2], XS[e * CAP:(e + 1) * CAP, :].rearrange("(s p) n -> p s n", p=128), [B_XS], [B_XSL[e % 2]])

        def p4_T(e):
            p = e % 2
            for s_ in range(NST):
                pst, bpst = next_ps()
                pstb = pst[:, :].bitcast(BF16)
                for dtl in range(8):
                    tp(pstb[:, dtl * 128:(dtl + 1) * 128], XSL[p][:, s_, dtl * 128:(dtl + 1) * 128], IDB,
                       [B_XSL[p], B_CONST], [bpst])
                act(XST[p][:, :, s_ * 128:(s_ + 1) * 128], pstb.rearrange("p (a b) -> p a b", b=128), AF.Copy,
                    [bpst], [B_XST[p]])

        def p4_H(e):
            p = e % 2; w = e % NWB
            for ft in range(4):
                ps1, bps1 = next_ps()
                for kt in range(8):
                    mm(ps1[:, 0:CAP], EW1[w][:, kt, ft * 128:(ft + 1) * 128], XST[p][:, kt, :], kt == 0, kt == 7,
                       [B_EW[w][0], B_XST[p]], [bps1])
                ps3, bps3 = next_ps()
                for kt in range(8):
                    mm(ps3[:, 0:CAP], EW3[w][:, kt, ft * 128:(ft + 1) * 128], XST[p][:, kt, :], kt == 0, kt == 7,
                       [B_EW[w][1], B_XST[p]], [bps3])
                s1 = S1[n_s1[0] % 2]; bs1 = B_S1[n_s1[0] % 2]
                n_s1[0] += 1
                act(s1, ps1[:, 0:CAP], AF.Silu, [bps1], [bs1])
                tt("dve", HT[p][:, ft, :], ps3[:, 0:CAP], s1, ALU.mult, [bps3, bs1], [B_HT[p][ft]])

        def p4_Y(e):
            p = e % 2; w = e % NWB
            for s_ in range(NST):
                ysb = YSB[n_y[0] % 3]; bysb = B_YSB[n_y[0] % 3]
                n_y[0] += 1
                for half in range(2):
                    ps, bps = next_ps()
                    for ft in range(4):
                        mm(ps[:, :], HT[p][:, ft, s_ * 128:(s_ + 1) * 128], EW2[w][:, ft, half * 512:(half + 1) * 512],
                           ft == 0, ft == 3, [B_HT[p][ft], B_EW[w][2]], [bps])
                    if half == 0:
                        act(ysb[:, 0:512], ps[:, :], AF.Copy, [bps], [bysb])
                    else:
                        cp("dve", ysb[:, 512:1024], ps[:, :], [bps], [bysb])
                r0 = e * CAP + s_ * 128
                dma("sp", YS[r0:r0 + 128, :], ysb, [bysb], [B_YS])

        p4_load(0)
        p4_load(1)
        p4_T(0)
        for e in range(NEXP):
            if e + 2 < NEXP:
                p4_load(e + 2)
            p4_H(e)
            if e + 1 < NEXP:
                p4_T(e + 1)
            p4_Y(e)
        P.barrier()

        _hw.append(off[0])
        off[0] = g_mark
        NR5 = 4
        L2 = view([128, 2048]); B_L2 = Buf()
        Y1 = [view([128, 1024]) for _ in range(NR5)]; B_Y1 = [Buf() for _ in range(NR5)]
        Y2 = [view([128, 1024]) for _ in range(NR5)]; B_Y2 = [Buf() for _ in range(NR5)]
        XA = [view([128, 1024]) for _ in range(NR5)]; B_XA = [Buf() for _ in range(NR5)]
        STATS5 = [view([128, 2, 6]) for _ in range(2)]; B_ST5 = [Buf() for _ in range(2)]
        JUNK5 = view([128, 1024], BF16); B_J5 = Buf()
        MV5 = [view([128, 4]) for _ in range(2)]; B_MV5 = [Buf() for _ in range(2)]
        dma("sp", L2, rp[:, R_L2G:R_L2G + 2048].partition_broadcast(128), (), [B_L2])

        def p5_load(ch):
            t0 = ch * 128
            r = ch % NR5
            dma("sp", XA[r], X1S[t0:t0 + 128, :], [B_X1S], [B_XA[r]])
            P.op("pool", lambda e, ix=POS[:, ch, 0:1], o=Y1[r]: e.indirect_dma_start(
                out=o, out_offset=None, in_=YS, in_offset=bass.IndirectOffsetOnAxis(ap=ix, axis=0)),
                [B_POS, B_YS], [B_Y1[r]], dma=True)
            P.op("pool", lambda e, ix=POS[:, ch, 1:2], o=Y2[r]: e.indirect_dma_start(
                out=o, out_offset=None, in_=YS, in_offset=bass.IndirectOffsetOnAxis(ap=ix, axis=0)),
                [B_POS, B_YS], [B_Y2[r]], dma=True)

        def p5_comp(ch):
            t0 = ch * 128
            r = ch % NR5
            xa = XA[r]; bxa = B_XA[r]; y1 = Y1[r]; by1 = B_Y1[r]; y2 = Y2[r]; by2 = B_Y2[r]
            st5 = STATS5[ch % 2]; bst5 = B_ST5[ch % 2]; mv5 = MV5[ch % 2]; bmv5 = B_MV5[ch % 2]
            act(xa, xa, AF.Identity, [bxa], [bxa], scale=ALPHA)
            stt("dve", xa, y1, WTS[:, ch, 0:1], xa, ALU.mult, ALU.add, [by1, bxa, B_WTS], [bxa])
            stt("dve", xa, y2, WTS[:, ch, 1:2], xa, ALU.mult, ALU.add, [by2, bxa, B_WTS], [bxa])
            act(JUNK5, xa, AF.Identity, [bxa, B_J5], [B_J5, bmv5], scale=1.0 / 1024.0, accum=mv5[:, 0:1])
            act(JUNK5, xa, AF.Square, [bxa, B_J5], [B_J5, bmv5], scale=1.0 / 32.0, accum=mv5[:, 1:2])
            stt("dve", mv5[:, 1:2], mv5[:, 0:1], -1.0, mv5[:, 0:1], ALU.mult, ALU.mult, [bmv5], [bmv5]) if False else None
            tt("dve", mv5[:, 2:3], mv5[:, 0:1], mv5[:, 0:1], ALU.mult, [bmv5], [bmv5])
            tt("dve", mv5[:, 1:2], mv5[:, 1:2], mv5[:, 2:3], ALU.subtract, [bmv5], [bmv5])
            act(mv5[:, 2:3], mv5[:, 1:2], AF.Sqrt, [bmv5], [bmv5], bias=1e-5)
            P.op("dve", lambda e, o=mv5[:, 2:3]: e.reciprocal(out=o, in_=o), [bmv5], [bmv5])
            stt("dve", mv5[:, 3:4], mv5[:, 0:1], -1.0, mv5[:, 2:3], ALU.mult, ALU.mult, [bmv5], [bmv5])
            act(xa, xa, AF.Identity, [bxa, bmv5], [bxa], scale=mv5[:, 2:3], bias=mv5[:, 3:4])
            tt("dve", xa, xa, L2[:, 0:1024], ALU.mult, [bxa, B_L2], [bxa])
            tt("dve", xa, xa, L2[:, 1024:2048], ALU.add, [bxa, B_L2], [bxa])
            dma("sp", out[t0:t0 + 128, :], xa, [bxa], [Buf()])

        NCH5 = NTOK // 128
        p5_load(0)
        p5_load(1)
        for ch in range(NCH5):
            if ch + 2 < NCH5:
                p5_load(ch + 2)
            p5_comp(ch)
        P.barrier()

        _hw.append(off[0])
        build.hw = _hw
        with nc.Block() as block:
            P.emit(block)
    return nc


def _host_inputs(inputs):
    f = lambda k: np.ascontiguousarray(np.asarray(inputs[k], dtype=np.float32)[0])
    x = np.asarray(inputs["x"], dtype=np.float32)
    b_in = f("b_in")
    pp = np.zeros((128, 128), np.float32)

    def put(col, vec):
        n = vec.shape[0] // 128
        pp[:, col:col + n] = vec.reshape(n, 128).T

    put(P_BRX, b_in[C_RX:C_RX + 1024]); put(P_BRY, b_in[C_RY:C_RY + 1024]); put(P_BQ, b_in[C_Q:C_Q + 512])
    put(P_BK, b_in[C_K:C_K + 512]); put(P_BG, b_in[C_G:C_G + 1024])
    pp[0:16, P_BALR] = b_in[C_ALR:C_ALR + 16]
    put(P_BGA, b_in[C_GA:C_GA + 1024]); put(P_BGB, b_in[C_GB:C_GB + 1024])
    cw = f("conv_w")
    pp[:, P_CW:P_CW + 32] = cw.reshape(4, 8, 128).transpose(2, 1, 0).reshape(128, 32)
    put(P_CB, f("conv_b")); put(P_RBA, f("rg_b_a")); put(P_RBX, f("rg_b_x")); put(P_LAM, f("rg_lambda"))
    put(P_GBA, f("gla_b_a")); put(P_NG, f("gla_norm_g"))
    rp = np.zeros((1, NRP), np.float32)
    rp[0, R_BV:R_BV + 1024] = b_in[C_V:C_V + 1024]
    rp[0, R_BO:R_BO + 1024] = f("b_o"); rp[0, R_L1G:R_L1G + 1024] = f("ln1_g"); rp[0, R_L1B:R_L1B + 1024] = f("ln1_b")
    rp[0, R_L2G:R_L2G + 1024] = f("ln2_g"); rp[0, R_L2B:R_L2B + 1024] = f("ln2_b")
    rp[0, R_RB:R_RB + 4] = f("router_b_group"); rp[0, R_RB + 4:R_RB + 36] = f("router_b_expert")
    wr = np.ascontiguousarray(np.concatenate([f("router_w_group"), f("router_w_expert")], axis=1))
    rgw = np.ascontiguousarray(np.stack([f("rg_w_a"), f("rg_w_x")], axis=0))
    ii = np.arange(128)
    c_id = np.eye(128, dtype=np.float32)
    c_lt = (ii[:, None] < ii[None, :]).astype(np.float32)
    c_cm = np.tile((ii[None, :] >= ii[:, None]).astype(np.float32), (1, 4))
    c_mc = np.ones((128, 512), np.float32); c_mc[:, ::128] = 0.0
    c_eb = np.tile((np.arange(NEXP, dtype=np.float32) * CAP)[None, :], (128, 1))
    shared = {
        "w_in": f("w_in"), "pp": pp, "rp": rp, "wr": wr, "rgw": rgw, "wa2": f("gla_w_a2"),
        "wrnn": f("w_proj_rnn"), "wgla": f("w_proj_gla"), "wo": f("w_o"),
        "ew1": f("exp_w1"), "ew3": f("exp_w3"), "ew2": f("exp_w2"),
        "c_id": c_id, "c_lt": c_lt, "c_cm": c_cm, "c_mc": c_mc, "c_eb": c_eb,
    }
    in_maps = []
    for c in range(NCORES):
        xc = np.ascontiguousarray(x[2 * c:2 * c + 2].reshape(NTOK, 1024))
        m = dict(shared)
        m["xn"] = xc
        m["xT"] = np.ascontiguousarray(xc.T)
        in_maps.append(m)
    return in_maps


def kernel(**inputs):
    in_maps = _host_inputs(inputs)
    nc = build(debug=False)
    res = run_bass_kernel_spmd(nc, in_maps, core_ids=list(range(NCORES)))
    outs = [np.asarray(r["out"], dtype=np.float32).reshape(2, SEQ, 1024) for r in res.results]
    return np.concatenate(outs, axis=0)
```

```python
import numpy as np
from contextlib import ExitStack
import concourse.bass as bass
import concourse.mybir as mybir
from concourse.bass_utils import run_bass_kernel_spmd

F32 = mybir.dt.float32
BF16 = mybir.dt.bfloat16
I32 = mybir.dt.int32
AF = mybir.ActivationFunctionType
ALU = mybir.AluOpType
AX = mybir.AxisListType

SAME_ENGINE_SYNC = True
DMA_RING = 12
NCORES = 8
NTOK = 4096
SEQ = 2048
CAP = 384
NEXP = 32
ALPHA = 2.0 ** 0.25
SBUF_WORDS = 44 * 1024

C_RX, C_RY, C_Q, C_K, C_V, C_G, C_ALR, C_GA, C_GB = 0, 1024, 2048, 2560, 3072, 4096, 5120, 5136, 6160
P_BRX, P_BRY, P_BQ, P_BK, P_BG, P_BALR, P_BGA, P_BGB = 0, 8, 16, 20, 24, 32, 33, 41
P_CW, P_CB, P_RBA, P_RBX, P_LAM, P_GBA, P_NG = 49, 81, 89, 97, 105, 113, 117
R_BV, R_BO, R_L1G, R_L1B, R_L2G, R_L2B, R_RB = 0, 1024, 2048, 3072, 4096, 5120, 6144
NRP = 6180


class Buf:
    __slots__ = ("w", "r")

    def __init__(self):
        self.w = None
        self.r = {}


class _Eng:
    def __init__(self, name, sem, dma_sems):
        self.name = name
        self.sem = sem
        self.count = 0
        self.seen = {}
        self.ops = []
        self.dma_sems = dma_sems
        self.dma_n = 0


class Prog:
    def __init__(self, nc, stack):
        self.nc = nc
        self.E = {}
        for name in ("pe", "act", "dve", "pool", "sp"):
            sem = stack.enter_context(nc.semaphore("s_" + name))
            dsems = []
            if name in ("sp", "pool"):
                dsems = [stack.enter_context(nc.semaphore("d_%s%d" % (name, i))) for i in range(DMA_RING)]
            self.E[name] = _Eng(name, sem, dsems)

    def op(self, eng, fn, reads=(), writes=(), dma=False):
        E = self.E[eng]
        deps = {}

        def add(t):
            if t is None:
                return
            if deps.get(t[0], (None, 0))[1] < t[1]:
                deps[t[0]] = t

        for b in reads:
            add(b.w)
        for b in writes:
            add(b.w)
            for t in b.r.values():
                add(t)
        waits = []
        for s, v in deps.values():
            if E.seen.get(s, 0) >= v:
                continue
            if s is E.sem and (eng == "pe" or not SAME_ENGINE_SYNC):
                continue
            waits.append((s, v))
            E.seen[s] = v
        if dma:
            slot = E.dma_n % DMA_RING
            sem = E.dma_sems[slot]
            val = 16 * (E.dma_n // DMA_RING + 1)
            if val > 16 and E.seen.get(sem, 0) < val - 16:
                waits.append((sem, val - 16))
                E.seen[sem] = val - 16
            E.dma_n += 1
            tok = (sem, val)
            inc = 16
        else:
            E.count += 1
            tok = (E.sem, E.count)
            inc = 1
        E.ops.append((waits, fn, tok[0], inc))
        for b in reads:
            if b.r.get(tok[0], (None, 0))[1] < tok[1]:
                b.r[tok[0]] = tok
        for b in writes:
            b.w = tok
            b.r = {}
        return tok

    def barrier(self):
        toks = []
        for E in self.E.values():
            if E.count:
                toks.append((E.sem, E.count))
            for i, s in enumerate(E.dma_sems):
                n = (E.dma_n - 1 - i) // DMA_RING + 1 if E.dma_n > i else 0
                if n > 0:
                    toks.append((s, 16 * n))
        for E in self.E.values():
            waits = []
            for s, v in toks:
                if s is E.sem or E.seen.get(s, 0) >= v:
                    continue
                waits.append((s, v))
                E.seen[s] = v
            if waits:
                E.ops.append((waits, None, None, 0))

    def emit(self, block):
        def run(E):
            def body(eng):
                for waits, fn, sem, inc in E.ops:
                    for s, v in waits:
                        eng.wait_ge(s, v)
                    if fn is not None:
                        fn(eng).then_inc(sem, inc)
            return body

        block.tensor(run(self.E["pe"]))
        block.scalar(run(self.E["act"]))
        block.vector(run(self.E["dve"]))
        block.gpsimd(run(self.E["pool"]))
        block.sync(run(self.E["sp"]))


def build(debug=False):
    nc = bass.Bass("TRN2", target_bir_lowering=False)

    def din(name, shape, dt=F32):
        return nc.dram_tensor(name, list(shape), dt, kind="ExternalInput").ap()

    xT = din("xT", [1024, NTOK]); xn = din("xn", [NTOK, 1024]); w_in = din("w_in", [1024, 7184])
    pp = din("pp", [128, 128]); rp = din("rp", [1, NRP]); wr = din("wr", [1024, 36])
    rgw = din("rgw", [2, 8, 128, 128]); wa2 = din("wa2", [16, 512])
    wrnn = din("wrnn", [1024, 1024]); wgla = din("wgla", [1024, 1024]); wo = din("wo", [1024, 1024])
    ew1 = din("ew1", [NEXP, 1024, 512]); ew3 = din("ew3", [NEXP, 1024, 512]); ew2 = din("ew2", [NEXP, 512, 1024])
    c_id = din("c_id", [128, 128]); c_lt = din("c_lt", [128, 128]); c_cm = din("c_cm", [128, 512])
    c_mc = din("c_mc", [128, 512]); c_eb = din("c_eb", [128, 32])
    out = nc.dram_tensor("out", [NTOK, 1024], F32, kind="ExternalOutput").ap()
    sk = "ExternalOutput" if debug else "Internal"
    AG = nc.dram_tensor("AG", [1024, NTOK], BF16, kind=sk).ap()
    MG = nc.dram_tensor("MG", [1024, NTOK], BF16, kind=sk).ap()
    X1S = nc.dram_tensor("X1S", [NTOK, 1024], F32, kind=sk).ap()
    XS = nc.dram_tensor("XS", [NEXP * CAP, 1024], BF16, kind=sk).ap()
    YS = nc.dram_tensor("YS", [NEXP * CAP, 1024], F32, kind=sk).ap()
    if debug:
        d_pos = nc.dram_tensor("d_pos", [128, 64], I32, kind="ExternalOutput").ap()
        d_wts = nc.dram_tensor("d_wts", [128, 64], F32, kind="ExternalOutput").ap()

    st = ExitStack()
    with st:
        big = st.enter_context(nc.sbuf_tensor("big", [128, SBUF_WORDS], F32))
        PSB = [st.enter_context(nc.psum_tensor("ps%d" % i, [128, 512], F32)) for i in range(8)]
        PSBUF = [Buf() for _ in range(8)]
        P = Prog(nc, st)
        off = [0]
        psn = [0]
        _hw = []

        def view(shape, dt=F32):
            n = 1
            for s in shape[1:]:
                n *= s
            nw = (n * (2 if dt == BF16 else 4) + 3) // 4
            nw = (nw + 7) // 8 * 8
            assert off[0] + nw <= SBUF_WORDS, ("SBUF overflow", off[0], nw)
            v = big[:, off[0]:off[0] + nw]
            off[0] += nw
            if dt != F32:
                v = v.bitcast(dt)
            if dt == BF16 and n % 2:
                v = v[:, 0:n]
            elif dt == BF16:
                v = v[:, 0:n]
            else:
                v = v[:, 0:n]
            if len(shape) > 2:
                names = " ".join("a%d" % i for i in range(len(shape) - 1))
                kw = {"a%d" % i: shape[i + 1] for i in range(len(shape) - 1)}
                v = v.rearrange("p (%s) -> p %s" % (names, names), **kw)
            if shape[0] != 128:
                v = v[0:shape[0]]
            return v

        def next_ps():
            i = psn[0] % 8
            psn[0] += 1
            return PSB[i], PSBUF[i]

        def mm(o, lhsT, rhs, start, stop, reads, writes):
            P.op("pe", lambda e: e.matmul(o, lhsT=lhsT, rhs=rhs, start=start, stop=stop), reads, writes)

        def tp(o, in_, ident, reads, writes):
            P.op("pe", lambda e: e.transpose(o, in_, ident), reads, writes)

        def act(o, in_, func, reads, writes, bias=None, scale=None, accum=None):
            kw = {}
            if bias is not None:
                kw["bias"] = bias
            if scale is not None:
                kw["scale"] = scale
            if accum is not None:
                kw["accum_out"] = accum
            P.op("act", lambda e: e.activation(out=o, in_=in_, func=func, **kw), reads, writes)

        def ts(eng, o, in0, s1, s2, op0, op1, reads, writes):
            if op1 is None:
                P.op(eng, lambda e: e.tensor_scalar(out=o, in0=in0, scalar1=s1, scalar2=None, op0=op0), reads, writes)
            else:
                P.op(eng, lambda e: e.tensor_scalar(out=o, in0=in0, scalar1=s1, scalar2=s2, op0=op0, op1=op1), reads, writes)

        def tt(eng, o, in0, in1, op, reads, writes):
            P.op(eng, lambda e: e.tensor_tensor(out=o, in0=in0, in1=in1, op=op), reads, writes)

        def stt(eng, o, in0, sc, in1, op0, op1, reads, writes):
            P.op(eng, lambda e: e.scalar_tensor_tensor(out=o, in0=in0, scalar=sc, in1=in1, op0=op0, op1=op1), reads, writes)

        def cp(eng, o, in_, reads, writes):
            P.op(eng, lambda e: e.tensor_copy(out=o, in_=in_), reads, writes)

        def ms(eng, o, val, writes):
            P.op(eng, lambda e: e.memset(o, val), (), writes)

        def dma(eng, o, in_, reads, writes):
            P.op(eng, lambda e: e.dma_start(out=o, in_=in_), reads, writes, dma=True)

        def wview(w, c0, c1):
            return w[:, c0:c1].rearrange("(kt p) n -> p kt n", p=128)

        PP = view([128, 128]); B_PP = Buf()
        DER = view([128, 32]); B_DER = Buf()
        TMP8 = view([128, 8]); B_TMP8 = Buf()
        IDB = view([128, 128], BF16); IDF = view([128, 128]); LTB = view([128, 128], BF16)
        ALL1 = view([128, 128], BF16); ONES256 = view([128, 128], BF16); ONE1 = view([128, 128], BF16)
        CMB = view([128, 512], BF16); MCF = view([128, 512]); CNTB = view([128, 32])
        POS = view([128, 32, 2], I32); WTS = view([128, 32, 2])
        B_CONST = Buf(); B_CNT = Buf(); B_POS = Buf(); B_WTS = Buf()
        dma("sp", PP, pp, (), [B_PP])
        dma("sp", IDF, c_id, (), [B_CONST])
        dma("sp", MCF, c_mc, (), [B_CONST])
        dma("sp", CNTB, c_eb, (), [B_CNT])
        dma("pool", IDB, c_id, (), [B_CONST])
        dma("pool", LTB, c_lt, (), [B_CONST])
        dma("pool", CMB, c_cm, (), [B_CONST])
        ms("dve", ALL1, 1.0, [B_CONST])
        ms("dve", ONES256, 1.0 / 256.0, [B_CONST])
        ms("dve", ONE1, 1.0, [B_CONST])
        act(TMP8, PP[:, P_LAM:P_LAM + 8], AF.Exp, [B_PP], [B_TMP8], scale=-1.0)
        act(TMP8, TMP8, AF.Ln, [B_TMP8], [B_TMP8], bias=1.0)
        ts("dve", DER[:, 0:8], TMP8, -8.0, None, ALU.mult, None, [B_TMP8], [B_DER])
        ts("dve", DER[:, 8:16], TMP8, -16.0, None, ALU.mult, None, [B_TMP8], [B_DER])
        ts("dve", DER[:, 16:20], PP[:, P_GBA:P_GBA + 4], -1.0, None, ALU.mult, None, [B_PP], [B_DER])
        g_mark = off[0]
        PR = [B_PP, B_DER]

        def pcol(c):
            return PP[:, c:c + 1]

        W1 = view([128, 8, 3072], BF16); B_W1 = [Buf() for _ in range(3)]
        WRNN = view([128, 8, 1024], BF16); B_WRNN = Buf()
        RGW = view([128, 2, 8, 128], BF16); B_RGW = Buf()
        XT = [view([128, 8, 512], BF16) for _ in range(2)]; B_XT = [Buf() for _ in range(2)]
        NS = 2
        RX = [view([128, 2, 516]) for _ in range(NS)]; B_RX = [[Buf(), Buf()] for _ in range(NS)]
        U = [view([128, 2, 512]) for _ in range(NS)]; B_U = [[Buf(), Buf()] for _ in range(NS)]
        UBF = [view([128, 2, 512], BF16) for _ in range(NS)]; B_UBF = [[Buf(), Buf()] for _ in range(NS)]
        THR = [view([128, 2, 512]) for _ in range(NS)]; B_THR = [[Buf(), Buf()] for _ in range(NS)]
        A2 = [view([128, 2, 512]) for _ in range(NS)]; B_A2 = [[Buf(), Buf()] for _ in range(NS)]
        THI = [view([128, 2, 512]) for _ in range(NS)]; B_THI = [[Buf(), Buf()] for _ in range(NS)]
        GY = [view([128, 2, 512], BF16) for _ in range(NS)]; B_GY = [[Buf(), Buf()] for _ in range(NS)]
        HG = [view([128, 8, 512], BF16) for _ in range(2)]; B_HG = [[Buf() for _ in range(8)] for _ in range(2)]
        HALO = view([128, 8, 4]); B_HALO = [Buf() for _ in range(8)]
        HST = view([128, 8]); B_HST = [Buf() for _ in range(8)]
        SGA = [view([128, 512]) for _ in range(2)]; B_SGA = [Buf() for _ in range(2)]
        AGO = [view([128, 512], BF16) for _ in range(2)]; B_AGO = [Buf() for _ in range(2)]
        B_AG = Buf()
        DER2 = view([128, 32]); B_DER2 = Buf()
        ts("dve", DER2[:, 0:8], DER[:, 0:8], 0.5, None, ALU.mult, None, [B_DER], [B_DER2])
        ts("dve", DER2[:, 8:16], PP[:, P_RBA:P_RBA + 8], 0.5, None, ALU.mult, None, [B_PP], [B_DER2])
        ts("dve", DER2[:, 16:24], PP[:, P_RBX:P_RBX + 8], 0.5, None, ALU.mult, None, [B_PP], [B_DER2])
        ts("dve", DER2[:, 24:32], PP[:, P_BGA:P_BGA + 8], 0.5, None, ALU.mult, None, [B_PP], [B_DER2])
        PR1 = PR + [B_DER2]

        for i, c0 in enumerate((C_RX, C_RY, C_GA)):
            dma("pool", W1[:, :, i * 1024:(i + 1) * 1024], wview(w_in, c0, c0 + 1024), (), [B_W1[i]])
        dma("pool", RGW, rgw.rearrange("g h i j -> i g h j"), (), [B_RGW])
        dma("pool", WRNN, wview(wrnn, 0, 1024), (), [B_WRNN])

        def p1_A(G):
            j, g = G // 4, G % 4
            t0 = j * 512
            first = (j % 4 == 0)
            xt = XT[j % 2]; bxt = B_XT[j % 2]
            sset = G % NS
            if g == 0:
                dma("pool", xt, xT[:, t0:t0 + 512].rearrange("(kt p) n -> p kt n", p=128), (), [bxt])
            for ci in range(2):
                c = g * 2 + ci
                rx = RX[sset]; brx = B_RX[sset][ci]
                u = U[sset]; bu = B_U[sset][ci]
                ps, bps = next_ps()
                for kt in range(8):
                    mm(ps[:, :], W1[:, kt, c * 128:(c + 1) * 128], xt[:, kt, :], kt == 0, kt == 7, [B_W1[0], bxt], [bps])
                if first:
                    ms("pool", rx[:, ci, 0:3], 0.0, [brx])
                else:
                    cp("pool", rx[:, ci, 0:3], HALO[:, c, 0:3], [B_HALO[c]], [brx])
                act(rx[:, ci, 3:515], ps[:, :], AF.Identity, [bps] + PR1, [brx], bias=pcol(P_BRX + c))
                cp("pool", HALO[:, c, 0:3], rx[:, ci, 512:515], [brx], [B_HALO[c]])
                act(u[:, ci, :], rx[:, ci, 3:515], AF.Identity, [brx] + PR1, [bu], scale=pcol(P_CW + c * 4 + 3),
                    bias=pcol(P_CB + c))
                for k in (0, 1, 2):
                    stt("dve", u[:, ci, :], rx[:, ci, k:k + 512], pcol(P_CW + c * 4 + k), u[:, ci, :],
                        ALU.mult, ALU.add, [brx, bu] + PR1, [bu])
            for ci in range(2):
                c = g * 2 + ci
                ps, bps = next_ps()
                for kt in range(8):
                    mm(ps[:, :], W1[:, kt, 1024 + c * 128:1024 + (c + 1) * 128], xt[:, kt, :], kt == 0, kt == 7,
                       [B_W1[1], bxt], [bps])
                act(GY[sset][:, ci, :], ps[:, :], AF.Gelu_apprx_tanh, [bps] + PR1, [B_GY[sset][ci]], bias=pcol(P_BRY + c))
            for ci in range(2):
                cp("pool", UBF[sset][:, ci, :], U[sset][:, ci, :], [B_U[sset][ci]], [B_UBF[sset][ci]])

        def p1_B(G):
            j, g = G // 4, G % 4
            first = (j % 4 == 0)
            sset = G % NS
            u = U[sset]; thr = THR[sset]; thi = THI[sset]; a2 = A2[sset]; ubf = UBF[sset]; gy = GY[sset]
            for ci in range(2):
                c = g * 2 + ci
                ps, bps = next_ps()
                mm(ps[:, :], RGW[:, 0, c, :], ubf[:, ci, :], True, True, [B_RGW, B_UBF[sset][ci]], [bps])
                act(thr[:, ci, :], ps[:, :], AF.Tanh, [bps] + PR1, [B_THR[sset][ci]], bias=DER2[:, 8 + c:9 + c], scale=0.5)
                ps, bps = next_ps()
                mm(ps[:, :], RGW[:, 1, c, :], ubf[:, ci, :], True, True, [B_RGW, B_UBF[sset][ci]], [bps])
                act(thi[:, ci, :], ps[:, :], AF.Tanh, [bps] + PR1, [B_THI[sset][ci]], bias=DER2[:, 16 + c:17 + c], scale=0.5)
            for ci in range(2):
                c = g * 2 + ci
                act(thr[:, ci, :], thr[:, ci, :], AF.Exp, [B_THR[sset][ci]] + PR1, [B_THR[sset][ci]],
                    scale=DER2[:, c:c + 1], bias=DER2[:, c:c + 1])
                tt("pool", a2[:, ci, :], thr[:, ci, :], thr[:, ci, :], ALU.mult, [B_THR[sset][ci]], [B_A2[sset][ci]])
            for ci in range(2):
                ts("dve", a2[:, ci, :], a2[:, ci, :], 0.99999994, -1.0, ALU.min, ALU.mult, [B_A2[sset][ci]], [B_A2[sset][ci]])
                stt("dve", thi[:, ci, :], thi[:, ci, :], 1.0, u[:, ci, :], ALU.add, ALU.mult,
                    [B_THI[sset][ci], B_U[sset][ci]], [B_THI[sset][ci]])
            for ci in range(2):
                act(a2[:, ci, :], a2[:, ci, :], AF.Sqrt, [B_A2[sset][ci]], [B_A2[sset][ci]], bias=0.25, scale=0.25)
            for ci in range(2):
                c = g * 2 + ci
                tt("dve", thi[:, ci, :], thi[:, ci, :], a2[:, ci, :], ALU.mult, [B_A2[sset][ci], B_THI[sset][ci]],
                   [B_THI[sset][ci]])
                init = 0.0 if first else HST[:, c:c + 1]
                P.op("dve", lambda e, o=u[:, ci, :], d0=thr[:, ci, :], d1=thi[:, ci, :], ini=init:
                     e.tensor_tensor_scan(out=o, data0=d0, data1=d1, initial=ini, op0=ALU.mult, op1=ALU.add),
                     [B_THR[sset][ci], B_THI[sset][ci], B_HST[c], B_U[sset][ci]], [B_U[sset][ci]])
                cp("dve", HST[:, c:c + 1], u[:, ci, 511:512], [B_U[sset][ci]], [B_HST[c]])
                stt("dve", HG[j % 2][:, c, :], u[:, ci, :], 0.5, gy[:, ci, :], ALU.mult, ALU.mult,
                    [B_U[sset][ci], B_GY[sset][ci]], [B_HG[j % 2][c]])

        n_ag = [0]

        def p1_C(j):
            t0 = j * 512
            xt = XT[j % 2]; bxt = B_XT[j % 2]
            for dt in range(8):
                ps, bps = next_ps()
                for kt in range(8):
                    mm(ps[:, :], W1[:, kt, 2048 + dt * 128:2048 + (dt + 1) * 128], xt[:, kt, :], kt == 0, kt == 7,
                       [B_W1[2], bxt], [bps])
                sga = SGA[n_ag[0] % 2]; bsga = B_SGA[n_ag[0] % 2]
                ago = AGO[n_ag[0] % 2]; bago = B_AGO[n_ag[0] % 2]
                n_ag[0] += 1
                act(sga, ps[:, :], AF.Tanh, [bps] + PR1, [bsga], bias=DER2[:, 24 + dt:25 + dt], scale=0.5)
                ps2, bps2 = next_ps()
                for c in range(8):
                    mm(ps2[:, :], WRNN[:, c, dt * 128:(dt + 1) * 128], HG[j % 2][:, c, :], c == 0, c == 7,
                       [B_WRNN, B_HG[j % 2][c]], [bps2])
                stt("dve", ago, sga, 1.0, ps2[:, :], ALU.add, ALU.mult, [bps2, bsga], [bago])
                dma("sp", AG[dt * 128:(dt + 1) * 128, t0:t0 + 512], ago, [bago], [B_AG])

        NG = (NTOK // 512) * 4
        p1_A(0)
        for G in range(NG):
            if G + 1 < NG:
                p1_A(G + 1)
            p1_B(G)
            if G % 4 == 0 and G >= 4:
                p1_C(G // 4 - 1)
        p1_C(NTOK // 512 - 1)
        P.barrier()

        _hw.append(off[0])
        off[0] = g_mark
        TB = 256
        NCH = TB // 128
        NW2 = 4112
        W2 = view([128, 8, NW2], BF16); B_W2 = Buf()
        o_q, o_k, o_v, o_g, o_alr, o_gb = 0, 512, 1024, 2048, 3072, 3088
        WGLA = view([128, 8, 1024], BF16); B_WGLA = Buf()
        WA2 = view([16, 512], BF16); B_WA2 = Buf()
        BVB = view([1, 1024], BF16); B_BVB = Buf()
        XT2 = [view([128, 8, TB], BF16) for _ in range(3)]; B_XT2 = [Buf() for _ in range(3)]
        ALRB = view([16, TB], BF16); B_ALRB = Buf()
        ECS = view([128, 4, TB]); B_ECS = [Buf() for _ in range(4)]
        CS = view([128, 4, TB]); B_CS = [Buf() for _ in range(4)]
        EINV = [view([128, TB]) for _ in range(2)]; B_EINV = [Buf() for _ in range(2)]
        EBL = [view([128, 4, NCH]) for _ in range(2)]; B_EBL = [[Buf() for _ in range(4)] for _ in range(2)]
        QD = [view([128, 4, TB], BF16) for _ in range(2)]; B_QD = [[Buf() for _ in range(4)] for _ in range(2)]
        KI = [view([128, 4, TB], BF16) for _ in range(2)]; B_KI = [[Buf() for _ in range(4)] for _ in range(2)]
        KT = [view([128, NCH, 512], BF16) for _ in range(2)]; B_KT = [[Buf() for _ in range(NCH)] for _ in range(2)]
        VT = [view([128, NCH, 1024], BF16) for _ in range(2)]; B_VT = [[Buf() for _ in range(NCH)] for _ in range(2)]
        SC = [view([128, NCH, 512], BF16) for _ in range(2)]; B_SC = [[Buf() for _ in range(NCH)] for _ in range(2)]
        S = view([128, 4, 256]); B_S = [Buf() for _ in range(4)]
        SBF = view([128, 4, 256], BF16); B_SBF = [Buf() for _ in range(4)]
        T1 = [view([128, 256]) for _ in range(2)]; B_T1 = [Buf() for _ in range(2)]
        SQ = view([128, 2, 512], BF16); B_SQ = [Buf() for _ in range(2)]
        RSTD = [view([128, 512]) for _ in range(2)]; B_RSTD = [Buf() for _ in range(2)]
        EPS6 = view([128, 8]); B_EPS6 = Buf()
        ms("dve", EPS6, 1e-6, [B_EPS6])
        ON = view([128, 8, TB]); B_ON = [Buf() for _ in range(8)]
        SG = view([128, 8, TB], BF16); B_SG = [Buf() for _ in range(8)]
        OFIN = view([128, 8, TB], BF16); B_OFIN = [Buf() for _ in range(8)]
        AGI = view([128, 8, TB], BF16); B_AGI = Buf()
        SGB = view([128, 8, TB], BF16); B_SGB = [Buf() for _ in range(8)]
        TBB = [view([128, TB]) for _ in range(2)]; B_TBB = [Buf() for _ in range(2)]
        B_MG = Buf()
        pso_n = [0]; psg_n = [0]

        def ps_o():
            i = pso_n[0] % 4
            pso_n[0] += 1
            return PSB[i], PSBUF[i]

        def ps_g():
            i = 4 + psg_n[0] % 4
            psg_n[0] += 1
            return PSB[i], PSBUF[i]

        B_W2A = Buf(); B_W2QK = Buf(); B_W2V = Buf(); B_W2G = Buf(); B_W2GB = Buf()
        dma("pool", W2[:, :, o_alr:o_alr + 16], wview(w_in, C_ALR, C_ALR + 16), (), [B_W2A])
        dma("pool", WA2, wa2, (), [B_WA2])
        dma("pool", BVB, rp[:, R_BV:R_BV + 1024], (), [B_BVB])
        dma("pool", W2[:, :, o_q:o_q + 1024], wview(w_in, C_Q, C_Q + 1024), (), [B_W2QK])
        dma("pool", W2[:, :, o_v:o_v + 1024], wview(w_in, C_V, C_V + 1024), (), [B_W2V])
        dma("pool", W2[:, :, o_g:o_g + 1024], wview(w_in, C_G, C_G + 1024), (), [B_W2G])
        dma("pool", W2[:, :, o_gb:o_gb + 1024], wview(w_in, C_GB, C_GB + 1024), (), [B_W2GB])
        dma("pool", WGLA, wview(wgla, 0, 1024), (), [B_WGLA])

        def p2_ab(j):
            par = j % 2
            t0 = j * TB
            xt = XT2[j % 3]; bxt = B_XT2[j % 3]
            dma("pool", xt, xT[:, t0:t0 + TB].rearrange("(kt p) n -> p kt n", p=128), (), [bxt])
            ps, bps = ps_g()
            for kt in range(8):
                mm(ps[0:16, 0:TB], W2[:, kt, o_alr:o_alr + 16], xt[:, kt, :], kt == 0, kt == 7, [B_W2A, bxt], [bps])
            act(ALRB, ps[0:16, 0:TB], AF.Identity, [bps] + PR, [B_ALRB], bias=PP[0:16, P_BALR:P_BALR + 1])
            for hd in range(4):
                psz, bpsz = ps_g()
                mm(psz[:, 0:TB], WA2[:, hd * 128:(hd + 1) * 128], ALRB, True, True, [B_WA2, B_ALRB], [bpsz])
                act(ECS[:, hd, :], psz[:, 0:TB], AF.Exp, [bpsz] + PR, [B_ECS[hd]], bias=DER[:, 16 + hd:17 + hd], scale=-1.0)
            for hd in range(4):
                act(ECS[:, hd, :], ECS[:, hd, :], AF.Ln, [B_ECS[hd]], [B_ECS[hd]], bias=1.0)
            for hd in range(4):
                P.op("dve", lambda e, o=CS[:, hd, :], d0=MCF[:, 0:TB], d1=ECS[:, hd, :]:
                     e.tensor_tensor_scan(out=o, data0=d0, data1=d1, initial=0.0, op0=ALU.mult, op1=ALU.add),
                     [B_ECS[hd], B_CONST], [B_CS[hd]])
            for hd in range(4):
                psq, bpsq = ps_g()
                for kt in range(8):
                    mm(psq[:, 0:TB], W2[:, kt, o_q + hd * 128:o_q + (hd + 1) * 128], xt[:, kt, :], kt == 0, kt == 7,
                       [B_W2QK, bxt], [bpsq])
                psk, bpsk = ps_g()
                for kt in range(8):
                    mm(psk[:, 0:TB], W2[:, kt, o_k + hd * 128:o_k + (hd + 1) * 128], xt[:, kt, :], kt == 0, kt == 7,
                       [B_W2QK, bxt], [bpsk])
                eb = ECS[:, hd, :]; beb = B_ECS[hd]
                einv = EINV[hd % 2]; beinv = B_EINV[hd % 2]
                act(eb, CS[:, hd, :], AF.Exp, [B_CS[hd]], [beb], scale=-1.0 / 16.0, bias=float(-0.5 * np.log(128.0)))
                act(einv, CS[:, hd, :], AF.Exp, [B_CS[hd]], [beinv], scale=1.0 / 16.0)
                act(EBL[par][:, hd, :], CS[:, hd, :].rearrange("p (c t) -> p c t", t=128)[:, :, 127], AF.Exp, [B_CS[hd]],
                    [B_EBL[par][hd]], scale=-1.0 / 16.0)
                stt("dve", QD[par][:, hd, :], psq[:, 0:TB], pcol(P_BQ + hd), eb, ALU.add, ALU.mult,
                    [bpsq, beb] + PR, [B_QD[par][hd]])
                stt("dve", KI[par][:, hd, :], psk[:, 0:TB], pcol(P_BK + hd), einv, ALU.add, ALU.mult,
                    [bpsk, beinv] + PR, [B_KI[par][hd]])
            for c in range(NCH):
                pst, bpst = ps_g()
                pstb = pst[:, :].bitcast(BF16)
                for hd in range(4):
                    tp(pstb[:, hd * 128:(hd + 1) * 128], KI[par][:, hd, c * 128:(c + 1) * 128], IDB,
                       [B_KI[par][hd], B_CONST], [bpst])
                act(KT[par][:, c, :], pstb[:, 0:512], AF.Copy, [bpst], [B_KT[par][c]])
                for half in range(2):
                    psv, bpsv = ps_g()
                    for kt in range(8):
                        mm(psv[:, :], xt[:, kt, c * 128:(c + 1) * 128],
                           W2[:, kt, o_v + half * 512:o_v + (half + 1) * 512], kt == 0, False, [B_W2V, bxt], [bpsv])
                    mm(psv[:, :], ONE1[0:1, :], BVB[0:1, half * 512:(half + 1) * 512], False, True,
                       [B_CONST, B_BVB], [bpsv])
                    act(VT[par][:, c, half * 512:(half + 1) * 512], psv[:, :], AF.Copy, [bpsv], [B_VT[par][c]])
                pss, bpss = ps_g()
                for hd in range(4):
                    mm(pss[:, hd * 128:(hd + 1) * 128], KI[par][:, hd, c * 128:(c + 1) * 128],
                       QD[par][:, hd, c * 128:(c + 1) * 128], True, True, [B_KI[par][hd], B_QD[par][hd]], [bpss])
                tt("dve", SC[par][:, c, :], pss[:, :], CMB, ALU.mult, [bpss, B_CONST], [B_SC[par][c]])

        n_t1 = [0]

        def p2_rms(j, c, pso):
            for hh in range(2):
                po, bpo = pso[hh]
                act(SQ[:, hh, :], po[:, :], AF.Square, [bpo], [B_SQ[hh]])
            pn, bpn = ps_g()
            for hd in range(4):
                for vh in range(2):
                    col = ((hd % 2) * 2 + vh) * 128
                    mm(pn[:, hd * 128:(hd + 1) * 128], ONES256, SQ[:, hd // 2, col:col + 128], vh == 0, vh == 1,
                       [B_CONST, B_SQ[hd // 2]], [bpn])
            rstd = RSTD[c % 2]; brstd = B_RSTD[c % 2]
            act(rstd, pn[:, :], AF.Ln, [bpn, B_EPS6], [brstd], bias=EPS6[:, 0:1])
            return rstd, brstd

        def p2_rms2(j, c, pso, rstd, brstd):
            for hd in range(4):
                po, bpo = pso[hd // 2]
                for vh in range(2):
                    col = ((hd % 2) * 2 + vh) * 128
                    vt = hd * 2 + vh
                    tt("dve", ON[:, vt, c * 128:(c + 1) * 128], po[:, col:col + 128],
                       rstd[:, hd * 128:(hd + 1) * 128], ALU.mult, [bpo, brstd], [B_ON[vt]])

        def p2_c(j):
            par = j % 2
            t0 = j * TB
            first = (t0 % SEQ == 0)
            xt = XT2[j % 3]; bxt = B_XT2[j % 3]
            if first:
                for hd in range(4):
                    ms("dve", S[:, hd, :], 0.0, [B_S[hd]])
                    ms("dve", SBF[:, hd, :], 0.0, [B_SBF[hd]])
            pend = []
            for c in range(NCH):
                pso = [ps_o(), ps_o()]
                for hd in range(4):
                    po, bpo = pso[hd // 2]
                    for vh in range(2):
                        col = ((hd % 2) * 2 + vh) * 128
                        mm(po[:, col:col + 128], VT[par][:, c, hd * 256 + vh * 128:hd * 256 + (vh + 1) * 128],
                           SC[par][:, c, hd * 128:(hd + 1) * 128], True, False, [B_VT[par][c], B_SC[par][c]], [bpo])
                        mm(po[:, col:col + 128], SBF[:, hd, vh * 128:(vh + 1) * 128],
                           QD[par][:, hd, c * 128:(c + 1) * 128], False, True, [B_SBF[hd], B_QD[par][hd]], [bpo])
                    pkv, bpkv = ps_g()
                    mm(pkv[:, 0:256], KT[par][:, c, hd * 128:(hd + 1) * 128], VT[par][:, c, hd * 256:(hd + 1) * 256],
                       True, True, [B_KT[par][c], B_VT[par][c]], [bpkv])
                    t1 = T1[n_t1[0] % 2]; bt1 = B_T1[n_t1[0] % 2]
                    n_t1[0] += 1
                    act(t1, S[:, hd, :], AF.Identity, [B_S[hd], B_EBL[par][hd]], [bt1], scale=EBL[par][:, hd, c:c + 1])
                    stt("dve", S[:, hd, :], pkv[:, 0:256], EBL[par][:, hd, c:c + 1], t1, ALU.mult, ALU.add,
                        [bpkv, B_EBL[par][hd], bt1], [B_S[hd]])
                    act(SBF[:, hd, :], S[:, hd, :], AF.Copy, [B_S[hd]], [B_SBF[hd]])
                    vt = c * 4 + hd
                    if vt < 8:
                        ps, bps = ps_g()
                        for kt in range(8):
                            mm(ps[:, 0:TB], W2[:, kt, o_g + vt * 128:o_g + (vt + 1) * 128], xt[:, kt, :], kt == 0,
                               kt == 7, [B_W2G, bxt], [bps])
                        act(SG[:, vt, :], ps[:, 0:TB], AF.Silu, [bps] + PR, [B_SG[vt]], bias=pcol(P_BG + vt))
                pend.append((c, pso))
            rs = [p2_rms(j, c_, pso_) for (c_, pso_) in pend]
            for (rstd, brstd) in rs:
                act(rstd, rstd, AF.Exp, [brstd], [brstd], scale=-0.5)
            p2_e1(j)
            for (c_, pso_), (rstd, brstd) in zip(pend, rs):
                p2_rms2(j, c_, pso_, rstd, brstd)
            for vt in range(8):
                stt("dve", OFIN[:, vt, :], ON[:, vt, :], pcol(P_NG + vt), SG[:, vt, :], ALU.mult, ALU.mult,
                    [B_ON[vt], B_SG[vt]] + PR, [B_OFIN[vt]])

        n_mg = [0]

        def p2_e1(j):
            par = j % 2
            xt = XT2[j % 3]; bxt = B_XT2[j % 3]
            for dt in range(8):
                ps, bps = ps_g()
                for kt in range(8):
                    mm(ps[:, 0:TB], W2[:, kt, o_gb + dt * 128:o_gb + (dt + 1) * 128], xt[:, kt, :], kt == 0, kt == 7,
                       [B_W2GB, bxt], [bps])
                act(SGB[:, dt, :], ps[:, 0:TB], AF.Sigmoid, [bps] + PR, [B_SGB[dt]], bias=pcol(P_BGB + dt))

        def p2_e(j):
            par = j % 2
            t0 = j * TB
            dma("sp", AGI, AG[:, t0:t0 + TB].rearrange("(dt p) n -> p dt n", p=128), [B_AG], [B_AGI])
            for dt in range(8):
                sgb = SGB[:, dt, :]; bsgb = B_SGB[dt]
                tbb = TBB[n_mg[0] % 2]; btbb = B_TBB[n_mg[0] % 2]
                n_mg[0] += 1
                ps2, bps2 = ps_g()
                for vt in range(8):
                    mm(ps2[:, 0:TB], WGLA[:, vt, dt * 128:(dt + 1) * 128], OFIN[:, vt, :], vt == 0, vt == 7,
                       [B_WGLA, B_OFIN[vt]], [bps2])
                tt("dve", tbb, ps2[:, 0:TB], sgb, ALU.mult, [bps2, bsgb], [btbb])
                tt("dve", AGI[:, dt, :], tbb, AGI[:, dt, :], ALU.add, [btbb, B_AGI], [B_AGI])
            dma("sp", MG[:, t0:t0 + TB].rearrange("(dt p) n -> p dt n", p=128), AGI, [B_AGI], [B_MG])

        NB2 = NTOK // TB
        p2_ab(0)
        for j in range(NB2):
            if j + 1 < NB2:
                p2_ab(j + 1)
            p2_c(j)
            p2_e(j)
        P.barrier()

        _hw.append(off[0])
        off[0] = g_mark
        WO = view([128, 8, 1024], BF16); B_WO = Buf()
        WRS = view([128, 8, 36]); B_WRS = Buf()
        RPB = view([128, 3 * 1024]); B_RPB = Buf()
        RBB = view([128, 36]); B_RBB = Buf()
        MGI = [view([128, 8, 512], BF16) for _ in range(2)]; B_MGI = [Buf() for _ in range(2)]
        NZ = 4
        XTM = [view([128, 1024]) for _ in range(NZ)]; B_XTM = [Buf() for _ in range(NZ)]
        Z = [view([128, 1024]) for _ in range(NZ)]; B_Z = [Buf() for _ in range(NZ)]
        X1B = [view([128, 1024], BF16) for _ in range(8)]; B_X1B = [Buf() for _ in range(8)]
        X1T = [view([128, 8, 128]) for _ in range(2)]; B_X1T = [Buf() for _ in range(2)]
        STATS = [view([128, 2, 6]) for _ in range(2)]; B_STATS = [Buf() for _ in range(2)]
        MV4 = [view([128, 4]) for _ in range(NZ)]; B_MV4 = [Buf() for _ in range(NZ)]
        LG4 = [view([128, 4, 36]) for _ in range(2)]; B_LG4 = [Buf() for _ in range(2)]
        RT = view([128, 1400]); B_RT = Buf()
        CBF = view([128, 4, 32], BF16); B_CBF = Buf()
        B_X1S = Buf(); B_XS = Buf()
        dma("pool", WO, wview(wo, 0, 1024), (), [B_WO])
        BOB = view([1, 1024], BF16); B_BOB = Buf()
        dma("pool", BOB, rp[:, R_BO:R_BO + 1024], (), [B_BOB])
        dma("sp", WRS, wr.rearrange("(kt p) n -> p kt n", p=128), (), [B_WRS])
        dma("sp", RPB, rp[:, R_BO:R_BO + 3072].partition_broadcast(128), (), [B_RPB])
        dma("sp", RBB, rp[:, R_RB:R_RB + 36].partition_broadcast(128), (), [B_RBB])
        _ro = [0]

        def rt(n, shape=None):
            v = RT[:, _ro[0]:_ro[0] + n]
            _ro[0] += n
            return v

        GMAX = rt(4); OHG = rt(16); DG = rt(16); SUMG = rt(4); PG = rt(4)
        T44 = rt(128); ESEL = rt(32); M1 = rt(4); OH1 = rt(32); E2 = rt(32); M2 = rt(4); OH2 = rt(32)
        DD = rt(4); W1c = rt(4); OH1F = rt(128); OH2F = rt(128); RKB = rt(128); RK = rt(128); TMPR = rt(128); PF = rt(8)
        v3 = lambda a, n: a.rearrange("p (c x) -> p c x", x=n)
        OHG3 = v3(OHG, 4); DG3 = v3(DG, 4); ESEL3 = v3(ESEL, 8); OH13 = v3(OH1, 8); E23 = v3(E2, 8); OH23 = v3(OH2, 8)
        T444 = T44.rearrange("p (c g e) -> p c g e", c=4, g=4)
        OH1F4 = OH1F.rearrange("p (c g e) -> p c g e", c=4, g=4); OH2F4 = OH2F.rearrange("p (c g e) -> p c g e", c=4, g=4)
        OH1F3 = v3(OH1F, 32); OH2F3 = v3(OH2F, 32); RKB3 = v3(RKB, 32); RK3 = v3(RK, 32); TMPR3 = v3(TMPR, 32)
        PF3 = v3(PF, 2)
        bc = lambda a, shp: a.broadcast_to(shp)
        R_ = [B_RT]

        def red(o, i, op):
            P.op("dve", lambda e: e.tensor_reduce(out=o, in_=i, axis=AX.X, op=op), R_, R_)

        def ld_mgi(j):
            dma("sp", MGI[j % 2], MG[:, j * 512:(j + 1) * 512].rearrange("(dt p) n -> p dt n", p=128), [B_MG],
                [B_MGI[j % 2]])

        def ld_x(ch):
            dma("sp", XTM[ch % NZ], xn[ch * 128:(ch + 1) * 128, :], (), [B_XTM[ch % NZ]])

        def stage1(ch):
            j, cc = ch // 4, ch % 4
            t0 = ch * 128
            if cc == 0 and j >= 1 and j + 1 < NTOK // 512:
                ld_mgi(j + 1)
            if ch + 3 < NTOK // 128:
                ld_x(ch + 3)
            mgi = MGI[j % 2]; bmgi = B_MGI[j % 2]
            xtm = XTM[ch % NZ]; bxtm = B_XTM[ch % NZ]
            z = Z[ch % NZ]; bz = B_Z[ch % NZ]
            mv = MV4[ch % NZ]; bmv = B_MV4[ch % NZ]
            stt_ = STATS[ch % 2]; bst = B_STATS[ch % 2]
            x1b = X1B[ch % 8]; bx1b = B_X1B[ch % 8]
            for half in range(2):
                ps, bps = next_ps()
                for jt in range(8):
                    mm(ps[:, :], mgi[:, jt, cc * 128:(cc + 1) * 128], WO[:, jt, half * 512:(half + 1) * 512],
                       jt == 0, False, [bmgi, B_WO], [bps])
                mm(ps[:, :], ONE1[0:1, :], BOB[0:1, half * 512:(half + 1) * 512], False, True, [B_CONST, B_BOB], [bps])
                stt("dve", z[:, half * 512:(half + 1) * 512], xtm[:, half * 512:(half + 1) * 512], ALPHA, ps[:, :],
                    ALU.mult, ALU.add, [bps, bxtm], [bz])
                P.op("dve", lambda e, o=stt_[:, half, :], i=z[:, half * 512:(half + 1) * 512]: e.bn_stats(out=o, in_=i),
                     [bz], [bst])
            P.op("dve", lambda e, o=mv[:, 0:2], i=stt_.rearrange("p a b -> p (a b)"): e.bn_aggr(out=o, in_=i),
                 [bst], [bmv])
            act(mv[:, 2:3], mv[:, 1:2], AF.Sqrt, [bmv], [bmv], bias=1e-5)
            P.op("dve", lambda e, o=mv[:, 2:3]: e.reciprocal(out=o, in_=o), [bmv], [bmv])
            stt("dve", mv[:, 3:4], mv[:, 0:1], -1.0, mv[:, 2:3], ALU.mult, ALU.mult, [bmv], [bmv])

        def stage1b(ch):
            t0 = ch * 128
            z = Z[ch % NZ]; bz = B_Z[ch % NZ]
            mv = MV4[ch % NZ]; bmv = B_MV4[ch % NZ]
            x1b = X1B[ch % 8]; bx1b = B_X1B[ch % 8]
            act(z, z, AF.Identity, [bz, bmv], [bz], scale=mv[:, 2:3], bias=mv[:, 3:4])
            tt("dve", z, z, RPB[:, 1024:2048], ALU.mult, [bz, B_RPB], [bz])
            tt("dve", z, z, RPB[:, 2048:3072], ALU.add, [bz, B_RPB], [bz])
            dma("sp", X1S[t0:t0 + 128, :], z, [bz], [B_X1S])
            act(x1b, z, AF.Copy, [bz], [bx1b])

        def stage2(ch):
            j, cc = ch // 4, ch % 4
            z = Z[ch % NZ]; bz = B_Z[ch % NZ]
            x1t = X1T[ch % 2]; bx1t = B_X1T[ch % 2]
            for half in range(2):
                ps, bps = next_ps()
                for q4 in range(4):
                    dtl = half * 4 + q4
                    tp(ps[:, q4 * 128:(q4 + 1) * 128], z[:, dtl * 128:(dtl + 1) * 128], IDF, [bz, B_CONST], [bps])
                act(x1t[:, half * 4:(half + 1) * 4, :], ps[:, :].rearrange("p (a b) -> p a b", b=128), AF.Copy,
                    [bps], [bx1t])

        def stage2b(ch):
            j, cc = ch // 4, ch % 4
            x1t = X1T[ch % 2]; bx1t = B_X1T[ch % 2]
            ps, bps = next_ps()
            for dtl in range(8):
                mm(ps[:, 0:36], x1t[:, dtl, :], WRS[:, dtl, :], dtl == 0, dtl == 7, [bx1t, B_WRS], [bps])
            tt("dve", LG4[j % 2][:, cc, :], ps[:, 0:36], RBB, ALU.add, [bps, B_RBB], [B_LG4[j % 2]])

        def stageB(j):
            LG = LG4[j % 2]; blg = B_LG4[j % 2]
            ch0 = j * 4
            LGg = LG[:, :, 0:4]
            LGe = LG[:, :, 4:36].rearrange("p c (g e) -> p c g e", e=8)
            P.op("dve", lambda e: e.tensor_reduce(out=GMAX, in_=LGg, axis=AX.X, op=ALU.max), [blg] + R_, R_)
            tt("dve", OHG3, LGg, bc(GMAX.unsqueeze(2), [128, 4, 4]), ALU.is_equal, [blg] + R_, R_)
            tt("dve", DG3, LGg, bc(GMAX.unsqueeze(2), [128, 4, 4]), ALU.subtract, [blg] + R_, R_)
            act(DG, DG, AF.Exp, R_, R_)
            red(SUMG, DG3, ALU.add)
            P.op("dve", lambda e: e.reciprocal(out=PG, in_=SUMG), R_, R_)
            tt("dve", T444, LGe, bc(OHG3.unsqueeze(3), [128, 4, 4, 8]), ALU.mult, [blg] + R_, R_)
            red(ESEL3, T444.rearrange("p c g e -> p c e g"), ALU.add)
            red(M1, ESEL3, ALU.max)
            tt("dve", OH13, ESEL3, bc(M1.unsqueeze(2), [128, 4, 8]), ALU.is_equal, R_, R_)
            stt("dve", E2, OH1, -1e30, ESEL, ALU.mult, ALU.add, R_, R_)
            red(M2, E23, ALU.max)
            tt("dve", OH23, E23, bc(M2.unsqueeze(2), [128, 4, 8]), ALU.is_equal, R_, R_)
            tt("dve", DD, M2, M1, ALU.subtract, R_, R_)
            act(DD, DD, AF.Exp, R_, R_)
            ts("dve", DD, DD, 1.0, None, ALU.add, None, R_, R_)
            P.op("dve", lambda e: e.reciprocal(out=W1c, in_=DD), R_, R_)
            tt("dve", WTS[:, ch0:ch0 + 4, 0], W1c, PG, ALU.mult, R_, [B_WTS])
            tt("dve", WTS[:, ch0:ch0 + 4, 1], PG, WTS[:, ch0:ch0 + 4, 0], ALU.subtract, R_ + [B_WTS], [B_WTS])
            tt("dve", OH1F4, bc(OHG3.unsqueeze(3), [128, 4, 4, 8]), bc(OH13.unsqueeze(2), [128, 4, 4, 8]), ALU.mult, R_, R_)
            tt("dve", OH2F4, bc(OHG3.unsqueeze(3), [128, 4, 4, 8]), bc(OH23.unsqueeze(2), [128, 4, 4, 8]), ALU.mult, R_, R_)
            tt("dve", CBF, OH1F3, OH2F3, ALU.add, R_, [B_CBF])
            ps, bps = next_ps()
            for c in range(4):
                mm(ps[:, c * 64:c * 64 + 32], LTB, CBF[:, c, :], True, True, [B_CONST, B_CBF], [bps])
                mm(ps[:, c * 64 + 32:c * 64 + 64], ALL1, CBF[:, c, :], True, True, [B_CONST, B_CBF], [bps])
            psv = ps[:, 0:256].rearrange("p (c x) -> p c x", x=64)
            cp("dve", RKB3[:, 0, :], CNTB, [B_CNT] + R_, R_)
            for c in range(1, 4):
                tt("dve", RKB3[:, c, :], RKB3[:, c - 1, :], psv[:, c - 1, 32:64], ALU.add, [bps] + R_, R_)
            tt("dve", CNTB, RKB3[:, 3, :], psv[:, 3, 32:64], ALU.add, [bps] + R_, [B_CNT])
            tt("dve", RK3, psv[:, :, 0:32], RKB3, ALU.add, [bps] + R_, R_)
            tt("dve", TMPR3, OH1F3, RK3, ALU.mult, R_, R_)
            red(PF3[:, :, 0], TMPR3, ALU.add)
            tt("dve", TMPR3, OH2F3, RK3, ALU.mult, R_, R_)
            red(PF3[:, :, 1], TMPR3, ALU.add)
            cp("dve", POS[:, ch0:ch0 + 4, :], PF3, R_, [B_POS])
            for c in range(4):
                ch = ch0 + c
                for k in range(2):
                    P.op("pool", lambda e, ix=POS[:, ch, k:k + 1], src=X1B[ch % 8]: e.indirect_dma_start(
                        out=XS, out_offset=bass.IndirectOffsetOnAxis(ap=ix, axis=0), in_=src, in_offset=None),
                        [B_POS, B_X1B[ch % 8]], [B_XS], dma=True)

        NCHK = NTOK // 128
        ld_mgi(0)
        ld_mgi(1)
        for ch in range(3):
            ld_x(ch)
        NWB = 3
        EW1 = [view([128, 8, 512], BF16) for _ in range(2)]
        EW3 = [view([128, 8, 512], BF16) for _ in range(2)]
        EW2 = [view([128, 4, 1024], BF16) for _ in range(2)]
        B_EW = [[Buf(), Buf(), Buf()] for _ in range(NWB)]
        for e in range(2):
            dma("pool", EW1[e], ew1[e].rearrange("(kt p) n -> p kt n", p=128), (), [B_EW[e][0]])
            dma("pool", EW3[e], ew3[e].rearrange("(kt p) n -> p kt n", p=128), (), [B_EW[e][1]])
            dma("pool", EW2[e], ew2[e].rearrange("(kt p) n -> p kt n", p=128), (), [B_EW[e][2]])
        for step in range(NCHK + 2):
            if step - 2 >= 0:
                stage2(step - 2)
            if 0 <= step - 1 < NCHK:
                stage1b(step - 1)
            if step < NCHK:
                stage1(step)
            if step - 2 >= 0:
                stage2b(step - 2)
                if (step - 2) % 4 == 3:
                    stageB((step - 2) // 4)
        if debug:
            dma("sp", d_pos, POS.rearrange("p a b -> p (a b)"), [B_POS], [Buf()])
            dma("sp", d_wts, WTS.rearrange("p a b -> p (a b)"), [B_WTS], [Buf()])
        P.barrier()

        _hw.append(off[0])
        off[0] = g_mark
        NST = CAP // 128
        EW1.append(view([128, 8, 512], BF16))
        EW3.append(view([128, 8, 512], BF16))
        EW2.append(view([128, 4, 1024], BF16))
        XSL = [view([128, NST, 1024], BF16) for _ in range(2)]; B_XSL = [Buf() for _ in range(2)]
        XST = [view([128, 8, CAP], BF16) for _ in range(2)]; B_XST = [Buf() for _ in range(2)]
        HT = [view([128, 4, CAP], BF16) for _ in range(2)]; B_HT = [[Buf() for _ in range(4)] for _ in range(2)]
        S1 = [view([128, CAP]) for _ in range(2)]; B_S1 = [Buf() for _ in range(2)]
        YSB = [view([128, 1024]) for _ in range(3)]; B_YSB = [Buf() for _ in range(3)]
        B_YS = Buf()
        n_s1 = [0]
        n_y = [0]

        def p4_load(e):
            w = e % NWB
            if e >= 2:
                dma("pool", EW1[w], ew1[e].rearrange("(kt p) n -> p kt n", p=128), (), [B_EW[w][0]])
                dma("pool", EW3[w], ew3[e].rearrange("(kt p) n -> p kt n", p=128), (), [B_EW[w][1]])
                dma("pool", EW2[w], ew2[e].rearrange("(kt p) n -> p kt n", p=128), (), [B_EW[w][2]])
            dma("sp", XSL[e % 2], XS[e * CAP:(e + 1) * CAP, :].rearrange("(s p) n -> p s n", p=128), [B_XS], [B_XSL[e % 2]])

        def p4_T(e):
            p = e % 2
            for s_ in range(NST):
                pst, bpst = next_ps()
                pstb = pst[:, :].bitcast(BF16)
                for dtl in range(8):
                    tp(pstb[:, dtl * 128:(dtl + 1) * 128], XSL[p][:, s_, dtl * 128:(dtl + 1) * 128], IDB,
                       [B_XSL[p], B_CONST], [bpst])
                act(XST[p][:, :, s_ * 128:(s_ + 1) * 128], pstb.rearrange("p (a b) -> p a b", b=128), AF.Copy,
                    [bpst], [B_XST[p]])

        def p4_H(e):
            p = e % 2; w = e % NWB
            for ft in range(4):
                ps1, bps1 = next_ps()
                for kt in range(8):
                    mm(ps1[:, 0:CAP], EW1[w][:, kt, ft * 128:(ft + 1) * 128], XST[p][:, kt, :], kt == 0, kt == 7,
                       [B_EW[w][0], B_XST[p]], [bps1])
                ps3, bps3 = next_ps()
                for kt in range(8):
                    mm(ps3[:, 0:CAP], EW3[w][:, kt, ft * 128:(ft + 1) * 128], XST[p][:, kt, :], kt == 0, kt == 7,
                       [B_EW[w][1], B_XST[p]], [bps3])
                s1 = S1[n_s1[0] % 2]; bs1 = B_S1[n_s1[0] % 2]
                n_s1[0] += 1
                act(s1, ps1[:, 0:CAP], AF.Silu, [bps1], [bs1])
                tt("dve", HT[p][:, ft, :], ps3[:, 0:CAP], s1, ALU.mult, [bps3, bs1], [B_HT[p][ft]])

        def p4_Y(e):
            p = e % 2; w = e % NWB
            for s_ in range(NST):
                ysb = YSB[n_y[0] % 3]; bysb = B_YSB[n_y[0] % 3]
                n_y[0] += 1
                for half in range(2):
                    ps, bps = next_ps()
                    for ft in range(4):
                        mm(ps[:, :], HT[p][:, ft, s_ * 128:(s_ + 1) * 128], EW2[w][:, ft, half * 512:(half + 1) * 512],
                           ft == 0, ft == 3, [B_HT[p][ft], B_EW[w][2]], [bps])
                    if half == 0:
                        act(ysb[:, 0:512], ps[:, :], AF.Copy, [bps], [bysb])
                    else:
                        cp("dve", ysb[:, 512:1024], ps[:, :], [bps], [bysb])
                r0 = e * CAP + s_ * 128
                dma("sp", YS[r0:r0 + 128, :], ysb, [bysb], [B_YS])

        p4_load(0)
        p4_load(1)
        p4_T(0)
        for e in range(NEXP):
            if e + 2 < NEXP:
                p4_load(e + 2)
            p4_H(e)
            if e + 1 < NEXP:
                p4_T(e + 1)
            p4_Y(e)
        P.barrier()

        _hw.append(off[0])
        off[0] = g_mark
        NRX = 6
        NRY = 4
        L2 = view([128, 2048]); B_L2 = Buf()
        Y1 = [view([128, 1024]) for _ in range(NRY)]; B_Y1 = [Buf() for _ in range(NRY)]
        Y2 = [view([128, 1024]) for _ in range(NRY)]; B_Y2 = [Buf() for _ in range(NRY)]
        XA = [view([128, 1024]) for _ in range(NRX)]; B_XA = [Buf() for _ in range(NRX)]
        MV5 = [view([128, 4]) for _ in range(3)]; B_MV5 = [Buf() for _ in range(3)]
        JUNK5 = view([128, 1024], BF16); B_J5 = Buf()
        dma("sp", L2, rp[:, R_L2G:R_L2G + 2048].partition_broadcast(128), (), [B_L2])
        NCH5 = NTOK // 128

        def p5_load(ch):
            t0 = ch * 128
            dma("sp", XA[ch % NRX], X1S[t0:t0 + 128, :], [B_X1S], [B_XA[ch % NRX]])
            P.op("pool", lambda e, ix=POS[:, ch, 0:1], o=Y1[ch % NRY]: e.indirect_dma_start(
                out=o, out_offset=None, in_=YS, in_offset=bass.IndirectOffsetOnAxis(ap=ix, axis=0)),
                [B_POS, B_YS], [B_Y1[ch % NRY]], dma=True)
            P.op("pool", lambda e, ix=POS[:, ch, 1:2], o=Y2[ch % NRY]: e.indirect_dma_start(
                out=o, out_offset=None, in_=YS, in_offset=bass.IndirectOffsetOnAxis(ap=ix, axis=0)),
                [B_POS, B_YS], [B_Y2[ch % NRY]], dma=True)

        def p5_c1(ch):
            xa = XA[ch % NRX]; bxa = B_XA[ch % NRX]
            y1 = Y1[ch % NRY]; by1 = B_Y1[ch % NRY]; y2 = Y2[ch % NRY]; by2 = B_Y2[ch % NRY]
            act(xa, xa, AF.Identity, [bxa], [bxa], scale=ALPHA)
            stt("dve", xa, y1, WTS[:, ch, 0:1], xa, ALU.mult, ALU.add, [by1, bxa, B_WTS], [bxa])
            stt("dve", xa, y2, WTS[:, ch, 1:2], xa, ALU.mult, ALU.add, [by2, bxa, B_WTS], [bxa])

        def p5_c2(ch):
            xa = XA[ch % NRX]; bxa = B_XA[ch % NRX]
            mv5 = MV5[ch % 3]; bmv5 = B_MV5[ch % 3]
            act(JUNK5, xa, AF.Identity, [bxa, B_J5], [B_J5, bmv5], scale=1.0 / 1024.0, accum=mv5[:, 0:1])
            act(JUNK5, xa, AF.Square, [bxa, B_J5], [B_J5, bmv5], scale=1.0 / 32.0, accum=mv5[:, 1:2])
            tt("dve", mv5[:, 2:3], mv5[:, 0:1], mv5[:, 0:1], ALU.mult, [bmv5], [bmv5])
            tt("dve", mv5[:, 1:2], mv5[:, 1:2], mv5[:, 2:3], ALU.subtract, [bmv5], [bmv5])
            act(mv5[:, 2:3], mv5[:, 1:2], AF.Sqrt, [bmv5], [bmv5], bias=1e-5)
            P.op("dve", lambda e, o=mv5[:, 2:3]: e.reciprocal(out=o, in_=o), [bmv5], [bmv5])
            stt("dve", mv5[:, 3:4], mv5[:, 0:1], -1.0, mv5[:, 2:3], ALU.mult, ALU.mult, [bmv5], [bmv5])
            act(xa, xa, AF.Identity, [bxa, bmv5], [bxa], scale=mv5[:, 2:3], bias=mv5[:, 3:4])

        def p5_c3(ch):
            t0 = ch * 128
            xa = XA[ch % NRX]; bxa = B_XA[ch % NRX]
            tt("dve", xa, xa, L2[:, 0:1024], ALU.mult, [bxa, B_L2], [bxa])
            tt("dve", xa, xa, L2[:, 1024:2048], ALU.add, [bxa, B_L2], [bxa])
            dma("sp", out[t0:t0 + 128, :], xa, [bxa], [Buf()])

        for ch in range(4):
            p5_load(ch)
        for s_ in range(-2, NCH5):
            if 4 <= s_ + 4 < NCH5:
                p5_load(s_ + 4)
            if 0 <= s_ + 2 < NCH5:
                p5_c1(s_ + 2)
            if 0 <= s_ + 1 < NCH5:
                p5_c2(s_ + 1)
            if s_ >= 0:
                p5_c3(s_)
        P.barrier()

        _hw.append(off[0])
        build.hw = _hw
        with nc.Block() as block:
            P.emit(block)
    return nc


def _host_inputs(inputs):
    f = lambda k: np.ascontiguousarray(np.asarray(inputs[k], dtype=np.float32)[0])
    x = np.asarray(inputs["x"], dtype=np.float32)
    b_in = f("b_in")
    pp = np.zeros((128, 128), np.float32)

    def put(col, vec):
        n = vec.shape[0] // 128
        pp[:, col:col + n] = vec.reshape(n, 128).T

    put(P_BRX, b_in[C_RX:C_RX + 1024]); put(P_BRY, b_in[C_RY:C_RY + 1024]); put(P_BQ, b_in[C_Q:C_Q + 512])
    put(P_BK, b_in[C_K:C_K + 512]); put(P_BG, b_in[C_G:C_G + 1024])
    pp[0:16, P_BALR] = b_in[C_ALR:C_ALR + 16]
    put(P_BGA, b_in[C_GA:C_GA + 1024]); put(P_BGB, b_in[C_GB:C_GB + 1024])
    cw = f("conv_w")
    pp[:, P_CW:P_CW + 32] = cw.reshape(4, 8, 128).transpose(2, 1, 0).reshape(128, 32)
    put(P_CB, f("conv_b")); put(P_RBA, f("rg_b_a")); put(P_RBX, f("rg_b_x")); put(P_LAM, f("rg_lambda"))
    put(P_GBA, f("gla_b_a")); put(P_NG, f("gla_norm_g"))
    rp = np.zeros((1, NRP), np.float32)
    rp[0, R_BV:R_BV + 1024] = b_in[C_V:C_V + 1024]
    rp[0, R_BO:R_BO + 1024] = f("b_o"); rp[0, R_L1G:R_L1G + 1024] = f("ln1_g"); rp[0, R_L1B:R_L1B + 1024] = f("ln1_b")
    rp[0, R_L2G:R_L2G + 1024] = f("ln2_g"); rp[0, R_L2B:R_L2B + 1024] = f("ln2_b")
    rp[0, R_RB:R_RB + 4] = f("router_b_group"); rp[0, R_RB + 4:R_RB + 36] = f("router_b_expert")
    wr = np.ascontiguousarray(np.concatenate([f("router_w_group"), f("router_w_expert")], axis=1))
    rgw = np.ascontiguousarray(np.stack([f("rg_w_a"), f("rg_w_x")], axis=0))
    ii = np.arange(128)
    c_id = np.eye(128, dtype=np.float32)
    c_lt = (ii[:, None] < ii[None, :]).astype(np.float32)
    c_cm = np.tile((ii[None, :] >= ii[:, None]).astype(np.float32), (1, 4))
    c_mc = np.ones((128, 512), np.float32); c_mc[:, ::128] = 0.0
    c_eb = np.tile((np.arange(NEXP, dtype=np.float32) * CAP)[None, :], (128, 1))
    shared = {
        "w_in": f("w_in"), "pp": pp, "rp": rp, "wr": wr, "rgw": rgw, "wa2": f("gla_w_a2"),
        "wrnn": f("w_proj_rnn"), "wgla": f("w_proj_gla"), "wo": f("w_o"),
        "ew1": f("exp_w1"), "ew3": f("exp_w3"), "ew2": f("exp_w2"),
        "c_id": c_id, "c_lt": c_lt, "c_cm": c_cm, "c_mc": c_mc, "c_eb": c_eb,
    }
    in_maps = []
    for c in range(NCORES):
        xc = np.ascontiguousarray(x[2 * c:2 * c + 2].reshape(NTOK, 1024))
        m = dict(shared)
        m["xn"] = xc
        m["xT"] = np.ascontiguousarray(xc.T)
        in_maps.append(m)
    return in_maps


def kernel(**inputs):
    in_maps = _host_inputs(inputs)
    nc = build(debug=False)
    res = run_bass_kernel_spmd(nc, in_maps, core_ids=list(range(NCORES)))
    outs = [np.asarray(r["out"], dtype=np.float32).reshape(2, SEQ, 1024) for r in res.results]
    return np.concatenate(outs, axis=0)
```
